# Optimizing a Trainium2 kernel written in Bass

```python
import jax
import jax.numpy as jnp
from jax import lax
import numpy as np

D_MODEL = 2048
BATCH = 2
SEQ = 8192
DEPTH = 1

CONV_CHANNELS = D_MODEL // 2
CONV_TAPS = 31
HEAD_DIM = 128
ATTN_WIDTH = D_MODEL // 2
N_HEADS = ATTN_WIDTH // HEAD_DIM
MOBA_BLOCK = 256
MOBA_TOPK = 3
Q_CHUNK = 64
N_GROUPS = 8
EXPERTS_PER_GROUP = 8
N_EXPERTS = N_GROUPS * EXPERTS_PER_GROUP
EXPERT_TOPK = 2
D_EXPERT = D_MODEL // 4
MOE_BLOCK = 128
NORM_EPS = 1e-6
NEG_BIG = -1e30
IN_COLS = 2 * CONV_CHANNELS + 3 * ATTN_WIDTH + 2 * D_MODEL

kernel_name = 'hybrid_conformer_moba_hmoe_block'


def rms_norm(x, g):
    xf = x.astype(jnp.float32)
    y = xf * lax.rsqrt(jnp.mean(xf * xf, axis=-1, keepdims=True) + NORM_EPS)
    return (y * g.astype(jnp.float32)).astype(x.dtype)


def layer_norm(x, g, b):
    xf = x.astype(jnp.float32)
    mu = jnp.mean(xf, axis=-1, keepdims=True)
    xc = xf - mu
    y = xc * lax.rsqrt(jnp.mean(xc * xc, axis=-1, keepdims=True) + NORM_EPS)
    return (y * g.astype(jnp.float32) + b.astype(jnp.float32)).astype(x.dtype)


def alibi_slopes(n_heads):
    return jnp.exp2(-8.0 * jnp.arange(1, n_heads + 1, dtype=jnp.float32) / n_heads)


def conformer_conv(a, w_dw, b_dw, ln_g, ln_b, w_pw):
    val, gate = a[..., :CONV_CHANNELS], a[..., CONV_CHANNELS:]
    z = val * jax.nn.sigmoid(gate)
    z = lax.conv_general_dilated(
        z, w_dw[:, None, :], window_strides=(1,), padding=[(CONV_TAPS - 1, 0)],
        dimension_numbers=('NWC', 'WIO', 'NWC'),
        feature_group_count=CONV_CHANNELS) + b_dw
    z = jax.nn.silu(layer_norm(z, ln_g, ln_b))
    return z @ w_pw


def moba_attention(q, k, v):
    b, h, s, dh = q.shape
    nb = -(-s // MOBA_BLOCK)
    pad = nb * MOBA_BLOCK - s
    k_pad = jnp.pad(k, ((0, 0), (0, 0), (0, pad), (0, 0)))
    v_pad = jnp.pad(v, ((0, 0), (0, 0), (0, pad), (0, 0)))
    k_blk = k_pad.reshape(b, h, nb, MOBA_BLOCK, dh)
    v_blk = v_pad.reshape(b, h, nb, MOBA_BLOCK, dh)
    n_sel = min(MOBA_TOPK, nb)
    scale = dh ** -0.5
    slopes = alibi_slopes(h)

    k_mean = jnp.mean(k_blk.astype(jnp.float32), axis=3)
    gate = jnp.einsum('bhsd,bhnd->bhsn', q.astype(jnp.float32), k_mean)
    q_block = jnp.arange(s) // MOBA_BLOCK
    fully_past = jnp.arange(nb)[None, :] < q_block[:, None]
    gate = jnp.where(fully_past, gate, -jnp.inf)
    _, sel = lax.top_k(gate, n_sel)

    nc = s // Q_CHUNK
    qc = q.reshape(b, h, nc, Q_CHUNK, dh).transpose(2, 0, 1, 3, 4)
    selc = sel.reshape(b, h, nc, Q_CHUNK, n_sel).transpose(2, 0, 1, 3, 4)
    bi = jnp.arange(b)[:, None, None, None]
    hi = jnp.arange(h)[None, :, None, None]
    key_off = jnp.arange(MOBA_BLOCK)

    def chunk(args):
        c, qx, sx = args
        q_pos = c * Q_CHUNK + jnp.arange(Q_CHUNK)
        own = (c * Q_CHUNK) // MOBA_BLOCK
        ks = k_blk[bi, hi, sx]
        vs = v_blk[bi, hi, sx]
        s_sel = jnp.einsum('bhqd,bhqkld->bhqkl', qx, ks).astype(jnp.float32) * scale
        pos_sel = sx[..., None] * MOBA_BLOCK + key_off
        dist_sel = (q_pos[None, None, :, None, None] - pos_sel).astype(jnp.float32)
        s_sel = s_sel - slopes[None, :, None, None, None] * dist_sel
        s_sel = jnp.where((sx < own)[..., None], s_sel, NEG_BIG)
        s_sel = s_sel.reshape(b, h, Q_CHUNK, n_sel * MOBA_BLOCK)
        k_own = lax.dynamic_slice_in_dim(k_pad, own * MOBA_BLOCK, MOBA_BLOCK, axis=2)
        v_own = lax.dynamic_slice_in_dim(v_pad, own * MOBA_BLOCK, MOBA_BLOCK, axis=2)
        s_own = jnp.einsum('bhqd,bhld->bhql', qx, k_own).astype(jnp.float32) * scale
        dist_own = q_pos[:, None] - (own * MOBA_BLOCK + key_off)[None, :]
        s_own = s_own - slopes[None, :, None, None] * dist_own.astype(jnp.float32)
        s_own = jnp.where((dist_own >= 0)[None, None], s_own, NEG_BIG)
        p = jax.nn.softmax(jnp.concatenate([s_sel, s_own], axis=-1), axis=-1)
        p_sel = p[..., :n_sel * MOBA_BLOCK].reshape(b, h, Q_CHUNK, n_sel, MOBA_BLOCK).astype(v.dtype)
        p_own = p[..., n_sel * MOBA_BLOCK:].astype(v.dtype)
        return (jnp.einsum('bhqkl,bhqkld->bhqd', p_sel, vs)
                + jnp.einsum('bhql,bhld->bhqd', p_own, v_own))

    out = lax.map(chunk, (jnp.arange(nc), qc, selc))
    return out.transpose(1, 2, 0, 3, 4).reshape(b, h, s, dh)


def hierarchical_moe(xt, w_rg, b_rg, w_re, b_re, w_g, w_u, w_d):
    t, d = xt.shape
    g_prob = jax.nn.softmax((xt @ w_rg).astype(jnp.float32) + b_rg.astype(jnp.float32), axis=-1)
    g_p, g_idx = lax.top_k(g_prob, 1)
    e_logits = ((xt @ w_re).astype(jnp.float32) + b_re.astype(jnp.float32)).reshape(
        t, N_GROUPS, EXPERTS_PER_GROUP)
    e_logits = jnp.take_along_axis(e_logits, g_idx[:, :, None], axis=1)[:, 0]
    e_val, e_idx = lax.top_k(e_logits, EXPERT_TOPK)
    comb = (g_p * jax.nn.softmax(e_val, axis=-1)).reshape(-1)
    expert_id = (g_idx * EXPERTS_PER_GROUP + e_idx).reshape(-1)
    token_id = jnp.repeat(jnp.arange(t, dtype=jnp.int32), EXPERT_TOPK)

    n_assign = t * EXPERT_TOPK
    n_blocks = -(-n_assign // MOE_BLOCK) + N_EXPERTS
    n_slots = n_blocks * MOE_BLOCK
    order = jnp.argsort(expert_id)
    e_sorted = expert_id[order]
    counts = jnp.zeros((N_EXPERTS,), jnp.int32).at[expert_id].add(1)
    start = jnp.cumsum(counts) - counts
    padded = (counts + MOE_BLOCK - 1) // MOE_BLOCK * MOE_BLOCK
    pad_end = jnp.cumsum(padded)
    pad_start = pad_end - padded
    dest = pad_start[e_sorted] + jnp.arange(n_assign, dtype=jnp.int32) - start[e_sorted]
    slot_tok = jnp.zeros((n_slots,), jnp.int32).at[dest].set(token_id[order])
    slot_w = jnp.zeros((n_slots,), jnp.float32).at[dest].set(comb[order])
    block_expert = jnp.minimum(
        jnp.searchsorted(pad_end, jnp.arange(n_blocks, dtype=jnp.int32) * MOE_BLOCK, side='right'),
        N_EXPERTS - 1)

    def run_block(args):
        tok, e = args
        xb = xt[tok]
        hdn = jax.nn.silu(xb @ w_g[e]) * (xb @ w_u[e])
        return hdn @ w_d[e]

    y = lax.map(run_block, (slot_tok.reshape(n_blocks, MOE_BLOCK), block_expert))
    y = y.reshape(n_slots, d) * slot_w[:, None].astype(xt.dtype)
    return jnp.zeros_like(xt).at[slot_tok].add(y)


def setup_inputs(seed: int = 0) -> dict:
    key = jax.random.key(seed)
    ks = jax.random.split(key, 20)
    f32 = jnp.float32
    L = DEPTH

    def nrm(k, shape, fan_in):
        return jax.random.normal(k, shape, f32) * fan_in ** -0.5

    def small(k, shape, sc=0.02):
        return jax.random.normal(k, shape, f32) * sc

    return {
        'x': jax.random.normal(ks[0], (BATCH, SEQ, D_MODEL), f32),
        'norm1_g': 1.0 + small(ks[1], (L, D_MODEL)),
        'w_in': nrm(ks[2], (L, D_MODEL, IN_COLS), D_MODEL),
        'conv_dw_w': nrm(ks[3], (L, CONV_TAPS, CONV_CHANNELS), CONV_TAPS),
        'conv_dw_b': small(ks[4], (L, CONV_CHANNELS)),
        'conv_ln_g': 1.0 + small(ks[5], (L, CONV_CHANNELS)),
        'conv_ln_b': small(ks[6], (L, CONV_CHANNELS)),
        'w_conv_out': nrm(ks[7], (L, CONV_CHANNELS, D_MODEL), CONV_CHANNELS),
        'w_attn_out': nrm(ks[8], (L, ATTN_WIDTH, D_MODEL), ATTN_WIDTH),
        'gate_b': small(ks[9], (L, 2 * D_MODEL)),
        'w_out': nrm(ks[10], (L, D_MODEL, D_MODEL), D_MODEL),
        'norm2_g': 1.0 + small(ks[11], (L, D_MODEL)),
        'w_router_group': nrm(ks[12], (L, D_MODEL, N_GROUPS), D_MODEL),
        'b_router_group': small(ks[13], (L, N_GROUPS), 0.01),
        'w_router_expert': nrm(ks[14], (L, D_MODEL, N_EXPERTS), D_MODEL),
        'b_router_expert': small(ks[15], (L, N_EXPERTS), 0.01),
        'w_exp_gate': nrm(ks[16], (L, N_EXPERTS, D_MODEL, D_EXPERT), D_MODEL),
        'w_exp_up': nrm(ks[17], (L, N_EXPERTS, D_MODEL, D_EXPERT), D_MODEL),
        'w_exp_down': nrm(ks[18], (L, N_EXPERTS, D_EXPERT, D_MODEL), D_EXPERT),
        'norm_f_g': 1.0 + small(ks[19], (D_MODEL,)),
    }


def reference(x, norm1_g, w_in, conv_dw_w, conv_dw_b, conv_ln_g, conv_ln_b, w_conv_out,
              w_attn_out, gate_b, w_out, norm2_g, w_router_group, b_router_group,
              w_router_expert, b_router_expert, w_exp_gate, w_exp_up, w_exp_down, norm_f_g):
    b, s, d = x.shape
    c1 = 2 * CONV_CHANNELS
    c2 = c1 + 3 * ATTN_WIDTH
    h = x
    for l in range(DEPTH):
        u = rms_norm(h, norm1_g[l])
        proj = u @ w_in[l]
        a_conv, qkv, g_lin = proj[..., :c1], proj[..., c1:c2], proj[..., c2:]
        y_conv = conformer_conv(a_conv, conv_dw_w[l], conv_dw_b[l], conv_ln_g[l],
                                conv_ln_b[l], w_conv_out[l])
        qkv = qkv.reshape(b, s, 3, N_HEADS, HEAD_DIM).transpose(2, 0, 3, 1, 4)
        y_attn = moba_attention(qkv[0], qkv[1], qkv[2])
        y_attn = y_attn.transpose(0, 2, 1, 3).reshape(b, s, ATTN_WIDTH) @ w_attn_out[l]
        gates = jax.nn.sigmoid(g_lin + gate_b[l])
        merged = gates[..., :D_MODEL] * y_conv + gates[..., D_MODEL:] * y_attn
        h = h + merged @ w_out[l]
        hn = rms_norm(h, norm2_g[l]).reshape(b * s, d)
        y_moe = hierarchical_moe(hn, w_router_group[l], b_router_group[l], w_router_expert[l],
                                 b_router_expert[l], w_exp_gate[l], w_exp_up[l], w_exp_down[l])
        h = h + y_moe.reshape(b, s, d)
    return rms_norm(h, norm_f_g)
```

```python
import contextlib
import numpy as np
import ml_dtypes
import concourse.bass as bass
import concourse.mybir as mybir
from concourse.bass_utils import run_bass_kernel_spmd

F32 = mybir.dt.float32
BF16 = mybir.dt.bfloat16
I32 = mybir.dt.int32
U32 = mybir.dt.uint32
ALU = mybir.AluOpType
AF = mybir.ActivationFunctionType
AX = mybir.AxisListType

D = 2048
SEQ = 8192
NOWN = 2048
NCH = 16
HEADS = 8
HD = 128
CC = 1024
TAPS = 31
NBLK = 32
EPS = 1e-6
IN_COLS = 9216
C_VAL, C_GATE, C_Q, C_K, C_V, C_GC, C_GA = 0, 1024, 2048, 3072, 4096, 5120, 7168
NEXP = 64
DEXP = 512
MOE_BLOCKS = 96
NSLOT = MOE_BLOCKS * 128
VP = 132


class _Rec:
    def __init__(self):
        self.call = None

    def __getattr__(self, name):
        def f(*a, **kw):
            self.call = (name, a, kw)
            return self
        return f


def _bind(fn):
    r = _Rec()
    fn(r)
    name, a, kw = r.call
    return lambda e: getattr(e, name)(*a, **kw)


class Prog:
    CE = ("pe", "act", "dve", "pool")
    RING = 20

    def __init__(self, nc, stack):
        self.nc = nc
        self.eng = {"pe": nc.tensor, "act": nc.scalar, "dve": nc.vector,
                    "pool": nc.gpsimd, "sp": nc.sync}
        self.q = {k: [] for k in self.eng}
        self.sem = {e: stack.enter_context(nc.semaphore("c_" + e)) for e in self.CE}
        self.cnt = {e: 0 for e in self.CE}
        self.dsem = {qn: [stack.enter_context(nc.semaphore("d_%s%d" % (qn, i)))
                          for i in range(self.RING)] for qn in ("sp", "pool", "act")}
        self.dcnt = {qn: 0 for qn in self.dsem}
        self.last_w = {}
        self.readers = {}
        self.seen = {e: {} for e in self.eng}
        self.semobj = {}
        for e in self.CE:
            self.semobj[("c", e)] = self.sem[e]
        for qn in self.dsem:
            for i, s in enumerate(self.dsem[qn]):
                self.semobj[("d", qn, i)] = s

    def _deps(self, eng, reads, writes):
        deps = {}

        def add(ev):
            k, v = ev
            if k == ("c", "pe") and eng == "pe":
                return
            if deps.get(k, 0) < v:
                deps[k] = v
        for r in reads:
            if r in self.last_w:
                add(self.last_w[r])
        for w in writes:
            if w in self.last_w:
                add(self.last_w[w])
            for ev in self.readers.get(w, ()):
                add(ev)
        need = []
        for k, v in deps.items():
            if self.seen[eng].get(k, 0) < v:
                self.seen[eng][k] = v
                need.append((k, v))
        return need

    def _commit(self, ev, reads, writes):
        for r in reads:
            self.readers.setdefault(r, []).append(ev)
        for w in writes:
            self.last_w[w] = ev
            self.readers[w] = []

    def op(self, eng, fn, reads=(), writes=()):
        fn = _bind(fn)
        pr = [r for r in reads if r[:2] in ("pA", "pT", "pO")]
        if pr:
            reads = [r for r in reads if r not in pr]
            writes = list(writes) + pr
        need = self._deps(eng, reads, writes)
        self.cnt[eng] += 1
        ev = (("c", eng), self.cnt[eng])
        self.seen[eng][ev[0]] = max(self.seen[eng].get(ev[0], 0), 0)
        sem = self.sem[eng]
        waits = [(self.semobj[k], v) for k, v in need]

        def emit(e, fn=fn, waits=waits, sem=sem):
            for s, v in waits:
                e.wait_ge(s, v)
            fn(e).then_inc(sem, 1)
        self.q[eng].append(emit)
        self._commit(ev, reads, writes)
        return ev

    def dma(self, qn, fn, reads=(), writes=()):
        fn = _bind(fn)
        j = self.dcnt[qn]
        self.dcnt[qn] += 1
        slot, rnd = j % self.RING, j // self.RING
        key = ("d", qn, slot)
        need = self._deps(qn, reads, writes)
        if rnd > 0 and self.seen[qn].get(key, 0) < 16 * rnd:
            self.seen[qn][key] = 16 * rnd
            need.append((key, 16 * rnd))
        ev = (key, 16 * (rnd + 1))
        sem = self.semobj[key]
        waits = [(self.semobj[k], v) for k, v in need]

        def emit(e, fn=fn, waits=waits, sem=sem):
            for s, v in waits:
                e.wait_ge(s, v)
            fn(e).then_inc(sem, 16)
        self.q[qn].append(emit)
        self._commit(ev, reads, writes)
        return ev

    def finish(self, final_events):
        waits = {}
        for k, v in final_events:
            waits[k] = max(waits.get(k, 0), v)
        fw = [(self.semobj[k], v) for k, v in waits.items()]
        q = self.q
        with self.nc.Block() as block:
            @block.tensor
            def _(e):
                for f in q["pe"]:
                    f(e)

            @block.scalar
            def _(e):
                for f in q["act"]:
                    f(e)

            @block.vector
            def _(e):
                for f in q["dve"]:
                    f(e)

            @block.gpsimd
            def _(e):
                for f in q["pool"]:
                    f(e)

            @block.sync
            def _(e):
                for f in q["sp"]:
                    f(e)
                for s, v in fw:
                    e.wait_ge(s, v)


    def barrier(self):
        latest = {}
        for e in self.CE:
            if self.cnt[e]:
                latest[("c", e)] = self.cnt[e]
        for qn in self.dsem:
            for j in range(max(0, self.dcnt[qn] - self.RING), self.dcnt[qn]):
                k = ("d", qn, j % self.RING)
                latest[k] = max(latest.get(k, 0), 16 * (j // self.RING + 1))
        for eng in self.eng:
            need = []
            for k, v in latest.items():
                if k == ("c", eng):
                    continue
                if self.seen[eng].get(k, 0) < v:
                    self.seen[eng][k] = v
                    need.append((self.semobj[k], v))
            if need:
                def emit(e, need=need):
                    for s_, v in need:
                        e.wait_ge(s_, v)
                self.q[eng].append(emit)
        self.last_w = {}
        self.readers = {}


def build(stage=4, debug=False):
    nc = bass.Bass("TRN2", target_bir_lowering=False)

    def din(name, shape, dt=F32):
        return nc.dram_tensor(name, list(shape), dt, kind="ExternalInput").ap()

    dbg_out = {1: ("kt_scr", "v_scr"), 2: ("h_scr", "hn_scr"), 2.5: ("xs_scr", "hn_scr"), 3: ("ys_scr", "xs_scr", "hn_scr", "h_scr")}.get(stage, ()) if debug else ()

    def dscr(name, shape, dt):
        return nc.dram_tensor(name, list(shape), dt, kind=("ExternalOutput" if name in dbg_out else "Internal")).ap()

    xw = din("xw", [SEQ, D])
    w_in = din("w_in", [D, IN_COLS])
    w_co = din("w_conv_out", [CC, D])
    w_ao = din("w_attn_out", [CC, D])
    w_out = din("w_out", [D, D])
    if stage >= 3:
        w_eg = din("w_exp_gate", [NEXP, D, DEXP])
        w_eu = din("w_exp_up", [NEXP, D, DEXP])
        w_ed = din("w_exp_down", [NEXP, DEXP, D])
    g1_d = din("norm1_g", [1, D])
    g2_d = din("norm2_g", [1, D])
    gf_d = din("norm_f_g", [1, D])
    dww_d = din("dw_w", [128, 8, TAPS])
    chv_d = din("chvec", [128, 8, 3])
    gb_d = din("gate_b", [128, 32])
    wr_d = din("w_router", [128, NCH, 72])
    br_d = din("b_router", [1, 72])
    idb_d = din("ident_bf", [128, 128], BF16)
    idf_d = din("ident_f", [128, 128])
    tri_d = din("tri_bf", [128, 128], BF16)
    ust_d = din("ustrict_bf", [128, 128], BF16)
    bb_d = din("bias_tab", [128, HEADS, 3])
    fd_d = din("facd_tab", [128, HEADS, 2])
    dist_d = din("dist_tab", [128, 16, NBLK])
    gm_d = din("gmask", [128, 16, NBLK])
    pidx_d = din("pidx", [128, 1])
    bst_d = din("blkstart", [128, MOE_BLOCKS])
    out_d = nc.dram_tensor("out", [NOWN, D], F32, kind="ExternalOutput").ap()

    kt_scr = dscr("kt_scr", [HEADS, 128, SEQ], BF16)
    v_scr = dscr("v_scr", [HEADS, 128, 64, VP], BF16)
    h_scr = dscr("h_scr", [NOWN, D], F32)
    hn_scr = dscr("hn_scr", [NOWN, D], BF16)
    xs_scr = dscr("xs_scr", [NSLOT, D], BF16)
    ys_scr = dscr("ys_scr", [NSLOT, D], F32)

    w_in_v = w_in.rearrange("(c p) n -> p c n", p=128)

    with contextlib.ExitStack() as st:
        P = Prog(nc, st)

        uniq = [0]

        def sb(stack, name, shape, dt):
            uniq[0] += 1
            return stack.enter_context(nc.sbuf_tensor("s%d_%s" % (uniq[0], name), list(shape), dt))

        def psum(stack, name, shape, dt):
            return stack.enter_context(nc.psum_tensor("p_" + name, list(shape), dt))

        idb = sb(st, "idb", [128, 128], BF16)
        idf = sb(st, "idf", [128, 128], F32)
        tri = sb(st, "tri", [128, 128], BF16)
        ust = sb(st, "ust", [128, 128], BF16)
        ones_b = sb(st, "ones_b", [128, 128], BF16)
        ones_f = sb(st, "ones_f", [128, 128], F32)
        bbt = sb(st, "bbt", [128, HEADS, 3], F32)
        fdt = sb(st, "fdt", [128, HEADS, 2], F32)
        distt = sb(st, "distt", [128, 16, NBLK], F32)
        gmt = sb(st, "gmt", [128, 16, NBLK], F32)
        pidx = sb(st, "pidx", [128, 1], F32)
        bstt = sb(st, "bstt", [128, MOE_BLOCKS], F32)
        dww = sb(st, "dww", [128, 8, TAPS], F32)
        chv = sb(st, "chv", [128, 8, 3], F32)
        gbt = sb(st, "gbt", [128, 32], F32)
        wr32 = sb(st, "wr32", [128, NCH, 72], F32)
        brb = sb(st, "brb", [128, 72], F32)
        gA = sb(st, "gA", [128, D], F32)
        gB = sb(st, "gB", [128, D], F32)
        kmean = sb(st, "kmean", [128, HEADS, NBLK], F32)
        kmb = sb(st, "kmb", [128, HEADS, NBLK], BF16)
        Mall = sb(st, "Mall", [128, 16, NEXP], BF16)
        M1all = sb(st, "M1all", [128, 16, NEXP], BF16)
        M2all = sb(st, "M2all", [128, 16, NEXP], BF16)
        zhalo = sb(st, "zhalo", [128, 8, 32], F32)
        comb = sb(st, "comb", [128, 16, 2], F32)
        desti = sb(st, "desti", [128, 16, 2], I32)
        xt = [sb(st, "xt%d" % i, [128, D], F32) for i in range(2)]
        junk = sb(st, "junk", [128, D], BF16)
        xs = [sb(st, "xs%d" % i, [128, D], BF16) for i in range(2)]
        ssr = [sb(st, "ss%d" % i, [128, 1], F32) for i in range(2)]
        rsr = [sb(st, "rs%d" % i, [128, 1], F32) for i in range(2)]

        pA = [psum(st, "pA%d" % i, [128, 512], F32) for i in range(4)]
        pT = psum(st, "pT", [128, 2, 8, 128], BF16)
        pO = psum(st, "pO", [128, 2, 512], F32)
        pa_i = [0]

        def nextpA():
            i = pa_i[0] % 4
            pa_i[0] += 1
            return i

        for (t_, d_, nm) in [(idb, idb_d, "idb"), (idf, idf_d, "idf"), (tri, tri_d, "tri"), (ust, ust_d, "ust"),
                             (bbt, bb_d, "bbt"), (fdt, fd_d, "fdt"), (distt, dist_d, "distt"), (gmt, gm_d, "gmt"),
                             (pidx, pidx_d, "pidx"), (bstt, bst_d, "bstt"), (dww, dww_d, "dww"), (chv, chv_d, "chv"),
                             (gbt, gb_d, "gbt"), (wr32, wr_d, "wr32")]:
            P.dma("sp", lambda e, t_=t_, d_=d_: e.dma_start(out=t_[:], in_=d_), writes=[nm])
        P.dma("sp", lambda e: e.dma_start(out=brb[:], in_=br_d.partition_broadcast(128)), writes=["brb"])
        P.dma("sp", lambda e: e.dma_start(out=gA[:], in_=g1_d.partition_broadcast(128)), writes=["gA"])
        P.dma("sp", lambda e: e.dma_start(out=gB[:], in_=g2_d.partition_broadcast(128)), writes=["gB"])
        P.op("dve", lambda e: e.memset(ones_b[:], 1.0), writes=["ones_b"])
        P.op("dve", lambda e: e.memset(ones_f[:], 1.0), writes=["ones_f"])

        if stage == 0:
            dbg_km = nc.dram_tensor("dbg_km", [128, HEADS, NBLK], F32, kind="ExternalOutput").ap()
            P.dma("sp", lambda e: e.dma_start(out=dbg_km, in_=distt[:, 0:8, :]), reads=["distt"])
            P.barrier()
            P.finish([])
            return nc
        nt_i = [0]

        def norm_tile(src_rows, gbuf, gname, dstT, dname, col):
            i = nt_i[0] % 2
            nt_i[0] += 1
            P.dma("sp", lambda e: e.dma_start(out=xt[i][:], in_=src_rows), writes=["xt%d" % i])
            rms_scale(xt[i], "xt%d" % i, i, gbuf, gname, xs[i], "xs%d" % i)
            transp16(xs[i], "xs%d" % i, dstT, dname, col)

        def rms_scale(src, sname, i, gbuf, gname, dst, dname):
            P.op("act", lambda e: e.activation(out=junk[:], in_=src[:], func=AF.Square, accum_out=ssr[i][:]),
                 reads=[sname], writes=["junk", "ss%d" % i])
            P.op("dve", lambda e: e.tensor_scalar(out=rsr[i][:], in0=ssr[i][:], scalar1=1.0 / D, scalar2=EPS,
                                                 op0=ALU.mult, op1=ALU.add), reads=["ss%d" % i], writes=["rs%d" % i])
            P.op("act", lambda e: e.sqrt(out=rsr[i][:], in_=rsr[i][:]), reads=["rs%d" % i], writes=["rs%d" % i])
            P.op("dve", lambda e: e.reciprocal(out=rsr[i][:], in_=rsr[i][:]), reads=["rs%d" % i], writes=["rs%d" % i])
            P.op("dve", lambda e: e.scalar_tensor_tensor(out=dst[:], in0=src[:], scalar=rsr[i][:], in1=gbuf[:],
                                                        op0=ALU.mult, op1=ALU.mult),
                 reads=[sname, "rs%d" % i, gname], writes=[dname])

        def transp16(src, sname, dstT, dname, col):
            for c in range(NCH):
                P.op("pe", lambda e, c=c: e.transpose(out=pT[:, c // 8, c % 8, :], in_=src[:, c * 128:(c + 1) * 128],
                                                      identity=idb[:]),
                     reads=[sname, "idb"], writes=["pT%d" % (c // 8)])
            P.op("dve", lambda e: e.tensor_copy(out=dstT[:, 0:8, col:col + 128], in_=pT[:, 0, :, :]),
                 reads=["pT0"], writes=[dname])
            P.op("act", lambda e: e.copy(out=dstT[:, 8:16, col:col + 128], in_=pT[:, 1, :, :]),
                 reads=["pT1"], writes=[dname])

        def load_w(dst_ap, src_ap, name, nsplit=4):
            C = src_ap.shape[1]
            step = max(1, C // nsplit)
            for c0 in range(0, C, step):
                P.dma("pool", lambda e, c0=c0: e.dma_start(out=dst_ap[:, c0:c0 + step, :],
                                                          in_=src_ap[:, c0:c0 + step, :]),
                      writes=[name])

        with contextlib.ExitStack() as s1:
            wk = sb(s1, "wk", [128, NCH, 1024], BF16)
            wv = sb(s1, "wv", [128, NCH, 1024], BF16)
            uT = [sb(s1, "uT%d" % i, [128, NCH, 512], BF16) for i in range(2)]
            ktg = [sb(s1, "ktg%d" % i, [128, HEADS, 512], BF16) for i in range(2)]
            vg = [sb(s1, "vg%d" % i, [128, HEADS, 4, VP], BF16) for i in range(2)]
            load_w(wk[:], w_in_v[:, :, C_K:C_K + 1024], "wk")
            load_w(wv[:], w_in_v[:, :, C_V:C_V + 1024], "wv")
            for i in range(2):
                P.op("pool", lambda e, i=i: e.memset(vg[i][:], 1.0), writes=["vg%d" % i])
            for g in range(16 if stage >= 1 else 1):
                b = g % 2
                for t in range(4):
                    r0 = g * 512 + t * 128
                    norm_tile(xw[r0:r0 + 128, :], gA, "gA", uT[b], "uT%d" % b, t * 128)
                for h in range(HEADS):
                    k = nextpA()
                    for c in range(NCH):
                        P.op("pe", lambda e, c=c, k=k, h=h: e.matmul(pA[k][:], lhsT=wk[:, c, h * 128:(h + 1) * 128],
                                                                    rhs=uT[b][:, c, :], start=(c == 0), stop=(c == NCH - 1)),
                             reads=["wk", "uT%d" % b], writes=["pA%d" % k])
                    P.op("act", lambda e, k=k, h=h: e.copy(out=ktg[b][:, h, :], in_=pA[k][:]),
                         reads=["pA%d" % k], writes=["ktg%d" % b])
                    P.op("dve", lambda e, k=k, h=h: e.tensor_reduce(
                        out=kmean[:, h, 2 * g:2 * g + 2], in_=pA[k][:].rearrange("p (b t) -> p b t", b=2),
                        axis=AX.X, op=ALU.add), reads=["pA%d" % k], writes=["kmean"])
                for t in range(4):
                    for half in range(2):
                        k = nextpA()
                        for c in range(NCH):
                            P.op("pe", lambda e, c=c, k=k, t=t, half=half: e.matmul(
                                pA[k][:], lhsT=uT[b][:, c, t * 128:(t + 1) * 128],
                                rhs=wv[:, c, half * 512:(half + 1) * 512], start=(c == 0), stop=(c == NCH - 1)),
                                reads=["wv", "uT%d" % b], writes=["pA%d" % k])
                        eng = "act" if half == 0 else "dve"
                        if eng == "act":
                            P.op("act", lambda e, k=k, t=t, half=half: e.copy(
                                out=vg[b][:, half * 4:half * 4 + 4, t, 0:128],
                                in_=pA[k][:].rearrange("p (h d) -> p h d", h=4)),
                                reads=["pA%d" % k], writes=["vg%d" % b])
                        else:
                            P.op("dve", lambda e, k=k, t=t, half=half: e.tensor_copy(
                                out=vg[b][:, half * 4:half * 4 + 4, t, 0:128],
                                in_=pA[k][:].rearrange("p (h d) -> p h d", h=4)),
                                reads=["pA%d" % k], writes=["vg%d" % b])
                P.dma("sp", lambda e, g=g, b=b: e.dma_start(
                    out=kt_scr[:, :, g * 512:(g + 1) * 512].rearrange("h p t -> p h t"), in_=ktg[b][:]),
                    reads=["ktg%d" % b], writes=["kt_scr"])
                P.dma("sp", lambda e, g=g, b=b: e.dma_start(
                    out=v_scr[:, :, g * 4:(g + 1) * 4, :].rearrange("h p t v -> p h t v"), in_=vg[b][:]),
                    reads=["vg%d" % b], writes=["v_scr"])
            P.op("dve", lambda e: e.tensor_scalar(out=kmb[:], in0=kmean[:], scalar1=1.0 / 256.0, scalar2=None,
                                                 op0=ALU.mult), reads=["kmean"], writes=["kmb"])
        P.barrier()
        if stage == 1:
            dbg_km = nc.dram_tensor("dbg_km", [128, HEADS, NBLK], F32, kind="ExternalOutput").ap()
            P.dma("sp", lambda e: e.dma_start(out=dbg_km, in_=kmean[:]))
            P.barrier()
            P.finish([])
            return nc

        SCALE = float(HD) ** -0.5
        SLOPES = [2.0 ** (-8.0 * (i + 1) / HEADS) for i in range(HEADS)]

        with contextlib.ExitStack() as s2:
            wst = [sb(s2, "wst%d" % i, [128, NCH, 512], BF16) for i in range(2)]
            uTo = sb(s2, "uTo", [128, NCH, 512], BF16)
            qT = sb(s2, "qT", [128, HEADS, 512], BF16)
            mcT = sb(s2, "mcT", [128, NCH, 512], BF16)
            ws_i = [0]

            def stream_w(src_ap, buf=None):
                if buf is None:
                    i = ws_i[0] % 2
                    ws_i[0] += 1
                else:
                    i = buf
                C, N = src_ap.shape[1], src_ap.shape[2]
                dst = wst[i][:].rearrange("p c n -> p (c n)").rearrange("p (c n) -> p c n", c=C)
                load_w(dst, src_ap, "wst%d" % i)
                return dst, "wst%d" % i

            def lin_fm(wt, wname, col0, src, sname, K, N=512):
                k = nextpA()
                for c in range(K):
                    P.op("pe", lambda e, c=c: e.matmul(pA[k][:, 0:N], lhsT=wt[:, c, col0:col0 + 128],
                                                       rhs=src[:, c, 0:N], start=(c == 0), stop=(c == K - 1)),
                         reads=[wname, sname], writes=["pA%d" % k])
                return k

            def glu(src, sname, N, zdst):
                for i2 in range(2):
                    wv_, nv = stream_w(w_in_v[:, :, C_VAL + i2 * 512:C_VAL + (i2 + 1) * 512])
                    wg_, ng = stream_w(w_in_v[:, :, C_GATE + i2 * 512:C_GATE + (i2 + 1) * 512])
                    for c4 in range(4):
                        cc = i2 * 4 + c4
                        kv = lin_fm(wv_, nv, c4 * 128, src, sname, NCH, N)
                        kg = lin_fm(wg_, ng, c4 * 128, src, sname, NCH, N)
                        P.op("act", lambda e, kg=kg: e.activation(out=sgt[:, 0:N], in_=pA[kg][:, 0:N], func=AF.Sigmoid),
                             reads=["pA%d" % kg], writes=["sgt"])
                        P.op("dve", lambda e, kv=kv, cc=cc: e.tensor_tensor(out=zdst(cc), in0=pA[kv][:, 0:N],
                                                                           in1=sgt[:, 0:N], op=ALU.mult),
                             reads=["pA%d" % kv, "sgt"], writes=["z"])

            for gi in range(4):
                g = 12 + gi
                row0 = g * 512
                for t in range(4):
                    norm_tile(xw[row0 + t * 128:row0 + (t + 1) * 128, :], gA, "gA", uTo, "uTo", t * 128)
                for i2 in range(2):
                    wq_, nq = stream_w(w_in_v[:, :, C_Q + i2 * 512:C_Q + (i2 + 1) * 512])
                    for c4 in range(4):
                        k = lin_fm(wq_, nq, c4 * 128, uTo, "uTo", NCH)
                        P.op("act", lambda e, k=k, h=i2 * 4 + c4: e.copy(out=qT[:, h, :], in_=pA[k][:]),
                             reads=["pA%d" % k], writes=["qT"])
                with contextlib.ExitStack() as sA:
                    z = sb(sA, "z", [128, 8, 544], F32)
                    zc = sb(sA, "zc", [128, 8, 512], F32)
                    sgt = sb(sA, "sgt", [128, 512], F32)
                    sqb = sb(sA, "sqb", [128, 512], F32)
                    mean = sb(sA, "mean", [128, 512], F32)
                    rstd = sb(sA, "rstd", [128, 512], F32)
                    tmpc = [sb(sA, "tmpc%d" % i, [128, 512], F32) for i in range(2)]
                    zact = sb(sA, "zact", [128, 8, 512], BF16)
                    if gi == 0:
                        zh = sb(sA, "zh", [128, 8, 128], F32)
                        uTh = sb(sA, "uTh", [128, NCH, 128], BF16)
                        norm_tile(xw[row0 - 128:row0, :], gA, "gA", uTh, "uTh", 0)
                        glu(uTh, "uTh", 128, lambda cc: zh[:, cc, :])
                        P.op("dve", lambda e: e.tensor_copy(out=zhalo[:], in_=zh[:, :, 96:128]),
                             reads=["z"], writes=["zhalo"])
                    P.op("dve", lambda e: e.tensor_copy(out=z[:, :, 0:32], in_=zhalo[:]),
                         reads=["zhalo"], writes=["z"])
                    glu(uTo, "uTo", 512, lambda cc: z[:, cc, 32:544])
                    P.op("dve", lambda e: e.tensor_copy(out=zhalo[:], in_=z[:, :, 512:544]),
                         reads=["z"], writes=["zhalo"])
                    tmpp = sb(sA, "tmpp", [128, 512], F32)
                    for cc in range(8):
                        en = "pool" if cc in (3, 7) else "dve"
                        rn = "zc%d" % cc
                        P.op(en, lambda e, cc=cc: e.tensor_scalar(out=zc[:, cc, :], in0=z[:, cc, 2:514],
                                                                 scalar1=dww[:, cc, 0:1], scalar2=chv[:, cc, 0:1],
                                                                 op0=ALU.mult, op1=ALU.add),
                             reads=["z", "dww", "chv"], writes=[rn])
                        for kk in range(1, TAPS):
                            if en == "dve":
                                P.op(en, lambda e, cc=cc, kk=kk: e.scalar_tensor_tensor(
                                    out=zc[:, cc, :], in0=z[:, cc, 2 + kk:514 + kk], scalar=dww[:, cc, kk:kk + 1],
                                    in1=zc[:, cc, :], op0=ALU.mult, op1=ALU.add),
                                    reads=["z", rn], writes=[rn])
                            else:
                                P.op(en, lambda e, cc=cc, kk=kk: e.tensor_scalar(
                                    out=tmpp[:], in0=z[:, cc, 2 + kk:514 + kk], scalar1=dww[:, cc, kk:kk + 1],
                                    scalar2=None, op0=ALU.mult), reads=["z"], writes=["tmpp"])
                                P.op(en, lambda e, cc=cc: e.tensor_tensor(out=zc[:, cc, :], in0=zc[:, cc, :], in1=tmpp[:],
                                                                          op=ALU.add), reads=["tmpp", rn], writes=[rn])
                    k1 = nextpA()
                    for cc in range(8):
                        P.op("pe", lambda e, cc=cc: e.matmul(pA[k1][:], lhsT=ones_f[:], rhs=zc[:, cc, :],
                                                             start=(cc == 0), stop=(cc == 7)),
                             reads=["ones_f", "zc%d" % cc], writes=["pA%d" % k1])
                    k2 = nextpA()
                    for cc in range(8):
                        tb = tmpc[cc % 2]
                        P.op("act", lambda e, cc=cc, tb=tb: e.activation(out=tb[:], in_=zc[:, cc, :], func=AF.Square),
                             reads=["zc%d" % cc], writes=["tmpc%d" % (cc % 2)])
                        P.op("pe", lambda e, cc=cc, tb=tb: e.matmul(pA[k2][:], lhsT=ones_f[:], rhs=tb[:],
                                                                    start=(cc == 0), stop=(cc == 7)),
                             reads=["ones_f", "tmpc%d" % (cc % 2)], writes=["pA%d" % k2])
                    P.op("dve", lambda e: e.tensor_scalar(out=mean[:], in0=pA[k1][:], scalar1=1.0 / CC, scalar2=None,
                                                         op0=ALU.mult), reads=["pA%d" % k1], writes=["mean"])
                    P.op("dve", lambda e: e.tensor_tensor(out=sqb[:], in0=mean[:], in1=mean[:], op=ALU.mult),
                         reads=["mean"], writes=["sqb"])
                    P.op("dve", lambda e: e.scalar_tensor_tensor(out=rstd[:], in0=pA[k2][:], scalar=1.0 / CC, in1=sqb[:],
                                                                op0=ALU.mult, op1=ALU.subtract),
                         reads=["pA%d" % k2, "sqb"], writes=["rstd"])
                    P.op("dve", lambda e: e.tensor_scalar(out=rstd[:], in0=rstd[:], scalar1=EPS, scalar2=None,
                                                         op0=ALU.add), reads=["rstd"], writes=["rstd"])
                    P.op("act", lambda e: e.sqrt(out=rstd[:], in_=rstd[:]), reads=["rstd"], writes=["rstd"])
                    P.op("dve", lambda e: e.reciprocal(out=rstd[:], in_=rstd[:]), reads=["rstd"], writes=["rstd"])
                    for cc in range(8):
                        tb = tmpc[cc % 2]
                        tn = "tmpc%d" % (cc % 2)
                        P.op("dve", lambda e, cc=cc, tb=tb: e.tensor_tensor(out=tb[:], in0=zc[:, cc, :], in1=mean[:],
                                                                            op=ALU.subtract),
                             reads=["zc%d" % cc, "mean"], writes=[tn])
                        P.op("dve", lambda e, tb=tb: e.tensor_tensor(out=tb[:], in0=tb[:], in1=rstd[:], op=ALU.mult),
                             reads=[tn, "rstd"], writes=[tn])
                        P.op("act", lambda e, cc=cc, tb=tb: e.activation(out=zact[:, cc, :], in_=tb[:], func=AF.Silu,
                                                                         bias=chv[:, cc, 2:3], scale=chv[:, cc, 1:2]),
                             reads=[tn, "chv"], writes=["zact"])
                    w_co_v = w_co.rearrange("(c p) n -> p c n", p=128)
                    for half in range(2):
                        wco_, nco = stream_w(w_co_v[:, :, half * 1024:(half + 1) * 1024], buf=0)
                        for q2 in range(2):
                            gcol = C_GC + (half * 2 + q2) * 512
                            wgc_, ngc = stream_w(w_in_v[:, :, gcol:gcol + 512], buf=1)
                            for d4 in range(4):
                                dc = half * 8 + q2 * 4 + d4
                                ky = lin_fm(wco_, nco, (q2 * 4 + d4) * 128, zact, "zact", 8)
                                kg = lin_fm(wgc_, ngc, d4 * 128, uTo, "uTo", NCH)
                                P.op("act", lambda e, kg=kg, dc=dc: e.activation(out=sgt[:], in_=pA[kg][:], func=AF.Sigmoid,
                                                                                 bias=gbt[:, dc:dc + 1]),
                                     reads=["pA%d" % kg, "gbt"], writes=["sgt"])
                                P.op("dve", lambda e, ky=ky, dc=dc: e.tensor_tensor(out=mcT[:, dc, :], in0=pA[ky][:],
                                                                                   in1=sgt[:], op=ALU.mult),
                                     reads=["pA%d" % ky, "sgt"], writes=["mcT"])
                    if stage == 1.3:
                        dbg_mc = nc.dram_tensor("dbg_mc", [128, NCH, 512], BF16, kind="ExternalOutput").ap()
                        dbg_qt = nc.dram_tensor("dbg_qt", [128, HEADS, 512], BF16, kind="ExternalOutput").ap()
                        P.dma("sp", lambda e: e.dma_start(out=dbg_mc, in_=mcT[:]), reads=["mcT"])
                        P.dma("sp", lambda e: e.dma_start(out=dbg_qt, in_=qT[:]), reads=["qT"])
                        P.barrier()
                        P.finish([])
                        return nc
                    P.barrier()
                with contextlib.ExitStack() as sB:
                    KT = sb(sB, "KT", [128, SEQ], BF16)
                    VH = sb(sB, "VH", [128, 64, VP], BF16)
                    PT = [sb(sB, "PT%d" % i, [128, 512], BF16) for i in range(4)]
                    acc = sb(sB, "acc", [128, 4, 129], F32)
                    rec = sb(sB, "rec", [128, 4], F32)
                    gs = sb(sB, "gs", [128, 4, NBLK], F32)
                    top8 = sb(sB, "top8", [128, 4, 8], F32)
                    fac = sb(sB, "fac", [128, 4, NBLK], F32)
                    wsel = sb(sB, "wsel", [128, 4, NBLK], F32)
                    atok = sb(sB, "atok", [128, 4, CC], BF16)
                    attnT = sb(sB, "attnT", [128, HEADS, 512], BF16)
                    pt_i = [0]
                    po_i = [0]
                    nkeys = (g + 1) * 512

                    def s_exp(h, ktile, q0, q1, bcol):
                        k = nextpA()
                        P.op("pe", lambda e: e.matmul(pA[k][:, q0:q1], lhsT=KT[:, ktile * 128:(ktile + 1) * 128],
                                                      rhs=qT[:, h, q0:q1], start=True, stop=True),
                             reads=["KT", "qT"], writes=["pA%d" % k])
                        pi = pt_i[0] % 4
                        pt_i[0] += 1
                        P.op("act", lambda e: e.activation(out=PT[pi][:, q0:q1], in_=pA[k][:, q0:q1], func=AF.Exp,
                                                           bias=bbt[:, h, bcol:bcol + 1], scale=SCALE),
                             reads=["pA%d" % k, "bbt"], writes=["PT%d" % pi])
                        return pi

                    def pv_acc(pairs, qt, wap, wname):
                        r = po_i[0] % 2
                        po_i[0] += 1
                        oap = pO[:, r, 0:129]
                        for j, (pi, ktile) in enumerate(pairs):
                            P.op("pe", lambda e, j=j, pi=pi, ktile=ktile: e.matmul(
                                oap, lhsT=PT[pi][:, qt * 128:(qt + 1) * 128], rhs=VH[:, ktile, 0:129],
                                start=(j == 0), stop=(j == len(pairs) - 1)),
                                reads=["PT%d" % pi, "VH"], writes=["pO%d" % r])
                        P.op("dve", lambda e: e.scalar_tensor_tensor(out=acc[:, qt, :], in0=oap, scalar=wap,
                                                                    in1=acc[:, qt, :], op0=ALU.mult, op1=ALU.add),
                             reads=["pO%d" % r, wname, "acc"], writes=["acc"])

                    for h in range(HEADS):
                        P.dma("sp", lambda e, h=h: e.dma_start(out=KT[:, 0:nkeys], in_=kt_scr[h, :, 0:nkeys]),
                              reads=["kt_scr"], writes=["KT"])
                        P.dma("sp", lambda e, h=h: e.dma_start(out=VH[:, 0:nkeys // 128, :],
                                                              in_=v_scr[h, :, 0:nkeys // 128, :]),
                              reads=["v_scr"], writes=["VH"])
                        P.op("pool", lambda e: e.memset(acc[:], 0.0), writes=["acc"])
                        kq = nextpA()
                        for qt in range(4):
                            P.op("pe", lambda e, qt=qt: e.matmul(pA[kq][:, qt * NBLK:(qt + 1) * NBLK],
                                                                 lhsT=qT[:, h, qt * 128:(qt + 1) * 128], rhs=kmb[:, h, :],
                                                                 start=True, stop=True),
                                 reads=["qT", "kmb"], writes=["pA%d" % kq])
                        P.op("dve", lambda e: e.tensor_tensor(out=gs[:], in0=pA[kq][:, 0:4 * NBLK].rearrange("p (a b) -> p a b", a=4),
                                                              in1=gmt[:, gi * 4:gi * 4 + 4, :], op=ALU.add),
                             reads=["pA%d" % kq, "gmt"], writes=["gs"])
                        for qt in range(4):
                            P.op("dve", lambda e, qt=qt: e.max(out=top8[:, qt, :], in_=gs[:, qt, :]),
                                 reads=["gs"], writes=["top8"])
                        P.op("dve", lambda e: e.tensor_scalar(out=top8[:, :, 2:3], in0=top8[:, :, 2:3], scalar1=-1e29,
                                                             scalar2=None, op0=ALU.max), reads=["top8"], writes=["top8"])
                        P.op("act", lambda e, h=h: e.activation(out=fac[:], in_=distt[:, gi * 4:gi * 4 + 4, :], func=AF.Exp,
                                                                scale=-SLOPES[h]), reads=["distt"], writes=["fac"])
                        for qt in range(4):
                            P.op("dve", lambda e, qt=qt: e.scalar_tensor_tensor(
                                out=wsel[:, qt, :], in0=gs[:, qt, :], scalar=top8[:, qt, 2:3], in1=fac[:, qt, :],
                                op0=ALU.is_ge, op1=ALU.mult), reads=["gs", "top8", "fac"], writes=["wsel"])
                        for n in range(2 * g):
                            pis = [s_exp(h, 2 * n + kt, 0, 512, kt) for kt in range(2)]
                            for qt in range(4):
                                pv_acc([(pis[0], 2 * n), (pis[1], 2 * n + 1)], qt, wsel[:, qt, n:n + 1], "wsel")
                        n = 2 * g
                        pis = [s_exp(h, 2 * n + kt, 256, 512, kt) for kt in range(2)]
                        for qt in (2, 3):
                            pv_acc([(pis[0], 2 * n), (pis[1], 2 * n + 1)], qt, wsel[:, qt, n:n + 1], "wsel")
                        for (qt, ktile, diag) in [(0, 4 * g, True), (1, 4 * g, False), (1, 4 * g + 1, True),
                                                  (2, 4 * g + 2, True), (3, 4 * g + 2, False), (3, 4 * g + 3, True)]:
                            pi = s_exp(h, ktile, qt * 128, (qt + 1) * 128, 2 if diag else 1)
                            if diag:
                                P.op("pool", lambda e, pi=pi, qt=qt: e.tensor_tensor(
                                    out=PT[pi][:, qt * 128:(qt + 1) * 128], in0=PT[pi][:, qt * 128:(qt + 1) * 128],
                                    in1=tri[:], op=ALU.mult), reads=["PT%d" % pi, "tri"], writes=["PT%d" % pi])
                            pv_acc([(pi, ktile)], qt, fdt[:, h, 0:1] if diag else fdt[:, h, 1:2], "fdt")
                        P.op("dve", lambda e: e.reciprocal(out=rec[:], in_=acc[:, :, 128]), reads=["acc"], writes=["rec"])
                        for qt in range(4):
                            P.op("dve", lambda e, qt=qt, h=h: e.tensor_scalar(
                                out=atok[:, qt, h * 128:(h + 1) * 128], in0=acc[:, qt, 0:128], scalar1=rec[:, qt:qt + 1],
                                scalar2=None, op0=ALU.mult), reads=["acc", "rec"], writes=["atok"])
                    for qt in range(4):
                        bk = qt % 2
                        for h in range(HEADS):
                            P.op("pe", lambda e, qt=qt, h=h, bk=bk: e.transpose(
                                out=pT[:, bk, h, :], in_=atok[:, qt, h * 128:(h + 1) * 128], identity=idb[:]),
                                reads=["atok", "idb"], writes=["pT%d" % bk])
                        P.op("act", lambda e, qt=qt, bk=bk: e.copy(out=attnT[:, :, qt * 128:(qt + 1) * 128], in_=pT[:, bk, :, :]),
                             reads=["pT%d" % bk], writes=["attnT"])
                    sgt2 = sb(sB, "sgt2", [128, 512], F32)
                    tmpm = sb(sB, "tmpm", [128, 512], F32)
                    w_ao_v = w_ao.rearrange("(c p) n -> p c n", p=128)
                    for half in range(2):
                        wao_, nao = stream_w(w_ao_v[:, :, half * 1024:(half + 1) * 1024], buf=0)
                        for q2 in range(2):
                            gcol = C_GA + (half * 2 + q2) * 512
                            wga_, nga = stream_w(w_in_v[:, :, gcol:gcol + 512], buf=1)
                            for d4 in range(4):
                                dc = half * 8 + q2 * 4 + d4
                                ky = lin_fm(wao_, nao, (q2 * 4 + d4) * 128, attnT, "attnT", 8)
                                kg = lin_fm(wga_, nga, d4 * 128, uTo, "uTo", NCH)
                                P.op("act", lambda e, kg=kg, dc=dc: e.activation(out=sgt2[:], in_=pA[kg][:], func=AF.Sigmoid,
                                                                                 bias=gbt[:, 16 + dc:17 + dc]),
                                     reads=["pA%d" % kg, "gbt"], writes=["sgt2"])
                                P.op("dve", lambda e, ky=ky: e.tensor_tensor(out=tmpm[:], in0=pA[ky][:], in1=sgt2[:],
                                                                            op=ALU.mult),
                                     reads=["pA%d" % ky, "sgt2"], writes=["tmpm"])
                                P.op("dve", lambda e, dc=dc: e.tensor_tensor(out=mcT[:, dc, :], in0=tmpm[:], in1=mcT[:, dc, :],
                                                                            op=ALU.add),
                                     reads=["tmpm", "mcT"], writes=["mcT"])
                    if stage == 1.6:
                        dbg_mc = nc.dram_tensor("dbg_mc", [128, NCH, 512], BF16, kind="ExternalOutput").ap()
                        dbg_at = nc.dram_tensor("dbg_at", [128, HEADS, 512], BF16, kind="ExternalOutput").ap()
                        P.dma("sp", lambda e: e.dma_start(out=dbg_mc, in_=mcT[:]), reads=["mcT"])
                        P.dma("sp", lambda e: e.dma_start(out=dbg_at, in_=attnT[:]), reads=["attnT"])
                        P.barrier()
                        P.finish([])
                        return nc
                    P.barrier()
                with contextlib.ExitStack() as sC:
                    hj = [sb(sC, "hj%d" % i, [128, 4, 512], F32) for i in range(2)]
                    hn32 = sb(sC, "hn32", [128, D], F32)
                    hnT = sb(sC, "hnT", [128, NCH, 128], F32)
                    lg = sb(sC, "lg", [128, 72], F32)
                    sm = sb(sC, "sm", [128, 16], F32)
                    oh = sb(sC, "oh", [128, 8], F32)
                    el = sb(sC, "el", [128, 8], F32)
                    t64 = sb(sC, "t64", [128, 8, 8], F32)
                    e8 = sb(sC, "e8", [128, 8], F32)
                    m12 = sb(sC, "m12", [128, 2, 8], F32)
                    w_out_v = w_out.rearrange("(c p) n -> p c n", p=128)
                    for j in range(4):
                        wo_, no = stream_w(w_out_v[:, :, j * 512:(j + 1) * 512])
                        hb = hj[j % 2]
                        hbn = "hj%d" % (j % 2)
                        P.dma("sp", lambda e, j=j, hb=hb: e.dma_start(
                            out=hb[:], in_=xw[row0:row0 + 512, j * 512:(j + 1) * 512].rearrange("(t p) c -> p t c", p=128)),
                            writes=[hbn])
                        for t in range(4):
                            k = nextpA()
                            for c in range(NCH):
                                P.op("pe", lambda e, c=c, k=k, t=t: e.matmul(pA[k][:], lhsT=mcT[:, c, t * 128:(t + 1) * 128],
                                                                            rhs=wo_[:, c, :], start=(c == 0), stop=(c == NCH - 1)),
                                     reads=["mcT", no], writes=["pA%d" % k])
                            P.op("dve", lambda e, k=k, t=t, hb=hb: e.tensor_tensor(out=hb[:, t, :], in0=pA[k][:], in1=hb[:, t, :],
                                                                                  op=ALU.add),
                                 reads=["pA%d" % k, hbn], writes=[hbn])
                        P.dma("sp", lambda e, j=j, hb=hb: e.dma_start(
                            out=h_scr[gi * 512:(gi + 1) * 512, j * 512:(j + 1) * 512].rearrange("(t p) c -> p t c", p=128),
                            in_=hb[:]), reads=[hbn], writes=["h_scr"])
                    for t in range(4):
                        ti = gi * 4 + t
                        i = nt_i[0] % 2
                        nt_i[0] += 1
                        r_ = gi * 512 + t * 128
                        P.dma("sp", lambda e, i=i, r_=r_: e.dma_start(out=xt[i][:], in_=h_scr[r_:r_ + 128, :]),
                              reads=["h_scr"], writes=["xt%d" % i])
                        rms_scale(xt[i], "xt%d" % i, i, gB, "gB", hn32, "hn32")
                        P.op("act", lambda e, i=i: e.copy(out=xs[i][:], in_=hn32[:]), reads=["hn32"], writes=["xs%d" % i])
                        P.dma("sp", lambda e, i=i, r_=r_: e.dma_start(out=hn_scr[r_:r_ + 128, :], in_=xs[i][:]),
                              reads=["xs%d" % i], writes=["hn_scr"])
                        for c4 in range(4):
                            k = nextpA()
                            for c1 in range(4):
                                c = c4 * 4 + c1
                                P.op("pe", lambda e, c=c, c1=c1, k=k: e.transpose(out=pA[k][:, c1 * 128:(c1 + 1) * 128],
                                                                                  in_=hn32[:, c * 128:(c + 1) * 128], identity=idf[:]),
                                     reads=["hn32", "idf"], writes=["pA%d" % k])
                            P.op("dve" if c4 % 2 else "act",
                                 (lambda e, k=k, c4=c4: e.tensor_copy(out=hnT[:, c4 * 4:c4 * 4 + 4, :],
                                                                      in_=pA[k][:].rearrange("p (a b) -> p a b", a=4))) if c4 % 2 else
                                 (lambda e, k=k, c4=c4: e.copy(out=hnT[:, c4 * 4:c4 * 4 + 4, :],
                                                               in_=pA[k][:].rearrange("p (a b) -> p a b", a=4))),
                                 reads=["pA%d" % k], writes=["hnT"])
                        k = nextpA()
                        for c in range(NCH):
                            P.op("pe", lambda e, c=c, k=k: e.matmul(pA[k][:, 0:72], lhsT=hnT[:, c, :], rhs=wr32[:, c, :],
                                                                    start=(c == 0), stop=(c == NCH - 1)),
                                 reads=["hnT", "wr32"], writes=["pA%d" % k])
                        P.op("dve", lambda e, k=k: e.tensor_tensor(out=lg[:], in0=pA[k][:, 0:72], in1=brb[:], op=ALU.add),
                             reads=["pA%d" % k, "brb"], writes=["lg"])
                        P.op("dve", lambda e: e.max(out=e8[:], in_=lg[:, 0:8]), reads=["lg"], writes=["e8"])
                        P.op("dve", lambda e: e.tensor_scalar(out=oh[:], in0=lg[:, 0:8], scalar1=e8[:, 0:1], scalar2=None,
                                                             op0=ALU.is_ge), reads=["lg", "e8"], writes=["oh"])
                        P.op("dve", lambda e: e.tensor_scalar(out=sm[:, 0:1], in0=e8[:, 0:1], scalar1=-1.0, scalar2=None,
                                                             op0=ALU.mult), reads=["e8"], writes=["sm"])
                        P.op("act", lambda e: e.activation(out=sm[:, 8:16], in_=lg[:, 0:8], func=AF.Exp, bias=sm[:, 0:1],
                                                           accum_out=sm[:, 1:2]), reads=["lg", "sm"], writes=["sm"])
                        P.op("dve", lambda e: e.tensor_tensor(out=t64[:], in0=lg[:, 8:72].rearrange("p (g e) -> p g e", g=8),
                                                              in1=oh[:].unsqueeze(2).to_broadcast([128, 8, 8]), op=ALU.mult),
                             reads=["lg", "oh"], writes=["t64"])
                        P.op("dve", lambda e: e.tensor_reduce(out=el[:], in_=t64[:].rearrange("p g e -> p e g"), axis=AX.X,
                                                              op=ALU.add), reads=["t64"], writes=["el"])
                        P.op("dve", lambda e: e.max(out=e8[:], in_=el[:]), reads=["el"], writes=["e8"])
                        P.op("dve", lambda e: e.tensor_scalar(out=m12[:, 0, :], in0=el[:], scalar1=e8[:, 0:1], scalar2=None,
                                                             op0=ALU.is_equal), reads=["el", "e8"], writes=["m12"])
                        P.op("dve", lambda e: e.tensor_scalar(out=m12[:, 1, :], in0=el[:], scalar1=e8[:, 1:2], scalar2=None,
                                                             op0=ALU.is_equal), reads=["el", "e8"], writes=["m12"])
                        P.op("dve", lambda e: e.tensor_tensor(out=sm[:, 2:3], in0=e8[:, 1:2], in1=e8[:, 0:1], op=ALU.subtract),
                             reads=["e8"], writes=["sm"])
                        P.op("act", lambda e: e.activation(out=sm[:, 3:4], in_=sm[:, 2:3], func=AF.Exp),
                             reads=["sm"], writes=["sm"])
                        P.op("dve", lambda e: e.tensor_scalar(out=sm[:, 3:4], in0=sm[:, 3:4], scalar1=1.0, scalar2=None,
                                                             op0=ALU.add), reads=["sm"], writes=["sm"])
                        P.op("dve", lambda e: e.tensor_tensor(out=sm[:, 4:5], in0=sm[:, 3:4], in1=sm[:, 1:2], op=ALU.mult),
                             reads=["sm"], writes=["sm"])
                        P.op("dve", lambda e, ti=ti: e.reciprocal(out=comb[:, ti, 0:1], in_=sm[:, 4:5]),
                             reads=["sm"], writes=["comb"])
                        P.op("dve", lambda e: e.reciprocal(out=sm[:, 5:6], in_=sm[:, 1:2]), reads=["sm"], writes=["sm"])
                        P.op("dve", lambda e, ti=ti: e.tensor_tensor(out=comb[:, ti, 1:2], in0=sm[:, 5:6], in1=comb[:, ti, 0:1],
                                                                    op=ALU.subtract), reads=["sm", "comb"], writes=["comb"])
                        for kx, Mk in ((0, M1all), (1, M2all)):
                            P.op("dve", lambda e, kx=kx, Mk=Mk, ti=ti: e.tensor_tensor(
                                out=Mk[:, ti, :].rearrange("p (g e) -> p g e", g=8),
                                in0=oh[:].unsqueeze(2).to_broadcast([128, 8, 8]),
                                in1=m12[:, kx, :].unsqueeze(1).to_broadcast([128, 8, 8]), op=ALU.mult),
                                reads=["oh", "m12"], writes=["M%d" % kx])
                        P.op("dve", lambda e, ti=ti: e.tensor_tensor(out=Mall[:, ti, :], in0=M1all[:, ti, :], in1=M2all[:, ti, :],
                                                                    op=ALU.add), reads=["M0", "M1"], writes=["Mall"])
                    P.barrier()
        P.barrier()
        if stage == 2:
            dbg_cb = nc.dram_tensor("dbg_cb", [128, 16, 2], F32, kind="ExternalOutput").ap()
            dbg_m = nc.dram_tensor("dbg_m", [128, 16, NEXP], BF16, kind="ExternalOutput").ap()
            P.dma("sp", lambda e: e.dma_start(out=dbg_cb, in_=comb[:]))
            P.dma("sp", lambda e: e.dma_start(out=dbg_m, in_=Mall[:]))
            P.barrier()
            P.finish([])
            return nc
        IOA = bass.IndirectOffsetOnAxis
        with contextlib.ExitStack() as s3:
            cntf = sb(s3, "cntf", [128, NEXP], F32)
            sa = sb(s3, "sa", [128, NEXP], F32)
            sbb = sb(s3, "sbb", [128, NEXP], F32)
            padded = sb(s3, "padded", [128, NEXP], F32)
            pstart = sb(s3, "pstart", [128, NEXP], F32)
            posf = sb(s3, "posf", [128, NEXP], F32)
            tmp64 = sb(s3, "tmp64", [128, NEXP], F32)
            destf = sb(s3, "destf", [128, 16, 2], F32)
            bef = sb(s3, "bef", [128, MOE_BLOCKS], F32)
            idxw = sb(s3, "idxw", [128, MOE_BLOCKS, 4], I32)
            idx4f = sb(s3, "idx4f", [128, MOE_BLOCKS, 4], F32)
            P.op("pool", lambda e: e.memset(junk[:], 0.0), writes=["junk"])
            for b1 in range(MOE_BLOCKS):
                P.dma("sp", lambda e, b1=b1: e.dma_start(out=xs_scr[b1 * 128:(b1 + 1) * 128, :], in_=junk[:]),
                      reads=["junk"], writes=["xs_scr"])
            kc = nextpA()
            for j in range(16):
                P.op("pe", lambda e, j=j: e.matmul(pA[kc][:, 0:NEXP], lhsT=ones_b[:], rhs=Mall[:, j, :],
                                                   start=(j == 0), stop=(j == 15)),
                     reads=["ones_b", "Mall"], writes=["pA%d" % kc])
            P.op("dve", lambda e: e.tensor_copy(out=cntf[:], in_=pA[kc][:, 0:NEXP]), reads=["pA%d" % kc], writes=["cntf"])
            with contextlib.ExitStack() as s3b:
                cmpb = sb(s3b, "cmpb", [128, NEXP, 16], F32)
                P.op("dve", lambda e: e.tensor_tensor(
                    out=cmpb[:], in0=cntf[:].unsqueeze(2).to_broadcast([128, NEXP, 16]),
                    in1=bstt[:, 0:16].unsqueeze(1).to_broadcast([128, NEXP, 16]), op=ALU.is_gt),
                    reads=["cntf", "bstt"], writes=["cmpb"])
                P.op("dve", lambda e: e.tensor_reduce(out=padded[:], in_=cmpb[:], axis=AX.X, op=ALU.add),
                     reads=["cmpb"], writes=["padded"])
                P.op("dve", lambda e: e.tensor_scalar(out=padded[:], in0=padded[:], scalar1=128.0, scalar2=None,
                                                     op0=ALU.mult), reads=["padded"], writes=["padded"])
                P.barrier()
            P.op("dve", lambda e: e.tensor_copy(out=sa[:], in_=padded[:]), reads=["padded", "sbb"], writes=["sa"])
            cur, oth, cn, on = sa, sbb, "sa", "sbb"
            for sft in (1, 2, 4, 8, 16, 32):
                P.op("dve", lambda e, cur=cur, oth=oth, sft=sft: e.tensor_copy(out=oth[:, 0:sft], in_=cur[:, 0:sft]),
                     reads=[cn], writes=[on])
                P.op("dve", lambda e, cur=cur, oth=oth, sft=sft: e.tensor_tensor(
                    out=oth[:, sft:NEXP], in0=cur[:, sft:NEXP], in1=cur[:, 0:NEXP - sft], op=ALU.add),
                    reads=[cn], writes=[on])
                cur, oth, cn, on = oth, cur, on, cn
            pend, pendn = cur, cn
            P.op("dve", lambda e: e.tensor_tensor(out=pstart[:], in0=pend[:], in1=padded[:], op=ALU.subtract),
                 reads=[pendn, "padded"], writes=["pstart"])
            for ti in range(16):
                kr = nextpA()
                for j in range(ti):
                    P.op("pe", lambda e, j=j: e.matmul(pA[kr][:, 0:NEXP], lhsT=ones_b[:], rhs=Mall[:, j, :],
                                                       start=(j == 0), stop=False),
                         reads=["ones_b", "Mall"], writes=["pA%d" % kr])
                P.op("pe", lambda e, ti=ti: e.matmul(pA[kr][:, 0:NEXP], lhsT=ust[:], rhs=Mall[:, ti, :],
                                                     start=(ti == 0), stop=True),
                     reads=["ust", "Mall"], writes=["pA%d" % kr])
                P.op("dve", lambda e: e.tensor_tensor(out=posf[:], in0=pA[kr][:, 0:NEXP], in1=pstart[:], op=ALU.add),
                     reads=["pA%d" % kr, "pstart"], writes=["posf"])
                for kx, Mk in ((0, M1all), (1, M2all)):
                    P.op("dve", lambda e, Mk=Mk, ti=ti: e.tensor_tensor(out=tmp64[:], in0=posf[:], in1=Mk[:, ti, :], op=ALU.mult),
                         reads=["posf", "M%d" % kx], writes=["tmp64"])
                    P.op("dve", lambda e, ti=ti, kx=kx: e.tensor_reduce(out=destf[:, ti, kx:kx + 1], in_=tmp64[:], axis=AX.X,
                                                                        op=ALU.add), reads=["tmp64"], writes=["destf"])
            P.op("dve", lambda e: e.tensor_copy(out=desti[:], in_=destf[:]), reads=["destf"], writes=["desti"])
            with contextlib.ExitStack() as s3a:
                cmp = sb(s3a, "cmp", [128, MOE_BLOCKS, NEXP], F32)
                P.op("dve", lambda e: e.tensor_tensor(
                    out=cmp[:], in0=pend[:].unsqueeze(1).to_broadcast([128, MOE_BLOCKS, NEXP]),
                    in1=bstt[:].unsqueeze(2).to_broadcast([128, MOE_BLOCKS, NEXP]), op=ALU.is_le),
                    reads=[pendn, "bstt"], writes=["cmp"])
                P.op("dve", lambda e: e.tensor_reduce(out=bef[:], in_=cmp[:], axis=AX.X, op=ALU.add),
                     reads=["cmp"], writes=["bef"])
                P.op("dve", lambda e: e.tensor_scalar(out=bef[:], in0=bef[:], scalar1=float(NEXP - 1), scalar2=None,
                                                     op0=ALU.min), reads=["bef"], writes=["bef"])
                P.op("dve", lambda e: e.tensor_scalar(out=bef[:], in0=bef[:], scalar1=128.0, scalar2=pidx[:, 0:1],
                                                     op0=ALU.mult, op1=ALU.add), reads=["bef", "pidx"], writes=["bef"])
                for a in range(4):
                    P.op("dve", lambda e: e.tensor_scalar(out=idx4f[:, :, a], in0=bef[:], scalar1=4.0, scalar2=float(a),
                                                         op0=ALU.mult, op1=ALU.add), reads=["bef"], writes=["idx4f"])
                P.op("dve", lambda e: e.tensor_copy(out=idxw[:], in_=idx4f[:]), reads=["idx4f"], writes=["idxw"])
                P.barrier()
            for ti in range(16):
                i = ti % 2
                P.dma("sp", lambda e, ti=ti, i=i: e.dma_start(out=xs[i][:], in_=hn_scr[ti * 128:(ti + 1) * 128, :]),
                      reads=["hn_scr"], writes=["xs%d" % i])
                for kx in range(2):
                    P.dma("pool", lambda e, ti=ti, i=i, kx=kx: e.indirect_dma_start(
                        out=xs_scr, out_offset=IOA(ap=desti[:, ti, kx:kx + 1], axis=0), in_=xs[i][:], in_offset=None),
                        reads=["xs%d" % i, "desti", "xs_scr"], writes=["xs_scr%d" % (ti * 2 + kx)])
            P.barrier()
            if stage == 2.5:
                dbg_di = nc.dram_tensor("dbg_di", [128, 16, 2], I32, kind="ExternalOutput").ap()
                dbg_ix = nc.dram_tensor("dbg_ix", [128, MOE_BLOCKS, 4], I32, kind="ExternalOutput").ap()
                P.dma("sp", lambda e: e.dma_start(out=dbg_di, in_=desti[:]))
                P.dma("sp", lambda e: e.dma_start(out=dbg_ix, in_=idxw[:]))
                P.barrier()
                P.finish([])
                return nc
            wgb = [sb(s3, "wgb%d" % i, [128, 4, 2048], BF16) for i in range(2)]
            wub = [sb(s3, "wub%d" % i, [128, 4, 2048], BF16) for i in range(2)]
            wdb = [sb(s3, "wdb%d" % i, [128, 4, 2048], BF16) for i in range(2)]
            xbb = [sb(s3, "xbb%d" % i, [128, D], BF16) for i in range(2)]
            xTb = [sb(s3, "xTb%d" % i, [128, NCH, 128], BF16) for i in range(2)]
            sgm = sb(s3, "sgm", [128, DEXP], F32)
            hdn = sb(s3, "hdn", [128, DEXP], BF16)
            hTb = sb(s3, "hTb", [128, 4, 128], BF16)
            yb = [sb(s3, "yb%d" % i, [128, D], F32) for i in range(2)]
            weg_v = w_eg.rearrange("e (p a b) f -> (e p a) (b f)", a=4, b=4)
            weu_v = w_eu.rearrange("e (p a b) f -> (e p a) (b f)", a=4, b=4)
            wed_v = w_ed.rearrange("e (p c) f -> (e p c) f", c=4)
            for blk in range(MOE_BLOCKS):
                b = blk % 2
                for (dst, src, nm) in ((wgb[b], weg_v, "wgb%d" % b), (wub[b], weu_v, "wub%d" % b), (wdb[b], wed_v, "wdb%d" % b)):
                    for a in range(4):
                        P.dma("pool", lambda e: e.indirect_dma_start(
                            out=dst[:, a, :], out_offset=None, in_=src, in_offset=IOA(ap=idxw[:, blk, a:a + 1], axis=0)),
                            reads=["idxw"], writes=[nm + "_%d" % a])
                P.dma("sp", lambda e, blk=blk, b=b: e.dma_start(out=xbb[b][:], in_=xs_scr[blk * 128:(blk + 1) * 128, :]),
                      reads=["xs_scr"], writes=["xbb%d" % b])
                xv = xbb[b][:].rearrange("s (p c) -> s c p", c=16)
                for c in range(NCH):
                    P.op("pe", lambda e, c=c, xv=xv: e.transpose(out=pT[:, c // 8, c % 8, :], in_=xv[:, c, :], identity=idb[:]),
                         reads=["xbb%d" % b, "idb"], writes=["pT%d" % (c // 8)])
                P.op("dve", lambda e, b=b: e.tensor_copy(out=xTb[b][:, 0:8, :], in_=pT[:, 0, :, :]),
                     reads=["pT0"], writes=["xTb%d" % b])
                P.op("act", lambda e, b=b: e.copy(out=xTb[b][:, 8:16, :], in_=pT[:, 1, :, :]),
                     reads=["pT1"], writes=["xTb%d" % b])
                kg = nextpA()
                ku = nextpA()
                for (kk_, wt, wn) in ((kg, wgb[b], "wgb%d" % b), (ku, wub[b], "wub%d" % b)):
                    wv2 = wt[:].rearrange("p a (b f) -> p (a b) f", b=4)
                    for c in range(NCH):
                        P.op("pe", lambda e, c=c, kk_=kk_, wv2=wv2, b=b: e.matmul(pA[kk_][:], lhsT=xTb[b][:, c, :], rhs=wv2[:, c, :],
                                                                                start=(c == 0), stop=(c == NCH - 1)),
                             reads=["xTb%d" % b] + [wn + "_%d" % a for a in range(4)], writes=["pA%d" % kk_])
                P.op("act", lambda e, kg=kg: e.activation(out=sgm[:], in_=pA[kg][:], func=AF.Silu),
                     reads=["pA%d" % kg], writes=["sgm"])
                P.op("dve", lambda e, ku=ku: e.tensor_tensor(out=hdn[:], in0=pA[ku][:], in1=sgm[:], op=ALU.mult),
                     reads=["pA%d" % ku, "sgm"], writes=["hdn"])
                hv = hdn[:].rearrange("s (p c) -> s c p", c=4)
                for c in range(4):
                    P.op("pe", lambda e, c=c, hv=hv: e.transpose(out=pT[:, 0, c, :], in_=hv[:, c, :], identity=idb[:]),
                         reads=["hdn", "idb"], writes=["pT0"])
                P.op("dve", lambda e: e.tensor_copy(out=hTb[:], in_=pT[:, 0, 0:4, :]), reads=["pT0"], writes=["hTb"])
                for j in range(4):
                    k = nextpA()
                    for c in range(4):
                        P.op("pe", lambda e, c=c, k=k, j=j, b=b: e.matmul(pA[k][:], lhsT=hTb[:, c, :],
                                                                         rhs=wdb[b][:, c, j * 512:(j + 1) * 512],
                                                                         start=(c == 0), stop=(c == 3)),
                             reads=["hTb"] + ["wdb%d_%d" % (b, a) for a in range(4)], writes=["pA%d" % k])
                    if j % 2 == 0:
                        P.op("act", lambda e, k=k, j=j, b=b: e.copy(out=yb[b][:, j * 512:(j + 1) * 512], in_=pA[k][:]),
                             reads=["pA%d" % k], writes=["yb%d" % b])
                    else:
                        P.op("dve", lambda e, k=k, j=j, b=b: e.tensor_copy(out=yb[b][:, j * 512:(j + 1) * 512], in_=pA[k][:]),
                             reads=["pA%d" % k], writes=["yb%d" % b])
                P.dma("sp", lambda e, blk=blk, b=b: e.dma_start(out=ys_scr[blk * 128:(blk + 1) * 128, :], in_=yb[b][:]),
                      reads=["yb%d" % b], writes=["ys_scr"])
            P.barrier()
            if stage == 3:
                dbg_di = nc.dram_tensor("dbg_di", [128, 16, 2], I32, kind="ExternalOutput").ap()
                dbg_ix = nc.dram_tensor("dbg_ix", [128, MOE_BLOCKS, 4], I32, kind="ExternalOutput").ap()
                P.dma("sp", lambda e: e.dma_start(out=dbg_di, in_=desti[:]))
                P.dma("sp", lambda e: e.dma_start(out=dbg_ix, in_=idxw[:]))
                P.barrier()
                P.finish([])
                return nc
        finals = []
        with contextlib.ExitStack() as s4:
            yg = [[sb(s4, "yg%d_%d" % (i, kx), [128, D], F32) for kx in range(2)] for i in range(2)]
            of = [sb(s4, "of%d" % i, [128, D], F32) for i in range(2)]
            P.dma("sp", lambda e: e.dma_start(out=gA[:], in_=gf_d.partition_broadcast(128)), writes=["gA"])
            for ti in range(16):
                i = ti % 2
                P.dma("sp", lambda e, ti=ti, i=i: e.dma_start(out=xt[i][:], in_=h_scr[ti * 128:(ti + 1) * 128, :]),
                      reads=["h_scr"], writes=["xt%d" % i])
                for kx in range(2):
                    P.dma("pool", lambda e, ti=ti, i=i, kx=kx: e.indirect_dma_start(
                        out=yg[i][kx][:], out_offset=None, in_=ys_scr, in_offset=IOA(ap=desti[:, ti, kx:kx + 1], axis=0)),
                        reads=["ys_scr", "desti"], writes=["yg%d_%d" % (i, kx)])
                for kx in range(2):
                    P.op("dve", lambda e, ti=ti, i=i, kx=kx: e.scalar_tensor_tensor(
                        out=xt[i][:], in0=yg[i][kx][:], scalar=comb[:, ti, kx:kx + 1], in1=xt[i][:],
                        op0=ALU.mult, op1=ALU.add), reads=["yg%d_%d" % (i, kx), "comb", "xt%d" % i], writes=["xt%d" % i])
                rms_scale(xt[i], "xt%d" % i, i, gA, "gA", of[i], "of%d" % i)
                finals.append(P.dma("sp", lambda e, ti=ti, i=i: e.dma_start(out=out_d[ti * 128:(ti + 1) * 128, :], in_=of[i][:]),
                                    reads=["of%d" % i]))
        P.finish(finals)
    return nc


def _consts():
    p = np.arange(128, dtype=np.float64)
    slopes = np.array([2.0 ** (-8.0 * (i + 1) / HEADS) for i in range(HEADS)])
    ident = np.eye(128)
    tri = (p[:, None] <= p[None, :]).astype(np.float64)
    ust = (p[:, None] < p[None, :]).astype(np.float64)
    bias = np.stack([slopes[None, :] * (p[:, None] - 255.0), slopes[None, :] * (p[:, None] - 127.0),
                     slopes[None, :] * p[:, None]], axis=-1)
    facd = np.stack([np.exp(-slopes[None, :] * p[:, None]), np.exp(-slopes[None, :] * (p[:, None] + 1.0))], axis=-1)
    qpos = 6144.0 + np.arange(16)[None, :, None] * 128.0 + p[:, None, None]
    kend = (np.arange(NBLK) * 256.0 + 255.0)[None, None, :]
    dist = np.maximum(qpos - kend, 0.0)
    return dict(
        ident_bf=ident.astype(ml_dtypes.bfloat16), ident_f=ident.astype(np.float32),
        tri_bf=tri.astype(ml_dtypes.bfloat16), ustrict_bf=ust.astype(ml_dtypes.bfloat16),
        bias_tab=bias.astype(np.float32), facd_tab=facd.astype(np.float32), dist_tab=dist.astype(np.float32),
        pidx=p.astype(np.float32)[:, None],
        blkstart=np.broadcast_to((np.arange(MOE_BLOCKS) * 128.0)[None, :], (128, MOE_BLOCKS)).astype(np.float32).copy())


def kernel(x, norm1_g, w_in, conv_dw_w, conv_dw_b, conv_ln_g, conv_ln_b, w_conv_out, w_attn_out, gate_b, w_out,
           norm2_g, w_router_group, b_router_group, w_router_expert, b_router_expert, w_exp_gate, w_exp_up,
           w_exp_down, norm_f_g):
    f = lambda a: np.ascontiguousarray(np.asarray(a, dtype=np.float32))
    x = f(x)
    fm = lambda v, n: f(v).reshape(n, 128).T.copy()
    shared = dict(
        w_in=f(w_in)[0], w_conv_out=f(w_conv_out)[0], w_attn_out=f(w_attn_out)[0], w_out=f(w_out)[0],
        w_exp_gate=f(w_exp_gate)[0], w_exp_up=f(w_exp_up)[0], w_exp_down=f(w_exp_down)[0],
        norm1_g=f(norm1_g).reshape(1, D), norm2_g=f(norm2_g).reshape(1, D), norm_f_g=f(norm_f_g).reshape(1, D),
        dw_w=np.ascontiguousarray(f(conv_dw_w)[0].T.reshape(8, 128, TAPS).transpose(1, 0, 2)),
        chvec=np.ascontiguousarray(np.stack([fm(conv_dw_b, 8), fm(conv_ln_g, 8), fm(conv_ln_b, 8)], axis=-1)),
        gate_b=fm(gate_b, 32),
        w_router=np.ascontiguousarray(np.concatenate([f(w_router_group)[0], f(w_router_expert)[0]], axis=1)
                                      .reshape(NCH, 128, 72).transpose(1, 0, 2)),
        b_router=np.concatenate([f(b_router_group).reshape(-1), f(b_router_expert).reshape(-1)])[None, :].copy(),
    )
    shared.update(_consts())
    in_maps = []
    for c in range(8):
        b, r = c // 4, c % 4
        xwin = np.zeros((SEQ, D), np.float32)
        n_valid = (r + 1) * NOWN
        xwin[SEQ - n_valid:] = x[b, :n_valid]
        first_valid_blk = (SEQ - n_valid) // 256
        gm = np.full((128, 16, NBLK), -1e30, np.float32)
        for qt in range(16):
            own = (6144 + qt * 128) // 256
            gm[:, qt, first_valid_blk:own] = 0.0
        m = dict(shared)
        m["xw"] = xwin
        m["gmask"] = gm
        in_maps.append(m)
    nc = build()
    res = run_bass_kernel_spmd(nc, in_maps, core_ids=list(range(8)))
    out = np.zeros((2, SEQ, D), np.float32)
    for c in range(8):
        b, r = c // 4, c % 4
        out[b, r * NOWN:(r + 1) * NOWN] = res.results[c]["out"]
    return out
```

```python
import contextlib
import numpy as np
import ml_dtypes
import concourse.bass as bass
import concourse.mybir as mybir
from concourse.bass_utils import run_bass_kernel_spmd

F32 = mybir.dt.float32
BF16 = mybir.dt.bfloat16
I32 = mybir.dt.int32
U32 = mybir.dt.uint32
ALU = mybir.AluOpType
AF = mybir.ActivationFunctionType
AX = mybir.AxisListType

D = 2048
SEQ = 8192
NOWN = 2048
NCH = 16
HEADS = 8
HD = 128
CC = 1024
TAPS = 31
NBLK = 32
EPS = 1e-6
IN_COLS = 9216
C_VAL, C_GATE, C_Q, C_K, C_V, C_GC, C_GA = 0, 1024, 2048, 3072, 4096, 5120, 7168
NEXP = 64
DEXP = 512
MOE_BLOCKS = 96
NSLOT = MOE_BLOCKS * 128
VP = 132


class _Rec:
    def __init__(self):
        self.call = None

    def __getattr__(self, name):
        def f(*a, **kw):
            self.call = (name, a, kw)
            return self
        return f


def _bind(fn):
    r = _Rec()
    fn(r)
    name, a, kw = r.call
    return lambda e: getattr(e, name)(*a, **kw)


class Prog:
    CE = ("pe", "act", "dve", "pool")
    RING = 20

    def __init__(self, nc, stack):
        self.nc = nc
        self.eng = {"pe": nc.tensor, "act": nc.scalar, "dve": nc.vector,
                    "pool": nc.gpsimd, "sp": nc.sync}
        self.q = {k: [] for k in self.eng}
        self.sem = {e: stack.enter_context(nc.semaphore("c_" + e)) for e in self.CE}
        self.cnt = {e: 0 for e in self.CE}
        self.dsem = {qn: [stack.enter_context(nc.semaphore("d_%s%d" % (qn, i)))
                          for i in range(self.RING)] for qn in ("sp", "pool", "act")}
        self.dcnt = {qn: 0 for qn in self.dsem}
        self.last_w = {}
        self.readers = {}
        self.groups = {}
        self.seen = {e: {} for e in self.eng}
        self.semobj = {}
        for e in self.CE:
            self.semobj[("c", e)] = self.sem[e]
        for qn in self.dsem:
            for i, s in enumerate(self.dsem[qn]):
                self.semobj[("d", qn, i)] = s

    def _expand(self, names):
        out = []
        for n in names:
            out.extend(self.groups.get(n, (n,)))
        return out

    def _deps(self, eng, reads, writes):
        reads = self._expand(reads)
        writes = self._expand(writes)
        deps = {}

        def add(ev):
            k, v = ev
            if k == ("c", "pe") and eng == "pe":
                return
            if deps.get(k, 0) < v:
                deps[k] = v
        for r in reads:
            if r in self.last_w:
                add(self.last_w[r])
        for w in writes:
            if w in self.last_w:
                add(self.last_w[w])
            for ev in self.readers.get(w, ()):
                add(ev)
        need = []
        for k, v in deps.items():
            if self.seen[eng].get(k, 0) < v:
                self.seen[eng][k] = v
                need.append((k, v))
        return need

    def _commit(self, ev, reads, writes):
        reads = self._expand(reads)
        writes = self._expand(writes)
        for r in reads:
            self.readers.setdefault(r, []).append(ev)
        for w in writes:
            self.last_w[w] = ev
            self.readers[w] = []

    def op(self, eng, fn, reads=(), writes=()):
        fn = _bind(fn)
        pr = [r for r in reads if r[:2] in ("pA", "pT", "pO")]
        if pr:
            reads = [r for r in reads if r not in pr]
            writes = list(writes) + pr
        need = self._deps(eng, reads, writes)
        self.cnt[eng] += 1
        ev = (("c", eng), self.cnt[eng])
        self.seen[eng][ev[0]] = max(self.seen[eng].get(ev[0], 0), 0)
        sem = self.sem[eng]
        waits = [(self.semobj[k], v) for k, v in need]

        def emit(e, fn=fn, waits=waits, sem=sem):
            for s, v in waits:
                e.wait_ge(s, v)
            fn(e).then_inc(sem, 1)
        self.q[eng].append(emit)
        self._commit(ev, reads, writes)
        return ev

    def dma(self, qn, fn, reads=(), writes=()):
        fn = _bind(fn)
        j = self.dcnt[qn]
        self.dcnt[qn] += 1
        slot, rnd = j % self.RING, j // self.RING
        key = ("d", qn, slot)
        need = self._deps(qn, reads, writes)
        if rnd > 0 and self.seen[qn].get(key, 0) < 16 * rnd:
            self.seen[qn][key] = 16 * rnd
            need.append((key, 16 * rnd))
        ev = (key, 16 * (rnd + 1))
        sem = self.semobj[key]
        waits = [(self.semobj[k], v) for k, v in need]

        def emit(e, fn=fn, waits=waits, sem=sem):
            for s, v in waits:
                e.wait_ge(s, v)
            fn(e).then_inc(sem, 16)
        self.q[qn].append(emit)
        self._commit(ev, reads, writes)
        return ev

    def finish(self, final_events):
        waits = {}
        for k, v in final_events:
            waits[k] = max(waits.get(k, 0), v)
        fw = [(self.semobj[k], v) for k, v in waits.items()]
        q = self.q
        with self.nc.Block() as block:
            @block.tensor
            def _(e):
                for f in q["pe"]:
                    f(e)

            @block.scalar
            def _(e):
                for f in q["act"]:
                    f(e)

            @block.vector
            def _(e):
                for f in q["dve"]:
                    f(e)

            @block.gpsimd
            def _(e):
                for f in q["pool"]:
                    f(e)

            @block.sync
            def _(e):
                for f in q["sp"]:
                    f(e)
                for s, v in fw:
                    e.wait_ge(s, v)


    def barrier(self):
        latest = {}
        for e in self.CE:
            if self.cnt[e]:
                latest[("c", e)] = self.cnt[e]
        for qn in self.dsem:
            for j in range(max(0, self.dcnt[qn] - self.RING), self.dcnt[qn]):
                k = ("d", qn, j % self.RING)
                latest[k] = max(latest.get(k, 0), 16 * (j // self.RING + 1))
        for eng in self.eng:
            need = []
            for k, v in latest.items():
                if k == ("c", eng):
                    continue
                if self.seen[eng].get(k, 0) < v:
                    self.seen[eng][k] = v
                    need.append((self.semobj[k], v))
            if need:
                def emit(e, need=need):
                    for s_, v in need:
                        e.wait_ge(s_, v)
                self.q[eng].append(emit)
        self.last_w = {}
        self.readers = {}


def build(stage=4, debug=False):
    nc = bass.Bass("TRN2", target_bir_lowering=False)

    def din(name, shape, dt=F32):
        return nc.dram_tensor(name, list(shape), dt, kind="ExternalInput").ap()

    dbg_out = {1: ("kt_scr", "v_scr"), 2: ("h_scr", "hn_scr"), 2.5: ("xs_scr", "hn_scr"), 3: ("ys_scr", "xs_scr", "hn_scr", "h_scr")}.get(stage, ()) if debug else ()

    def dscr(name, shape, dt):
        return nc.dram_tensor(name, list(shape), dt, kind=("ExternalOutput" if name in dbg_out else "Internal")).ap()

    xw = din("xw", [SEQ, D])
    w_in = din("w_in", [D, IN_COLS])
    w_co = din("w_conv_out", [CC, D])
    w_ao = din("w_attn_out", [CC, D])
    w_out = din("w_out", [D, D])
    if stage >= 3:
        w_eg = din("w_exp_gate", [NEXP, D, DEXP])
        w_eu = din("w_exp_up", [NEXP, D, DEXP])
        w_ed = din("w_exp_down", [NEXP, DEXP, D])
    g1_d = din("norm1_g", [1, D])
    g2_d = din("norm2_g", [1, D])
    gf_d = din("norm_f_g", [1, D])
    dww_d = din("dw_w", [128, 8, TAPS])
    chv_d = din("chvec", [128, 8, 3])
    gb_d = din("gate_b", [128, 32])
    wr_d = din("w_router", [128, NCH, 72])
    br_d = din("b_router", [1, 72])
    idb_d = din("ident_bf", [128, 128], BF16)
    idf_d = din("ident_f", [128, 128])
    tri_d = din("tri_bf", [128, 128], BF16)
    ust_d = din("ustrict_bf", [128, 128], BF16)
    bb_d = din("bias_tab", [128, HEADS, 3])
    fd_d = din("facd_tab", [128, HEADS, 2])
    dist_d = din("dist_tab", [128, 16, NBLK])
    gm_d = din("gmask", [128, 16, NBLK])
    pidx_d = din("pidx", [128, 1])
    bst_d = din("blkstart", [128, MOE_BLOCKS])
    out_d = nc.dram_tensor("out", [NOWN, D], F32, kind="ExternalOutput").ap()

    kt_scr = dscr("kt_scr", [HEADS, 128, SEQ], BF16)
    v_scr = dscr("v_scr", [HEADS, 128, 64, VP], BF16)
    h_scr = dscr("h_scr", [NOWN, D], F32)
    hn_scr = dscr("hn_scr", [NOWN, D], BF16)
    xs_scr = dscr("xs_scr", [NSLOT, D], BF16)
    ys_scr = dscr("ys_scr", [NSLOT, D], F32)

    w_in_v = w_in.rearrange("(c p) n -> p c n", p=128)

    with contextlib.ExitStack() as st:
        P = Prog(nc, st)

        uniq = [0]

        def sb(stack, name, shape, dt):
            uniq[0] += 1
            return stack.enter_context(nc.sbuf_tensor("s%d_%s" % (uniq[0], name), list(shape), dt))

        def psum(stack, name, shape, dt):
            return stack.enter_context(nc.psum_tensor("p_" + name, list(shape), dt))

        idb = sb(st, "idb", [128, 128], BF16)
        idf = sb(st, "idf", [128, 128], F32)
        tri = sb(st, "tri", [128, 128], BF16)
        ust = sb(st, "ust", [128, 128], BF16)
        ones_b = sb(st, "ones_b", [128, 128], BF16)
        ones_f = sb(st, "ones_f", [128, 128], F32)
        bbt = sb(st, "bbt", [128, HEADS, 3], F32)
        fdt = sb(st, "fdt", [128, HEADS, 2], F32)
        distt = sb(st, "distt", [128, 16, NBLK], F32)
        gmt = sb(st, "gmt", [128, 16, NBLK], F32)
        pidx = sb(st, "pidx", [128, 1], F32)
        bstt = sb(st, "bstt", [128, MOE_BLOCKS], F32)
        dww = sb(st, "dww", [128, 8, TAPS], F32)
        chv = sb(st, "chv", [128, 8, 3], F32)
        gbt = sb(st, "gbt", [128, 32], F32)
        wr32 = sb(st, "wr32", [128, NCH, 72], F32)
        brb = sb(st, "brb", [128, 72], F32)
        gA = sb(st, "gA", [128, D], F32)
        gB = sb(st, "gB", [128, D], F32)
        kmean = sb(st, "kmean", [128, HEADS, NBLK], F32)
        kmb = sb(st, "kmb", [128, HEADS, NBLK], BF16)
        Mall = sb(st, "Mall", [128, 16, NEXP], BF16)
        M1all = sb(st, "M1all", [128, 16, NEXP], BF16)
        M2all = sb(st, "M2all", [128, 16, NEXP], BF16)
        zhalo = sb(st, "zhalo", [128, 8, 32], F32)
        comb = sb(st, "comb", [128, 16, 2], F32)
        desti = sb(st, "desti", [128, 16, 2], I32)
        xt = [sb(st, "xt%d" % i, [128, D], F32) for i in range(2)]
        junk = sb(st, "junk", [128, D], BF16)
        xs = [sb(st, "xs%d" % i, [128, D], BF16) for i in range(2)]
        ssr = [sb(st, "ss%d" % i, [128, 1], F32) for i in range(2)]
        rsr = [sb(st, "rs%d" % i, [128, 1], F32) for i in range(2)]

        pA = [psum(st, "pA%d" % i, [128, 512], F32) for i in range(4)]
        pT = psum(st, "pT", [128, 2, 8, 128], BF16)
        pO = psum(st, "pO", [128, 2, 512], F32)
        pa_i = [0]

        def nextpA():
            i = pa_i[0] % 4
            pa_i[0] += 1
            return i

        for (t_, d_, nm) in [(idb, idb_d, "idb"), (idf, idf_d, "idf"), (tri, tri_d, "tri"), (ust, ust_d, "ust"),
                             (bbt, bb_d, "bbt"), (fdt, fd_d, "fdt"), (distt, dist_d, "distt"), (gmt, gm_d, "gmt"),
                             (pidx, pidx_d, "pidx"), (bstt, bst_d, "bstt"), (dww, dww_d, "dww"), (chv, chv_d, "chv"),
                             (gbt, gb_d, "gbt"), (wr32, wr_d, "wr32")]:
            P.dma("sp", lambda e, t_=t_, d_=d_: e.dma_start(out=t_[:], in_=d_), writes=[nm])
        P.dma("sp", lambda e: e.dma_start(out=brb[:], in_=br_d.partition_broadcast(128)), writes=["brb"])
        P.dma("sp", lambda e: e.dma_start(out=gA[:], in_=g1_d.partition_broadcast(128)), writes=["gA"])
        P.dma("sp", lambda e: e.dma_start(out=gB[:], in_=g2_d.partition_broadcast(128)), writes=["gB"])
        P.op("dve", lambda e: e.memset(ones_b[:], 1.0), writes=["ones_b"])
        P.op("dve", lambda e: e.memset(ones_f[:], 1.0), writes=["ones_f"])

        if stage == 0:
            dbg_km = nc.dram_tensor("dbg_km", [128, HEADS, NBLK], F32, kind="ExternalOutput").ap()
            P.dma("sp", lambda e: e.dma_start(out=dbg_km, in_=distt[:, 0:8, :]), reads=["distt"])
            P.barrier()
            P.finish([])
            return nc
        nt_i = [0]

        def norm_tile(src_rows, gbuf, gname, dstT, dname, col):
            i = nt_i[0] % 2
            nt_i[0] += 1
            P.dma("sp", lambda e: e.dma_start(out=xt[i][:], in_=src_rows), writes=["xt%d" % i])
            rms_scale(xt[i], "xt%d" % i, i, gbuf, gname, xs[i], "xs%d" % i)
            transp16(xs[i], "xs%d" % i, dstT, dname, col)

        def rms_scale(src, sname, i, gbuf, gname, dst, dname):
            P.op("act", lambda e: e.activation(out=junk[:], in_=src[:], func=AF.Square, accum_out=ssr[i][:]),
                 reads=[sname], writes=["junk", "ss%d" % i])
            P.op("dve", lambda e: e.tensor_scalar(out=rsr[i][:], in0=ssr[i][:], scalar1=1.0 / D, scalar2=EPS,
                                                 op0=ALU.mult, op1=ALU.add), reads=["ss%d" % i], writes=["rs%d" % i])
            P.op("act", lambda e: e.sqrt(out=rsr[i][:], in_=rsr[i][:]), reads=["rs%d" % i], writes=["rs%d" % i])
            P.op("dve", lambda e: e.reciprocal(out=rsr[i][:], in_=rsr[i][:]), reads=["rs%d" % i], writes=["rs%d" % i])
            P.op("dve", lambda e: e.scalar_tensor_tensor(out=dst[:], in0=src[:], scalar=rsr[i][:], in1=gbuf[:],
                                                        op0=ALU.mult, op1=ALU.mult),
                 reads=[sname, "rs%d" % i, gname], writes=[dname])

        def transp16(src, sname, dstT, dname, col):
            for c in range(NCH):
                P.op("pe", lambda e, c=c: e.transpose(out=pT[:, c // 8, c % 8, :], in_=src[:, c * 128:(c + 1) * 128],
                                                      identity=idb[:]),
                     reads=[sname, "idb"], writes=["pT%d" % (c // 8)])
            P.op("dve", lambda e: e.tensor_copy(out=dstT[:, 0:8, col:col + 128], in_=pT[:, 0, :, :]),
                 reads=["pT0"], writes=[dname])
            P.op("act", lambda e: e.copy(out=dstT[:, 8:16, col:col + 128], in_=pT[:, 1, :, :]),
                 reads=["pT1"], writes=[dname])

        def load_w(dst_ap, src_ap, name, nsplit=4):
            C = src_ap.shape[1]
            step = max(1, C // nsplit)
            parts = ["%s#%d" % (name, c0) for c0 in range(0, C, step)]
            P.groups.pop(name, None)
            for c0 in range(0, C, step):
                P.dma("pool", lambda e, c0=c0: e.dma_start(out=dst_ap[:, c0:c0 + step, :],
                                                          in_=src_ap[:, c0:c0 + step, :]),
                      writes=["%s#%d" % (name, c0)])
            P.groups[name] = parts

        with contextlib.ExitStack() as s1:
            wk = sb(s1, "wk", [128, NCH, 1024], BF16)
            wv = sb(s1, "wv", [128, NCH, 1024], BF16)
            uT = [sb(s1, "uT%d" % i, [128, NCH, 512], BF16) for i in range(2)]
            ktg = [sb(s1, "ktg%d" % i, [128, HEADS, 512], BF16) for i in range(2)]
            vg = [sb(s1, "vg%d" % i, [128, HEADS, 4, VP], BF16) for i in range(2)]
            load_w(wk[:], w_in_v[:, :, C_K:C_K + 1024], "wk")
            load_w(wv[:], w_in_v[:, :, C_V:C_V + 1024], "wv")
            for i in range(2):
                P.op("pool", lambda e, i=i: e.memset(vg[i][:], 1.0), writes=["vg%d" % i])
            for g in range(16 if stage >= 1 else 1):
                b = g % 2
                for t in range(4):
                    r0 = g * 512 + t * 128
                    norm_tile(xw[r0:r0 + 128, :], gA, "gA", uT[b], "uT%d" % b, t * 128)
                for h in range(HEADS):
                    k = nextpA()
                    for c in range(NCH):
                        P.op("pe", lambda e, c=c, k=k, h=h: e.matmul(pA[k][:], lhsT=wk[:, c, h * 128:(h + 1) * 128],
                                                                    rhs=uT[b][:, c, :], start=(c == 0), stop=(c == NCH - 1)),
                             reads=["wk", "uT%d" % b], writes=["pA%d" % k])
                    P.op("act", lambda e, k=k, h=h: e.copy(out=ktg[b][:, h, :], in_=pA[k][:]),
                         reads=["pA%d" % k], writes=["ktg%d" % b])
                    P.op("dve", lambda e, k=k, h=h: e.tensor_reduce(
                        out=kmean[:, h, 2 * g:2 * g + 2], in_=pA[k][:].rearrange("p (b t) -> p b t", b=2),
                        axis=AX.X, op=ALU.add), reads=["pA%d" % k], writes=["kmean"])
                for t in range(4):
                    for half in range(2):
                        k = nextpA()
                        for c in range(NCH):
                            P.op("pe", lambda e, c=c, k=k, t=t, half=half: e.matmul(
                                pA[k][:], lhsT=uT[b][:, c, t * 128:(t + 1) * 128],
                                rhs=wv[:, c, half * 512:(half + 1) * 512], start=(c == 0), stop=(c == NCH - 1)),
                                reads=["wv", "uT%d" % b], writes=["pA%d" % k])
                        eng = "act" if half == 0 else "dve"
                        if eng == "act":
                            P.op("act", lambda e, k=k, t=t, half=half: e.copy(
                                out=vg[b][:, half * 4:half * 4 + 4, t, 0:128],
                                in_=pA[k][:].rearrange("p (h d) -> p h d", h=4)),
                                reads=["pA%d" % k], writes=["vg%d" % b])
                        else:
                            P.op("dve", lambda e, k=k, t=t, half=half: e.tensor_copy(
                                out=vg[b][:, half * 4:half * 4 + 4, t, 0:128],
                                in_=pA[k][:].rearrange("p (h d) -> p h d", h=4)),
                                reads=["pA%d" % k], writes=["vg%d" % b])
                P.dma("pool", lambda e, g=g, b=b: e.dma_start(
                    out=kt_scr[:, :, g * 512:(g + 1) * 512].rearrange("h p t -> p h t"), in_=ktg[b][:]),
                    reads=["ktg%d" % b], writes=["kt_scr%d" % g])
                P.dma("pool", lambda e, g=g, b=b: e.dma_start(
                    out=v_scr[:, :, g * 4:(g + 1) * 4, :].rearrange("h p t v -> p h t v"), in_=vg[b][:]),
                    reads=["vg%d" % b], writes=["v_scr%d" % g])
            P.op("dve", lambda e: e.tensor_scalar(out=kmb[:], in0=kmean[:], scalar1=1.0 / 256.0, scalar2=None,
                                                 op0=ALU.mult), reads=["kmean"], writes=["kmb"])
        P.barrier()
        if stage == 1:
            dbg_km = nc.dram_tensor("dbg_km", [128, HEADS, NBLK], F32, kind="ExternalOutput").ap()
            P.dma("sp", lambda e: e.dma_start(out=dbg_km, in_=kmean[:]))
            P.barrier()
            P.finish([])
            return nc

        SCALE = float(HD) ** -0.5
        SLOPES = [2.0 ** (-8.0 * (i + 1) / HEADS) for i in range(HEADS)]

        with contextlib.ExitStack() as s2:
            wst = [sb(s2, "wst%d" % i, [128, NCH, 512], BF16) for i in range(2)]
            uTo = sb(s2, "uTo", [128, NCH, 512], BF16)
            qT = sb(s2, "qT", [128, HEADS, 512], BF16)
            mcT = sb(s2, "mcT", [128, NCH, 512], BF16)
            ws_i = [0]

            def stream_w(src_ap, buf=None):
                if buf is None:
                    i = ws_i[0] % 2
                    ws_i[0] += 1
                else:
                    i = buf
                C, N = src_ap.shape[1], src_ap.shape[2]
                dst = wst[i][:].rearrange("p c n -> p (c n)").rearrange("p (c n) -> p c n", c=C)
                load_w(dst, src_ap, "wst%d" % i)
                return dst, "wst%d" % i

            def lin_fm(wt, wname, col0, src, sname, K, N=512):
                k = nextpA()
                for c in range(K):
                    P.op("pe", lambda e, c=c: e.matmul(pA[k][:, 0:N], lhsT=wt[:, c, col0:col0 + 128],
                                                       rhs=src[:, c, 0:N], start=(c == 0), stop=(c == K - 1)),
                         reads=[wname, sname], writes=["pA%d" % k])
                return k

            def glu(src, sname, N, zdst):
                for i2 in range(2):
                    wv_, nv = stream_w(w_in_v[:, :, C_VAL + i2 * 512:C_VAL + (i2 + 1) * 512])
                    wg_, ng = stream_w(w_in_v[:, :, C_GATE + i2 * 512:C_GATE + (i2 + 1) * 512])
                    for c4 in range(4):
                        cc = i2 * 4 + c4
                        kv = lin_fm(wv_, nv, c4 * 128, src, sname, NCH, N)
                        kg = lin_fm(wg_, ng, c4 * 128, src, sname, NCH, N)
                        P.op("act", lambda e, kg=kg: e.activation(out=sgt[:, 0:N], in_=pA[kg][:, 0:N], func=AF.Sigmoid),
                             reads=["pA%d" % kg], writes=["sgt"])
                        P.op("dve", lambda e, kv=kv, cc=cc: e.tensor_tensor(out=zdst(cc), in0=pA[kv][:, 0:N],
                                                                           in1=sgt[:, 0:N], op=ALU.mult),
                             reads=["pA%d" % kv, "sgt"], writes=["z"])

            for gi in range(4):
                g = 12 + gi
                row0 = g * 512
                for t in range(4):
                    norm_tile(xw[row0 + t * 128:row0 + (t + 1) * 128, :], gA, "gA", uTo, "uTo", t * 128)
                for i2 in range(2):
                    wq_, nq = stream_w(w_in_v[:, :, C_Q + i2 * 512:C_Q + (i2 + 1) * 512])
                    for c4 in range(4):
                        k = lin_fm(wq_, nq, c4 * 128, uTo, "uTo", NCH)
                        P.op("act", lambda e, k=k, h=i2 * 4 + c4: e.copy(out=qT[:, h, :], in_=pA[k][:]),
                             reads=["pA%d" % k], writes=["qT"])
                with contextlib.ExitStack() as sA:
                    z = sb(sA, "z", [128, 8, 544], F32)
                    zc = sb(sA, "zc", [128, 8, 512], F32)
                    sgt = sb(sA, "sgt", [128, 512], F32)
                    sqb = sb(sA, "sqb", [128, 512], F32)
                    mean = sb(sA, "mean", [128, 512], F32)
                    rstd = sb(sA, "rstd", [128, 512], F32)
                    tmpc = [sb(sA, "tmpc%d" % i, [128, 512], F32) for i in range(2)]
                    zact = sb(sA, "zact", [128, 8, 512], BF16)
                    if gi == 0:
                        zh = sb(sA, "zh", [128, 8, 128], F32)
                        uTh = sb(sA, "uTh", [128, NCH, 128], BF16)
                        norm_tile(xw[row0 - 128:row0, :], gA, "gA", uTh, "uTh", 0)
                        glu(uTh, "uTh", 128, lambda cc: zh[:, cc, :])
                        P.op("dve", lambda e: e.tensor_copy(out=zhalo[:], in_=zh[:, :, 96:128]),
                             reads=["z"], writes=["zhalo"])
                    P.op("dve", lambda e: e.tensor_copy(out=z[:, :, 0:32], in_=zhalo[:]),
                         reads=["zhalo"], writes=["z"])
                    glu(uTo, "uTo", 512, lambda cc: z[:, cc, 32:544])
                    P.op("dve", lambda e: e.tensor_copy(out=zhalo[:], in_=z[:, :, 512:544]),
                         reads=["z"], writes=["zhalo"])
                    tmpp = sb(sA, "tmpp", [128, 512], F32)
                    for cc in range(8):
                        en = "dve"
                        rn = "zc%d" % cc
                        P.op(en, lambda e, cc=cc: e.tensor_scalar(out=zc[:, cc, :], in0=z[:, cc, 2:514],
                                                                 scalar1=dww[:, cc, 0:1], scalar2=chv[:, cc, 0:1],
                                                                 op0=ALU.mult, op1=ALU.add),
                             reads=["z", "dww", "chv"], writes=[rn])
                        for kk in range(1, TAPS):
                            if en == "dve":
                                P.op(en, lambda e, cc=cc, kk=kk: e.scalar_tensor_tensor(
                                    out=zc[:, cc, :], in0=z[:, cc, 2 + kk:514 + kk], scalar=dww[:, cc, kk:kk + 1],
                                    in1=zc[:, cc, :], op0=ALU.mult, op1=ALU.add),
                                    reads=["z", rn], writes=[rn])
                            else:
                                P.op(en, lambda e, cc=cc, kk=kk: e.tensor_scalar(
                                    out=tmpp[:], in0=z[:, cc, 2 + kk:514 + kk], scalar1=dww[:, cc, kk:kk + 1],
                                    scalar2=None, op0=ALU.mult), reads=["z"], writes=["tmpp"])
                                P.op(en, lambda e, cc=cc: e.tensor_tensor(out=zc[:, cc, :], in0=zc[:, cc, :], in1=tmpp[:],
                                                                          op=ALU.add), reads=["tmpp", rn], writes=[rn])
                    k1 = nextpA()
                    for cc in range(8):
                        P.op("pe", lambda e, cc=cc: e.matmul(pA[k1][:], lhsT=ones_f[:], rhs=zc[:, cc, :],
                                                             start=(cc == 0), stop=(cc == 7)),
                             reads=["ones_f", "zc%d" % cc], writes=["pA%d" % k1])
                    k2 = nextpA()
                    for cc in range(8):
                        tb = tmpc[cc % 2]
                        P.op("act", lambda e, cc=cc, tb=tb: e.activation(out=tb[:], in_=zc[:, cc, :], func=AF.Square),
                             reads=["zc%d" % cc], writes=["tmpc%d" % (cc % 2)])
                        P.op("pe", lambda e, cc=cc, tb=tb: e.matmul(pA[k2][:], lhsT=ones_f[:], rhs=tb[:],
                                                                    start=(cc == 0), stop=(cc == 7)),
                             reads=["ones_f", "tmpc%d" % (cc % 2)], writes=["pA%d" % k2])
                    P.op("dve", lambda e: e.tensor_scalar(out=mean[:], in0=pA[k1][:], scalar1=1.0 / CC, scalar2=None,
                                                         op0=ALU.mult), reads=["pA%d" % k1], writes=["mean"])
                    P.op("dve", lambda e: e.tensor_tensor(out=sqb[:], in0=mean[:], in1=mean[:], op=ALU.mult),
                         reads=["mean"], writes=["sqb"])
                    P.op("dve", lambda e: e.scalar_tensor_tensor(out=rstd[:], in0=pA[k2][:], scalar=1.0 / CC, in1=sqb[:],
                                                                op0=ALU.mult, op1=ALU.subtract),
                         reads=["pA%d" % k2, "sqb"], writes=["rstd"])
                    P.op("dve", lambda e: e.tensor_scalar(out=rstd[:], in0=rstd[:], scalar1=EPS, scalar2=None,
                                                         op0=ALU.add), reads=["rstd"], writes=["rstd"])
                    P.op("act", lambda e: e.sqrt(out=rstd[:], in_=rstd[:]), reads=["rstd"], writes=["rstd"])
                    P.op("dve", lambda e: e.reciprocal(out=rstd[:], in_=rstd[:]), reads=["rstd"], writes=["rstd"])
                    for cc in range(8):
                        tb = tmpc[cc % 2]
                        tn = "tmpc%d" % (cc % 2)
                        P.op("dve", lambda e, cc=cc, tb=tb: e.tensor_tensor(out=tb[:], in0=zc[:, cc, :], in1=mean[:],
                                                                            op=ALU.subtract),
                             reads=["zc%d" % cc, "mean"], writes=[tn])
                        P.op("dve", lambda e, tb=tb: e.tensor_tensor(out=tb[:], in0=tb[:], in1=rstd[:], op=ALU.mult),
                             reads=[tn, "rstd"], writes=[tn])
                        P.op("act", lambda e, cc=cc, tb=tb: e.activation(out=zact[:, cc, :], in_=tb[:], func=AF.Silu,
                                                                         bias=chv[:, cc, 2:3], scale=chv[:, cc, 1:2]),
                             reads=[tn, "chv"], writes=["zact"])
                    w_co_v = w_co.rearrange("(c p) n -> p c n", p=128)
                    for half in range(2):
                        wco_, nco = stream_w(w_co_v[:, :, half * 1024:(half + 1) * 1024], buf=0)
                        for q2 in range(2):
                            gcol = C_GC + (half * 2 + q2) * 512
                            wgc_, ngc = stream_w(w_in_v[:, :, gcol:gcol + 512], buf=1)
                            for d4 in range(4):
                                dc = half * 8 + q2 * 4 + d4
                                ky = lin_fm(wco_, nco, (q2 * 4 + d4) * 128, zact, "zact", 8)
                                kg = lin_fm(wgc_, ngc, d4 * 128, uTo, "uTo", NCH)
                                P.op("act", lambda e, kg=kg, dc=dc: e.activation(out=sgt[:], in_=pA[kg][:], func=AF.Sigmoid,
                                                                                 bias=gbt[:, dc:dc + 1]),
                                     reads=["pA%d" % kg, "gbt"], writes=["sgt"])
                                P.op("dve", lambda e, ky=ky, dc=dc: e.tensor_tensor(out=mcT[:, dc, :], in0=pA[ky][:],
                                                                                   in1=sgt[:], op=ALU.mult),
                                     reads=["pA%d" % ky, "sgt"], writes=["mcT"])
                    if stage == 1.3:
                        dbg_mc = nc.dram_tensor("dbg_mc", [128, NCH, 512], BF16, kind="ExternalOutput").ap()
                        dbg_qt = nc.dram_tensor("dbg_qt", [128, HEADS, 512], BF16, kind="ExternalOutput").ap()
                        P.dma("sp", lambda e: e.dma_start(out=dbg_mc, in_=mcT[:]), reads=["mcT"])
                        P.dma("sp", lambda e: e.dma_start(out=dbg_qt, in_=qT[:]), reads=["qT"])
                        P.barrier()
                        P.finish([])
                        return nc
                    P.barrier()
                with contextlib.ExitStack() as sB:
                    KT = sb(sB, "KT", [128, SEQ], BF16)
                    VH = sb(sB, "VH", [128, 64, VP], BF16)
                    PT = [sb(sB, "PT%d" % i, [128, 512], BF16) for i in range(4)]
                    acc = sb(sB, "acc", [128, 4, 129], F32)
                    rec = sb(sB, "rec", [128, 4], F32)
                    gs = sb(sB, "gs", [128, 4, NBLK], F32)
                    top8 = sb(sB, "top8", [128, 4, 8], F32)
                    fac = sb(sB, "fac", [128, 4, NBLK], F32)
                    wsel = sb(sB, "wsel", [128, 4, NBLK], F32)
                    atok = sb(sB, "atok", [128, 4, CC], BF16)
                    attnT = sb(sB, "attnT", [128, HEADS, 512], BF16)
                    pt_i = [0]
                    po_i = [0]
                    nkeys = (g + 1) * 512

                    def s_exp(h, ktile, q0, q1, bcol):
                        k = nextpA()
                        P.op("pe", lambda e: e.matmul(pA[k][:, q0:q1], lhsT=KT[:, ktile * 128:(ktile + 1) * 128],
                                                      rhs=qT[:, h, q0:q1], start=True, stop=True),
                             reads=["KT", "qT"], writes=["pA%d" % k])
                        pi = pt_i[0] % 4
                        pt_i[0] += 1
                        P.op("act", lambda e: e.activation(out=PT[pi][:, q0:q1], in_=pA[k][:, q0:q1], func=AF.Exp,
                                                           bias=bbt[:, h, bcol:bcol + 1], scale=SCALE),
                             reads=["pA%d" % k, "bbt"], writes=["PT%d" % pi])
                        return pi

                    def pv_acc(pairs, qt, wap, wname):
                        r = po_i[0] % 2
                        po_i[0] += 1
                        oap = pO[:, r, 0:129]
                        for j, (pi, ktile) in enumerate(pairs):
                            P.op("pe", lambda e, j=j, pi=pi, ktile=ktile: e.matmul(
                                oap, lhsT=PT[pi][:, qt * 128:(qt + 1) * 128], rhs=VH[:, ktile, 0:129],
                                start=(j == 0), stop=(j == len(pairs) - 1)),
                                reads=["PT%d" % pi, "VH"], writes=["pO%d" % r])
                        P.op("dve", lambda e: e.scalar_tensor_tensor(out=acc[:, qt, :], in0=oap, scalar=wap,
                                                                    in1=acc[:, qt, :], op0=ALU.mult, op1=ALU.add),
                             reads=["pO%d" % r, wname, "acc"], writes=["acc"])

                    for h in range(HEADS):
                        P.dma("sp", lambda e, h=h: e.dma_start(out=KT[:, 0:nkeys], in_=kt_scr[h, :, 0:nkeys]),
                              reads=["kt_scr"], writes=["KT"])
                        P.dma("sp", lambda e, h=h: e.dma_start(out=VH[:, 0:nkeys // 128, :],
                                                              in_=v_scr[h, :, 0:nkeys // 128, :]),
                              reads=["v_scr"], writes=["VH"])
                        P.op("pool", lambda e: e.memset(acc[:], 0.0), writes=["acc"])
                        kq = nextpA()
                        for qt in range(4):
                            P.op("pe", lambda e, qt=qt: e.matmul(pA[kq][:, qt * NBLK:(qt + 1) * NBLK],
                                                                 lhsT=qT[:, h, qt * 128:(qt + 1) * 128], rhs=kmb[:, h, :],
                                                                 start=True, stop=True),
                                 reads=["qT", "kmb"], writes=["pA%d" % kq])
                        P.op("dve", lambda e: e.tensor_tensor(out=gs[:], in0=pA[kq][:, 0:4 * NBLK].rearrange("p (a b) -> p a b", a=4),
                                                              in1=gmt[:, gi * 4:gi * 4 + 4, :], op=ALU.add),
                             reads=["pA%d" % kq, "gmt"], writes=["gs"])
                        for qt in range(4):
                            P.op("dve", lambda e, qt=qt: e.max(out=top8[:, qt, :], in_=gs[:, qt, :]),
                                 reads=["gs"], writes=["top8"])
                        P.op("dve", lambda e: e.tensor_scalar(out=top8[:, :, 2:3], in0=top8[:, :, 2:3], scalar1=-1e29,
                                                             scalar2=None, op0=ALU.max), reads=["top8"], writes=["top8"])
                        P.op("act", lambda e, h=h: e.activation(out=fac[:], in_=distt[:, gi * 4:gi * 4 + 4, :], func=AF.Exp,
                                                                scale=-SLOPES[h]), reads=["distt"], writes=["fac"])
                        for qt in range(4):
                            P.op("dve", lambda e, qt=qt: e.scalar_tensor_tensor(
                                out=wsel[:, qt, :], in0=gs[:, qt, :], scalar=top8[:, qt, 2:3], in1=fac[:, qt, :],
                                op0=ALU.is_ge, op1=ALU.mult), reads=["gs", "top8", "fac"], writes=["wsel"])
                        for n in range(2 * g):
                            pis = [s_exp(h, 2 * n + kt, 0, 512, kt) for kt in range(2)]
                            for qt in range(4):
                                pv_acc([(pis[0], 2 * n), (pis[1], 2 * n + 1)], qt, wsel[:, qt, n:n + 1], "wsel")
                        n = 2 * g
                        pis = [s_exp(h, 2 * n + kt, 256, 512, kt) for kt in range(2)]
                        for qt in (2, 3):
                            pv_acc([(pis[0], 2 * n), (pis[1], 2 * n + 1)], qt, wsel[:, qt, n:n + 1], "wsel")
                        for (qt, ktile, diag) in [(0, 4 * g, True), (1, 4 * g, False), (1, 4 * g + 1, True),
                                                  (2, 4 * g + 2, True), (3, 4 * g + 2, False), (3, 4 * g + 3, True)]:
                            pi = s_exp(h, ktile, qt * 128, (qt + 1) * 128, 2 if diag else 1)
                            if diag:
                                P.op("pool", lambda e, pi=pi, qt=qt: e.tensor_tensor(
                                    out=PT[pi][:, qt * 128:(qt + 1) * 128], in0=PT[pi][:, qt * 128:(qt + 1) * 128],
                                    in1=tri[:], op=ALU.mult), reads=["PT%d" % pi, "tri"], writes=["PT%d" % pi])
                            pv_acc([(pi, ktile)], qt, fdt[:, h, 0:1] if diag else fdt[:, h, 1:2], "fdt")
                        P.op("dve", lambda e: e.reciprocal(out=rec[:], in_=acc[:, :, 128]), reads=["acc"], writes=["rec"])
                        for qt in range(4):
                            P.op("dve", lambda e, qt=qt, h=h: e.tensor_scalar(
                                out=atok[:, qt, h * 128:(h + 1) * 128], in0=acc[:, qt, 0:128], scalar1=rec[:, qt:qt + 1],
                                scalar2=None, op0=ALU.mult), reads=["acc", "rec"], writes=["atok"])
                    for qt in range(4):
                        bk = qt % 2
                        for h in range(HEADS):
                            P.op("pe", lambda e, qt=qt, h=h, bk=bk: e.transpose(
                                out=pT[:, bk, h, :], in_=atok[:, qt, h * 128:(h + 1) * 128], identity=idb[:]),
                                reads=["atok", "idb"], writes=["pT%d" % bk])
                        P.op("act", lambda e, qt=qt, bk=bk: e.copy(out=attnT[:, :, qt * 128:(qt + 1) * 128], in_=pT[:, bk, :, :]),
                             reads=["pT%d" % bk], writes=["attnT"])
                    sgt2 = sb(sB, "sgt2", [128, 512], F32)
                    tmpm = sb(sB, "tmpm", [128, 512], F32)
                    w_ao_v = w_ao.rearrange("(c p) n -> p c n", p=128)
                    for half in range(2):
                        wao_, nao = stream_w(w_ao_v[:, :, half * 1024:(half + 1) * 1024], buf=0)
                        for q2 in range(2):
                            gcol = C_GA + (half * 2 + q2) * 512
                            wga_, nga = stream_w(w_in_v[:, :, gcol:gcol + 512], buf=1)
                            for d4 in range(4):
                                dc = half * 8 + q2 * 4 + d4
                                ky = lin_fm(wao_, nao, (q2 * 4 + d4) * 128, attnT, "attnT", 8)
                                kg = lin_fm(wga_, nga, d4 * 128, uTo, "uTo", NCH)
                                P.op("act", lambda e, kg=kg, dc=dc: e.activation(out=sgt2[:], in_=pA[kg][:], func=AF.Sigmoid,
                                                                                 bias=gbt[:, 16 + dc:17 + dc]),
                                     reads=["pA%d" % kg, "gbt"], writes=["sgt2"])
                                P.op("dve", lambda e, ky=ky: e.tensor_tensor(out=tmpm[:], in0=pA[ky][:], in1=sgt2[:],
                                                                            op=ALU.mult),
                                     reads=["pA%d" % ky, "sgt2"], writes=["tmpm"])
                                P.op("dve", lambda e, dc=dc: e.tensor_tensor(out=mcT[:, dc, :], in0=tmpm[:], in1=mcT[:, dc, :],
                                                                            op=ALU.add),
                                     reads=["tmpm", "mcT"], writes=["mcT"])
                    if stage == 1.6:
                        dbg_mc = nc.dram_tensor("dbg_mc", [128, NCH, 512], BF16, kind="ExternalOutput").ap()
                        dbg_at = nc.dram_tensor("dbg_at", [128, HEADS, 512], BF16, kind="ExternalOutput").ap()
                        P.dma("sp", lambda e: e.dma_start(out=dbg_mc, in_=mcT[:]), reads=["mcT"])
                        P.dma("sp", lambda e: e.dma_start(out=dbg_at, in_=attnT[:]), reads=["attnT"])
                        P.barrier()
                        P.finish([])
                        return nc
                    P.barrier()
                with contextlib.ExitStack() as sC:
                    hj = [sb(sC, "hj%d" % i, [128, 4, 512], F32) for i in range(2)]
                    hn32 = sb(sC, "hn32", [128, D], F32)
                    hnT = sb(sC, "hnT", [128, NCH, 128], F32)
                    lg = sb(sC, "lg", [128, 72], F32)
                    sm = sb(sC, "sm", [128, 16], F32)
                    oh = sb(sC, "oh", [128, 8], F32)
                    el = sb(sC, "el", [128, 8], F32)
                    t64 = sb(sC, "t64", [128, 8, 8], F32)
                    e8 = sb(sC, "e8", [128, 8], F32)
                    m12 = sb(sC, "m12", [128, 2, 8], F32)
                    w_out_v = w_out.rearrange("(c p) n -> p c n", p=128)
                    for j in range(4):
                        wo_, no = stream_w(w_out_v[:, :, j * 512:(j + 1) * 512])
                        hb = hj[j % 2]
                        hbn = "hj%d" % (j % 2)
                        P.dma("sp", lambda e, j=j, hb=hb: e.dma_start(
                            out=hb[:], in_=xw[row0:row0 + 512, j * 512:(j + 1) * 512].rearrange("(t p) c -> p t c", p=128)),
                            writes=[hbn])
                        for t in range(4):
                            k = nextpA()
                            for c in range(NCH):
                                P.op("pe", lambda e, c=c, k=k, t=t: e.matmul(pA[k][:], lhsT=mcT[:, c, t * 128:(t + 1) * 128],
                                                                            rhs=wo_[:, c, :], start=(c == 0), stop=(c == NCH - 1)),
                                     reads=["mcT", no], writes=["pA%d" % k])
                            P.op("dve", lambda e, k=k, t=t, hb=hb: e.tensor_tensor(out=hb[:, t, :], in0=pA[k][:], in1=hb[:, t, :],
                                                                                  op=ALU.add),
                                 reads=["pA%d" % k, hbn], writes=[hbn])
                        P.dma("sp", lambda e, j=j, hb=hb: e.dma_start(
                            out=h_scr[gi * 512:(gi + 1) * 512, j * 512:(j + 1) * 512].rearrange("(t p) c -> p t c", p=128),
                            in_=hb[:]), reads=[hbn], writes=["h_scr"])
                    for t in range(4):
                        ti = gi * 4 + t
                        i = nt_i[0] % 2
                        nt_i[0] += 1
                        r_ = gi * 512 + t * 128
                        P.dma("sp", lambda e, i=i, r_=r_: e.dma_start(out=xt[i][:], in_=h_scr[r_:r_ + 128, :]),
                              reads=["h_scr"], writes=["xt%d" % i])
                        rms_scale(xt[i], "xt%d" % i, i, gB, "gB", hn32, "hn32")
                        P.op("act", lambda e, i=i: e.copy(out=xs[i][:], in_=hn32[:]), reads=["hn32"], writes=["xs%d" % i])
                        P.dma("sp", lambda e, i=i, r_=r_: e.dma_start(out=hn_scr[r_:r_ + 128, :], in_=xs[i][:]),
                              reads=["xs%d" % i], writes=["hn_scr"])
                        for c4 in range(4):
                            k = nextpA()
                            for c1 in range(4):
                                c = c4 * 4 + c1
                                P.op("pe", lambda e, c=c, c1=c1, k=k: e.transpose(out=pA[k][:, c1 * 128:(c1 + 1) * 128],
                                                                                  in_=hn32[:, c * 128:(c + 1) * 128], identity=idf[:]),
                                     reads=["hn32", "idf"], writes=["pA%d" % k])
                            P.op("dve" if c4 % 2 else "act",
                                 (lambda e, k=k, c4=c4: e.tensor_copy(out=hnT[:, c4 * 4:c4 * 4 + 4, :],
                                                                      in_=pA[k][:].rearrange("p (a b) -> p a b", a=4))) if c4 % 2 else
                                 (lambda e, k=k, c4=c4: e.copy(out=hnT[:, c4 * 4:c4 * 4 + 4, :],
                                                               in_=pA[k][:].rearrange("p (a b) -> p a b", a=4))),
                                 reads=["pA%d" % k], writes=["hnT"])
                        k = nextpA()
                        for c in range(NCH):
                            P.op("pe", lambda e, c=c, k=k: e.matmul(pA[k][:, 0:72], lhsT=hnT[:, c, :], rhs=wr32[:, c, :],
                                                                    start=(c == 0), stop=(c == NCH - 1)),
                                 reads=["hnT", "wr32"], writes=["pA%d" % k])
                        P.op("dve", lambda e, k=k: e.tensor_tensor(out=lg[:], in0=pA[k][:, 0:72], in1=brb[:], op=ALU.add),
                             reads=["pA%d" % k, "brb"], writes=["lg"])
                        P.op("dve", lambda e: e.max(out=e8[:], in_=lg[:, 0:8]), reads=["lg"], writes=["e8"])
                        P.op("dve", lambda e: e.tensor_scalar(out=oh[:], in0=lg[:, 0:8], scalar1=e8[:, 0:1], scalar2=None,
                                                             op0=ALU.is_ge), reads=["lg", "e8"], writes=["oh"])
                        P.op("dve", lambda e: e.tensor_scalar(out=sm[:, 0:1], in0=e8[:, 0:1], scalar1=-1.0, scalar2=None,
                                                             op0=ALU.mult), reads=["e8"], writes=["sm"])
                        P.op("act", lambda e: e.activation(out=sm[:, 8:16], in_=lg[:, 0:8], func=AF.Exp, bias=sm[:, 0:1],
                                                           accum_out=sm[:, 1:2]), reads=["lg", "sm"], writes=["sm"])
                        P.op("dve", lambda e: e.tensor_tensor(out=t64[:], in0=lg[:, 8:72].rearrange("p (g e) -> p g e", g=8),
                                                              in1=oh[:].unsqueeze(2).to_broadcast([128, 8, 8]), op=ALU.mult),
                             reads=["lg", "oh"], writes=["t64"])
                        P.op("dve", lambda e: e.tensor_reduce(out=el[:], in_=t64[:].rearrange("p g e -> p e g"), axis=AX.X,
                                                              op=ALU.add), reads=["t64"], writes=["el"])
                        P.op("dve", lambda e: e.max(out=e8[:], in_=el[:]), reads=["el"], writes=["e8"])
                        P.op("dve", lambda e: e.tensor_scalar(out=m12[:, 0, :], in0=el[:], scalar1=e8[:, 0:1], scalar2=None,
                                                             op0=ALU.is_equal), reads=["el", "e8"], writes=["m12"])
                        P.op("dve", lambda e: e.tensor_scalar(out=m12[:, 1, :], in0=el[:], scalar1=e8[:, 1:2], scalar2=None,
                                                             op0=ALU.is_equal), reads=["el", "e8"], writes=["m12"])
                        P.op("dve", lambda e: e.tensor_tensor(out=sm[:, 2:3], in0=e8[:, 1:2], in1=e8[:, 0:1], op=ALU.subtract),
                             reads=["e8"], writes=["sm"])
                        P.op("act", lambda e: e.activation(out=sm[:, 3:4], in_=sm[:, 2:3], func=AF.Exp),
                             reads=["sm"], writes=["sm"])
                        P.op("dve", lambda e: e.tensor_scalar(out=sm[:, 3:4], in0=sm[:, 3:4], scalar1=1.0, scalar2=None,
                                                             op0=ALU.add), reads=["sm"], writes=["sm"])
                        P.op("dve", lambda e: e.tensor_tensor(out=sm[:, 4:5], in0=sm[:, 3:4], in1=sm[:, 1:2], op=ALU.mult),
                             reads=["sm"], writes=["sm"])
                        P.op("dve", lambda e, ti=ti: e.reciprocal(out=comb[:, ti, 0:1], in_=sm[:, 4:5]),
                             reads=["sm"], writes=["comb"])
                        P.op("dve", lambda e: e.reciprocal(out=sm[:, 5:6], in_=sm[:, 1:2]), reads=["sm"], writes=["sm"])
                        P.op("dve", lambda e, ti=ti: e.tensor_tensor(out=comb[:, ti, 1:2], in0=sm[:, 5:6], in1=comb[:, ti, 0:1],
                                                                    op=ALU.subtract), reads=["sm", "comb"], writes=["comb"])
                        for kx, Mk in ((0, M1all), (1, M2all)):
                            P.op("dve", lambda e, kx=kx, Mk=Mk, ti=ti: e.tensor_tensor(
                                out=Mk[:, ti, :].rearrange("p (g e) -> p g e", g=8),
                                in0=oh[:].unsqueeze(2).to_broadcast([128, 8, 8]),
                                in1=m12[:, kx, :].unsqueeze(1).to_broadcast([128, 8, 8]), op=ALU.mult),
                                reads=["oh", "m12"], writes=["M%d" % kx])
                        P.op("dve", lambda e, ti=ti: e.tensor_tensor(out=Mall[:, ti, :], in0=M1all[:, ti, :], in1=M2all[:, ti, :],
                                                                    op=ALU.add), reads=["M0", "M1"], writes=["Mall"])
                    P.barrier()
        P.barrier()
        if stage == 2:
            dbg_cb = nc.dram_tensor("dbg_cb", [128, 16, 2], F32, kind="ExternalOutput").ap()
            dbg_m = nc.dram_tensor("dbg_m", [128, 16, NEXP], BF16, kind="ExternalOutput").ap()
            P.dma("sp", lambda e: e.dma_start(out=dbg_cb, in_=comb[:]))
            P.dma("sp", lambda e: e.dma_start(out=dbg_m, in_=Mall[:]))
            P.barrier()
            P.finish([])
            return nc
        IOA = bass.IndirectOffsetOnAxis
        with contextlib.ExitStack() as s3:
            cntf = sb(s3, "cntf", [128, NEXP], F32)
            sa = sb(s3, "sa", [128, NEXP], F32)
            sbb = sb(s3, "sbb", [128, NEXP], F32)
            padded = sb(s3, "padded", [128, NEXP], F32)
            pstart = sb(s3, "pstart", [128, NEXP], F32)
            posf = sb(s3, "posf", [128, NEXP], F32)
            tmp64 = sb(s3, "tmp64", [128, NEXP], F32)
            destf = sb(s3, "destf", [128, 16, 2], F32)
            bef = sb(s3, "bef", [128, MOE_BLOCKS], F32)
            idxw = sb(s3, "idxw", [128, MOE_BLOCKS, 4], I32)
            idx4f = sb(s3, "idx4f", [128, MOE_BLOCKS, 4], F32)
            bigp = sb(s3, "bigp", [128, 1], F32)
            unus = sb(s3, "unus", [128, MOE_BLOCKS], F32)
            P.op("pool", lambda e: e.memset(junk[:], 0.0), writes=["junk"])
            for b1 in range(MOE_BLOCKS):
                P.dma("sp", lambda e, b1=b1: e.dma_start(out=xs_scr[b1 * 128:(b1 + 1) * 128, :], in_=junk[:]),
                      reads=["junk"], writes=["xs_scr"])
            kc = nextpA()
            for j in range(16):
                P.op("pe", lambda e, j=j: e.matmul(pA[kc][:, 0:NEXP], lhsT=ones_b[:], rhs=Mall[:, j, :],
                                                   start=(j == 0), stop=(j == 15)),
                     reads=["ones_b", "Mall"], writes=["pA%d" % kc])
            P.op("dve", lambda e: e.tensor_copy(out=cntf[:], in_=pA[kc][:, 0:NEXP]), reads=["pA%d" % kc], writes=["cntf"])
            with contextlib.ExitStack() as s3b:
                cmpb = sb(s3b, "cmpb", [128, NEXP, 16], F32)
                P.op("dve", lambda e: e.tensor_tensor(
                    out=cmpb[:], in0=cntf[:].unsqueeze(2).to_broadcast([128, NEXP, 16]),
                    in1=bstt[:, 0:16].unsqueeze(1).to_broadcast([128, NEXP, 16]), op=ALU.is_gt),
                    reads=["cntf", "bstt"], writes=["cmpb"])
                P.op("dve", lambda e: e.tensor_reduce(out=padded[:], in_=cmpb[:], axis=AX.X, op=ALU.add),
                     reads=["cmpb"], writes=["padded"])
                P.op("dve", lambda e: e.tensor_scalar(out=padded[:], in0=padded[:], scalar1=128.0, scalar2=None,
                                                     op0=ALU.mult), reads=["padded"], writes=["padded"])
                P.barrier()
            P.op("dve", lambda e: e.tensor_copy(out=sa[:], in_=padded[:]), reads=["padded", "sbb"], writes=["sa"])
            cur, oth, cn, on = sa, sbb, "sa", "sbb"
            for sft in (1, 2, 4, 8, 16, 32):
                P.op("dve", lambda e, cur=cur, oth=oth, sft=sft: e.tensor_copy(out=oth[:, 0:sft], in_=cur[:, 0:sft]),
                     reads=[cn], writes=[on])
                P.op("dve", lambda e, cur=cur, oth=oth, sft=sft: e.tensor_tensor(
                    out=oth[:, sft:NEXP], in0=cur[:, sft:NEXP], in1=cur[:, 0:NEXP - sft], op=ALU.add),
                    reads=[cn], writes=[on])
                cur, oth, cn, on = oth, cur, on, cn
            pend, pendn = cur, cn
            P.op("dve", lambda e: e.tensor_tensor(out=pstart[:], in0=pend[:], in1=padded[:], op=ALU.subtract),
                 reads=[pendn, "padded"], writes=["pstart"])
            for ti in range(16):
                kr = nextpA()
                for j in range(ti):
                    P.op("pe", lambda e, j=j: e.matmul(pA[kr][:, 0:NEXP], lhsT=ones_b[:], rhs=Mall[:, j, :],
                                                       start=(j == 0), stop=False),
                         reads=["ones_b", "Mall"], writes=["pA%d" % kr])
                P.op("pe", lambda e, ti=ti: e.matmul(pA[kr][:, 0:NEXP], lhsT=ust[:], rhs=Mall[:, ti, :],
                                                     start=(ti == 0), stop=True),
                     reads=["ust", "Mall"], writes=["pA%d" % kr])
                P.op("dve", lambda e: e.tensor_tensor(out=posf[:], in0=pA[kr][:, 0:NEXP], in1=pstart[:], op=ALU.add),
                     reads=["pA%d" % kr, "pstart"], writes=["posf"])
                for kx, Mk in ((0, M1all), (1, M2all)):
                    P.op("dve", lambda e, Mk=Mk, ti=ti: e.tensor_tensor(out=tmp64[:], in0=posf[:], in1=Mk[:, ti, :], op=ALU.mult),
                         reads=["posf", "M%d" % kx], writes=["tmp64"])
                    P.op("dve", lambda e, ti=ti, kx=kx: e.tensor_reduce(out=destf[:, ti, kx:kx + 1], in_=tmp64[:], axis=AX.X,
                                                                        op=ALU.add), reads=["tmp64"], writes=["destf"])
            P.op("dve", lambda e: e.tensor_copy(out=desti[:], in_=destf[:]), reads=["destf"], writes=["desti"])
            with contextlib.ExitStack() as s3a:
                cmp = sb(s3a, "cmp", [128, MOE_BLOCKS, NEXP], F32)
                P.op("dve", lambda e: e.tensor_tensor(
                    out=cmp[:], in0=pend[:].unsqueeze(1).to_broadcast([128, MOE_BLOCKS, NEXP]),
                    in1=bstt[:].unsqueeze(2).to_broadcast([128, MOE_BLOCKS, NEXP]), op=ALU.is_le),
                    reads=[pendn, "bstt"], writes=["cmp"])
                P.op("dve", lambda e: e.tensor_reduce(out=bef[:], in_=cmp[:], axis=AX.X, op=ALU.add),
                     reads=["cmp"], writes=["bef"])
                P.op("dve", lambda e: e.tensor_scalar(out=bigp[:], in0=pidx[:], scalar1=0.5, scalar2=200000.0,
                                                     op0=ALU.is_gt, op1=ALU.mult), reads=["pidx"], writes=["bigp"])
                P.op("dve", lambda e: e.tensor_scalar(out=unus[:], in0=bef[:], scalar1=float(NEXP) - 0.5, scalar2=bigp[:, 0:1],
                                                     op0=ALU.is_gt, op1=ALU.mult), reads=["bef", "bigp"], writes=["unus"])
                P.op("dve", lambda e: e.tensor_scalar(out=bef[:], in0=bef[:], scalar1=float(NEXP - 1), scalar2=None,
                                                     op0=ALU.min), reads=["bef"], writes=["bef"])
                P.op("dve", lambda e: e.tensor_scalar(out=bef[:], in0=bef[:], scalar1=128.0, scalar2=pidx[:, 0:1],
                                                     op0=ALU.mult, op1=ALU.add), reads=["bef", "pidx"], writes=["bef"])
                for a in range(4):
                    P.op("dve", lambda e: e.tensor_scalar(out=idx4f[:, :, a], in0=bef[:], scalar1=4.0, scalar2=float(a),
                                                         op0=ALU.mult, op1=ALU.add), reads=["bef"], writes=["idx4f"])
                P.op("dve", lambda e: e.tensor_copy(out=idxw[:], in_=idx4f[:]), reads=["idx4f"], writes=["idxw"])
                P.barrier()
            for ti in range(16):
                i = ti % 2
                P.dma("sp", lambda e, ti=ti, i=i: e.dma_start(out=xs[i][:], in_=hn_scr[ti * 128:(ti + 1) * 128, :]),
                      reads=["hn_scr"], writes=["xs%d" % i])
                for kx in range(2):
                    P.dma("pool", lambda e, ti=ti, i=i, kx=kx: e.indirect_dma_start(
                        out=xs_scr, out_offset=IOA(ap=desti[:, ti, kx:kx + 1], axis=0), in_=xs[i][:], in_offset=None),
                        reads=["xs%d" % i, "desti", "xs_scr"], writes=["xs_scr%d" % (ti * 2 + kx)])
            P.barrier()
            if stage == 2.5:
                dbg_di = nc.dram_tensor("dbg_di", [128, 16, 2], I32, kind="ExternalOutput").ap()
                dbg_ix = nc.dram_tensor("dbg_ix", [128, MOE_BLOCKS, 4], I32, kind="ExternalOutput").ap()
                P.dma("sp", lambda e: e.dma_start(out=dbg_di, in_=desti[:]))
                P.dma("sp", lambda e: e.dma_start(out=dbg_ix, in_=idxw[:]))
                P.barrier()
                P.finish([])
                return nc
            wgb = [sb(s3, "wgb%d" % i, [128, 4, 2048], BF16) for i in range(2)]
            wub = [sb(s3, "wub%d" % i, [128, 4, 2048], BF16) for i in range(2)]
            wdb = [sb(s3, "wdb%d" % i, [128, 4, 2048], BF16) for i in range(2)]
            xbb = [sb(s3, "xbb%d" % i, [128, D], BF16) for i in range(2)]
            xTb = [sb(s3, "xTb%d" % i, [128, NCH, 128], BF16) for i in range(2)]
            sgm = sb(s3, "sgm", [128, DEXP], F32)
            hdn = sb(s3, "hdn", [128, DEXP], BF16)
            hTb = sb(s3, "hTb", [128, 4, 128], BF16)
            yb = [sb(s3, "yb%d" % i, [128, D], F32) for i in range(2)]
            weg_v = w_eg.rearrange("e (p a b) f -> (e p a) (b f)", a=4, b=4)
            weu_v = w_eu.rearrange("e (p a b) f -> (e p a) (b f)", a=4, b=4)
            wed_v = w_ed.rearrange("e (p c) f -> (e p c) f", c=4)
            for blk in range(MOE_BLOCKS):
                b = blk % 2
                for (dst, src, nm) in ((wgb[b], weg_v, "wgb%d" % b), (wub[b], weu_v, "wub%d" % b), (wdb[b], wed_v, "wdb%d" % b)):
                    for a in range(4):
                        P.dma("pool", lambda e: e.indirect_dma_start(
                            out=dst[:, a, :], out_offset=None, in_=src, in_offset=IOA(ap=idxw[:, blk, a:a + 1], axis=0)),
                            reads=["idxw"], writes=[nm + "_%d" % a])
                P.dma("sp", lambda e, blk=blk, b=b: e.dma_start(out=xbb[b][:], in_=xs_scr[blk * 128:(blk + 1) * 128, :]),
                      reads=["xs_scr"], writes=["xbb%d" % b])
                xv = xbb[b][:].rearrange("s (p c) -> s c p", c=16)
                for c in range(NCH):
                    P.op("pe", lambda e, c=c, xv=xv: e.transpose(out=pT[:, c // 8, c % 8, :], in_=xv[:, c, :], identity=idb[:]),
                         reads=["xbb%d" % b, "idb"], writes=["pT%d" % (c // 8)])
                P.op("dve", lambda e, b=b: e.tensor_copy(out=xTb[b][:, 0:8, :], in_=pT[:, 0, :, :]),
                     reads=["pT0"], writes=["xTb%d" % b])
                P.op("act", lambda e, b=b: e.copy(out=xTb[b][:, 8:16, :], in_=pT[:, 1, :, :]),
                     reads=["pT1"], writes=["xTb%d" % b])
                kg = nextpA()
                ku = nextpA()
                for (kk_, wt, wn) in ((kg, wgb[b], "wgb%d" % b), (ku, wub[b], "wub%d" % b)):
                    wv2 = wt[:].rearrange("p a (b f) -> p (a b) f", b=4)
                    for c in range(NCH):
                        P.op("pe", lambda e, c=c, kk_=kk_, wv2=wv2, b=b: e.matmul(pA[kk_][:], lhsT=xTb[b][:, c, :], rhs=wv2[:, c, :],
                                                                                start=(c == 0), stop=(c == NCH - 1)),
                             reads=["xTb%d" % b] + [wn + "_%d" % a for a in range(4)], writes=["pA%d" % kk_])
                P.op("act", lambda e, kg=kg: e.activation(out=sgm[:], in_=pA[kg][:], func=AF.Silu),
                     reads=["pA%d" % kg], writes=["sgm"])
                P.op("dve", lambda e, ku=ku: e.tensor_tensor(out=hdn[:], in0=pA[ku][:], in1=sgm[:], op=ALU.mult),
                     reads=["pA%d" % ku, "sgm"], writes=["hdn"])
                hv = hdn[:].rearrange("s (p c) -> s c p", c=4)
                for c in range(4):
                    P.op("pe", lambda e, c=c, hv=hv: e.transpose(out=pT[:, 0, c, :], in_=hv[:, c, :], identity=idb[:]),
                         reads=["hdn", "idb"], writes=["pT0"])
                P.op("dve", lambda e: e.tensor_copy(out=hTb[:], in_=pT[:, 0, 0:4, :]), reads=["pT0"], writes=["hTb"])
                for j in range(4):
                    k = nextpA()
                    for c in range(4):
                        P.op("pe", lambda e, c=c, k=k, j=j, b=b: e.matmul(pA[k][:], lhsT=hTb[:, c, :],
                                                                         rhs=wdb[b][:, c, j * 512:(j + 1) * 512],
                                                                         start=(c == 0), stop=(c == 3)),
                             reads=["hTb"] + ["wdb%d_%d" % (b, a) for a in range(4)], writes=["pA%d" % k])
                    if j % 2 == 0:
                        P.op("act", lambda e, k=k, j=j, b=b: e.copy(out=yb[b][:, j * 512:(j + 1) * 512], in_=pA[k][:]),
                             reads=["pA%d" % k], writes=["yb%d" % b])
                    else:
                        P.op("dve", lambda e, k=k, j=j, b=b: e.tensor_copy(out=yb[b][:, j * 512:(j + 1) * 512], in_=pA[k][:]),
                             reads=["pA%d" % k], writes=["yb%d" % b])
                P.dma("sp", lambda e, blk=blk, b=b: e.dma_start(out=ys_scr[blk * 128:(blk + 1) * 128, :], in_=yb[b][:]),
                      reads=["yb%d" % b], writes=["ys_scr"])
            P.barrier()
            if stage == 3:
                dbg_di = nc.dram_tensor("dbg_di", [128, 16, 2], I32, kind="ExternalOutput").ap()
                dbg_ix = nc.dram_tensor("dbg_ix", [128, MOE_BLOCKS, 4], I32, kind="ExternalOutput").ap()
                P.dma("sp", lambda e: e.dma_start(out=dbg_di, in_=desti[:]))
                P.dma("sp", lambda e: e.dma_start(out=dbg_ix, in_=idxw[:]))
                P.barrier()
                P.finish([])
                return nc
        finals = []
        with contextlib.ExitStack() as s4:
            yg = [[sb(s4, "yg%d_%d" % (i, kx), [128, D], F32) for kx in range(2)] for i in range(2)]
            of = [sb(s4, "of%d" % i, [128, D], F32) for i in range(2)]
            P.dma("sp", lambda e: e.dma_start(out=gA[:], in_=gf_d.partition_broadcast(128)), writes=["gA"])
            for ti in range(16):
                i = ti % 2
                P.dma("sp", lambda e, ti=ti, i=i: e.dma_start(out=xt[i][:], in_=h_scr[ti * 128:(ti + 1) * 128, :]),
                      reads=["h_scr"], writes=["xt%d" % i])
                for kx in range(2):
                    P.dma("pool", lambda e, ti=ti, i=i, kx=kx: e.indirect_dma_start(
                        out=yg[i][kx][:], out_offset=None, in_=ys_scr, in_offset=IOA(ap=desti[:, ti, kx:kx + 1], axis=0)),
                        reads=["ys_scr", "desti"], writes=["yg%d_%d" % (i, kx)])
                for kx in range(2):
                    P.op("dve", lambda e, ti=ti, i=i, kx=kx: e.scalar_tensor_tensor(
                        out=xt[i][:], in0=yg[i][kx][:], scalar=comb[:, ti, kx:kx + 1], in1=xt[i][:],
                        op0=ALU.mult, op1=ALU.add), reads=["yg%d_%d" % (i, kx), "comb", "xt%d" % i], writes=["xt%d" % i])
                rms_scale(xt[i], "xt%d" % i, i, gA, "gA", of[i], "of%d" % i)
                finals.append(P.dma("sp", lambda e, ti=ti, i=i: e.dma_start(out=out_d[ti * 128:(ti + 1) * 128, :], in_=of[i][:]),
                                    reads=["of%d" % i]))
        P.finish(finals)
    return nc


def _consts():
    p = np.arange(128, dtype=np.float64)
    slopes = np.array([2.0 ** (-8.0 * (i + 1) / HEADS) for i in range(HEADS)])
    ident = np.eye(128)
    tri = (p[:, None] <= p[None, :]).astype(np.float64)
    ust = (p[:, None] < p[None, :]).astype(np.float64)
    bias = np.stack([slopes[None, :] * (p[:, None] - 255.0), slopes[None, :] * (p[:, None] - 127.0),
                     slopes[None, :] * p[:, None]], axis=-1)
    facd = np.stack([np.exp(-slopes[None, :] * p[:, None]), np.exp(-slopes[None, :] * (p[:, None] + 1.0))], axis=-1)
    qpos = 6144.0 + np.arange(16)[None, :, None] * 128.0 + p[:, None, None]
    kend = (np.arange(NBLK) * 256.0 + 255.0)[None, None, :]
    dist = np.maximum(qpos - kend, 0.0)
    return dict(
        ident_bf=ident.astype(ml_dtypes.bfloat16), ident_f=ident.astype(np.float32),
        tri_bf=tri.astype(ml_dtypes.bfloat16), ustrict_bf=ust.astype(ml_dtypes.bfloat16),
        bias_tab=bias.astype(np.float32), facd_tab=facd.astype(np.float32), dist_tab=dist.astype(np.float32),
        pidx=p.astype(np.float32)[:, None],
        blkstart=np.broadcast_to((np.arange(MOE_BLOCKS) * 128.0)[None, :], (128, MOE_BLOCKS)).astype(np.float32).copy())


def kernel(x, norm1_g, w_in, conv_dw_w, conv_dw_b, conv_ln_g, conv_ln_b, w_conv_out, w_attn_out, gate_b, w_out,
           norm2_g, w_router_group, b_router_group, w_router_expert, b_router_expert, w_exp_gate, w_exp_up,
           w_exp_down, norm_f_g):
    f = lambda a: np.ascontiguousarray(np.asarray(a, dtype=np.float32))
    x = f(x)
    fm = lambda v, n: f(v).reshape(n, 128).T.copy()
    shared = dict(
        w_in=f(w_in)[0], w_conv_out=f(w_conv_out)[0], w_attn_out=f(w_attn_out)[0], w_out=f(w_out)[0],
        w_exp_gate=f(w_exp_gate)[0], w_exp_up=f(w_exp_up)[0], w_exp_down=f(w_exp_down)[0],
        norm1_g=f(norm1_g).reshape(1, D), norm2_g=f(norm2_g).reshape(1, D), norm_f_g=f(norm_f_g).reshape(1, D),
        dw_w=np.ascontiguousarray(f(conv_dw_w)[0].T.reshape(8, 128, TAPS).transpose(1, 0, 2)),
        chvec=np.ascontiguousarray(np.stack([fm(conv_dw_b, 8), fm(conv_ln_g, 8), fm(conv_ln_b, 8)], axis=-1)),
        gate_b=fm(gate_b, 32),
        w_router=np.ascontiguousarray(np.concatenate([f(w_router_group)[0], f(w_router_expert)[0]], axis=1)
                                      .reshape(NCH, 128, 72).transpose(1, 0, 2)),
        b_router=np.concatenate([f(b_router_group).reshape(-1), f(b_router_expert).reshape(-1)])[None, :].copy(),
    )
    shared.update(_consts())
    in_maps = []
    for c in range(8):
        b, r = c // 4, c % 4
        xwin = np.zeros((SEQ, D), np.float32)
        n_valid = (r + 1) * NOWN
        xwin[SEQ - n_valid:] = x[b, :n_valid]
        first_valid_blk = (SEQ - n_valid) // 256
        gm = np.full((128, 16, NBLK), -1e30, np.float32)
        for qt in range(16):
            own = (6144 + qt * 128) // 256
            gm[:, qt, first_valid_blk:own] = 0.0
        m = dict(shared)
        m["xw"] = xwin
        m["gmask"] = gm
        in_maps.append(m)
    nc = build()
    res = run_bass_kernel_spmd(nc, in_maps, core_ids=list(range(8)))
    out = np.zeros((2, SEQ, D), np.float32)
    for c in range(8):
        b, r = c // 4, c % 4
        out[b, r * NOWN:(r + 1) * NOWN] = res.results[c]["out"]
    return out
```

```python
import contextlib
import numpy as np
import ml_dtypes
import concourse.bass as bass
import concourse.mybir as mybir
from concourse.bass_utils import run_bass_kernel_spmd

F32 = mybir.dt.float32
BF16 = mybir.dt.bfloat16
I32 = mybir.dt.int32
U32 = mybir.dt.uint32
ALU = mybir.AluOpType
AF = mybir.ActivationFunctionType
AX = mybir.AxisListType

D = 2048
SEQ = 8192
NOWN = 2048
NCH = 16
HEADS = 8
HD = 128
CC = 1024
TAPS = 31
NBLK = 32
EPS = 1e-6
IN_COLS = 9216
C_VAL, C_GATE, C_Q, C_K, C_V, C_GC, C_GA = 0, 1024, 2048, 3072, 4096, 5120, 7168
NEXP = 64
DEXP = 512
MOE_BLOCKS = 96
NSLOT = MOE_BLOCKS * 128
VP = 132


class _Rec:
    def __init__(self):
        self.call = None

    def __getattr__(self, name):
        def f(*a, **kw):
            self.call = (name, a, kw)
            return self
        return f


def _bind(fn):
    r = _Rec()
    fn(r)
    name, a, kw = r.call
    return lambda e: getattr(e, name)(*a, **kw)


class Prog:
    CE = ("pe", "act", "dve", "pool")
    RING = 20

    def __init__(self, nc, stack):
        self.nc = nc
        self.eng = {"pe": nc.tensor, "act": nc.scalar, "dve": nc.vector,
                    "pool": nc.gpsimd, "sp": nc.sync}
        self.q = {k: [] for k in self.eng}
        self.sem = {e: stack.enter_context(nc.semaphore("c_" + e)) for e in self.CE}
        self.cnt = {e: 0 for e in self.CE}
        self.dsem = {qn: [stack.enter_context(nc.semaphore("d_%s%d" % (qn, i)))
                          for i in range(self.RING)] for qn in ("sp", "pool", "act")}
        self.dcnt = {qn: 0 for qn in self.dsem}
        self.last_w = {}
        self.readers = {}
        self.groups = {}
        self.seen = {e: {} for e in self.eng}
        self.semobj = {}
        for e in self.CE:
            self.semobj[("c", e)] = self.sem[e]
        for qn in self.dsem:
            for i, s in enumerate(self.dsem[qn]):
                self.semobj[("d", qn, i)] = s

    def _expand(self, names):
        out = []
        for n in names:
            out.extend(self.groups.get(n, (n,)))
        return out

    def _deps(self, eng, reads, writes):
        reads = self._expand(reads)
        writes = self._expand(writes)
        deps = {}

        def add(ev):
            k, v = ev
            if k == ("c", "pe") and eng == "pe":
                return
            if deps.get(k, 0) < v:
                deps[k] = v
        for r in reads:
            if r in self.last_w:
                add(self.last_w[r])
        for w in writes:
            if w in self.last_w:
                add(self.last_w[w])
            for ev in self.readers.get(w, ()):
                add(ev)
        need = []
        for k, v in deps.items():
            if self.seen[eng].get(k, 0) < v:
                self.seen[eng][k] = v
                need.append((k, v))
        return need

    def _commit(self, ev, reads, writes):
        reads = self._expand(reads)
        writes = self._expand(writes)
        for r in reads:
            self.readers.setdefault(r, []).append(ev)
        for w in writes:
            self.last_w[w] = ev
            self.readers[w] = []

    def op(self, eng, fn, reads=(), writes=()):
        fn = _bind(fn)
        pr = [r for r in reads if r[:2] in ("pA", "pT", "pO")]
        if pr:
            reads = [r for r in reads if r not in pr]
            writes = list(writes) + pr
        need = self._deps(eng, reads, writes)
        self.cnt[eng] += 1
        ev = (("c", eng), self.cnt[eng])
        self.seen[eng][ev[0]] = max(self.seen[eng].get(ev[0], 0), 0)
        sem = self.sem[eng]
        waits = [(self.semobj[k], v) for k, v in need]

        def emit(e, fn=fn, waits=waits, sem=sem):
            for s, v in waits:
                e.wait_ge(s, v)
            fn(e).then_inc(sem, 1)
        self.q[eng].append(emit)
        self._commit(ev, reads, writes)
        return ev

    def dma(self, qn, fn, reads=(), writes=()):
        fn = _bind(fn)
        j = self.dcnt[qn]
        self.dcnt[qn] += 1
        slot, rnd = j % self.RING, j // self.RING
        key = ("d", qn, slot)
        need = self._deps(qn, reads, writes)
        if rnd > 0 and self.seen[qn].get(key, 0) < 16 * rnd:
            self.seen[qn][key] = 16 * rnd
            need.append((key, 16 * rnd))
        ev = (key, 16 * (rnd + 1))
        sem = self.semobj[key]
        waits = [(self.semobj[k], v) for k, v in need]

        def emit(e, fn=fn, waits=waits, sem=sem):
            for s, v in waits:
                e.wait_ge(s, v)
            fn(e).then_inc(sem, 16)
        self.q[qn].append(emit)
        self._commit(ev, reads, writes)
        return ev

    def finish(self, final_events):
        waits = {}
        for k, v in final_events:
            waits[k] = max(waits.get(k, 0), v)
        fw = [(self.semobj[k], v) for k, v in waits.items()]
        q = self.q
        with self.nc.Block() as block:
            @block.tensor
            def _(e):
                for f in q["pe"]:
                    f(e)

            @block.scalar
            def _(e):
                for f in q["act"]:
                    f(e)

            @block.vector
            def _(e):
                for f in q["dve"]:
                    f(e)

            @block.gpsimd
            def _(e):
                for f in q["pool"]:
                    f(e)

            @block.sync
            def _(e):
                for f in q["sp"]:
                    f(e)
                for s, v in fw:
                    e.wait_ge(s, v)


    def barrier(self):
        latest = {}
        for e in self.CE:
            if self.cnt[e]:
                latest[("c", e)] = self.cnt[e]
        for qn in self.dsem:
            for j in range(max(0, self.dcnt[qn] - self.RING), self.dcnt[qn]):
                k = ("d", qn, j % self.RING)
                latest[k] = max(latest.get(k, 0), 16 * (j // self.RING + 1))
        for eng in self.eng:
            need = []
            for k, v in latest.items():
                if k == ("c", eng):
                    continue
                if self.seen[eng].get(k, 0) < v:
                    self.seen[eng][k] = v
                    need.append((self.semobj[k], v))
            if need:
                def emit(e, need=need):
                    for s_, v in need:
                        e.wait_ge(s_, v)
                self.q[eng].append(emit)
        self.last_w = {}
        self.readers = {}


def build(stage=4, debug=False):
    nc = bass.Bass("TRN2", target_bir_lowering=False)

    def din(name, shape, dt=F32):
        return nc.dram_tensor(name, list(shape), dt, kind="ExternalInput").ap()

    dbg_out = {1: ("kt_scr", "v_scr"), 2: ("h_scr", "hn_scr"), 2.5: ("xs_scr", "hn_scr"), 3: ("ys_scr", "xs_scr", "hn_scr", "h_scr")}.get(stage, ()) if debug else ()

    def dscr(name, shape, dt):
        return nc.dram_tensor(name, list(shape), dt, kind=("ExternalOutput" if name in dbg_out else "Internal")).ap()

    xw = din("xw", [SEQ, D])
    w_in = din("w_in", [D, IN_COLS])
    w_co = din("w_conv_out", [CC, D])
    w_ao = din("w_attn_out", [CC, D])
    w_out = din("w_out", [D, D])
    if stage >= 3:
        w_eg = din("w_exp_gate", [NEXP, D, DEXP])
        w_eu = din("w_exp_up", [NEXP, D, DEXP])
        w_ed = din("w_exp_down", [NEXP, DEXP, D])
    g1_d = din("norm1_g", [1, D])
    g2_d = din("norm2_g", [1, D])
    gf_d = din("norm_f_g", [1, D])
    dww_d = din("dw_w", [128, 8, TAPS])
    chv_d = din("chvec", [128, 8, 3])
    gb_d = din("gate_b", [128, 32])
    wr_d = din("w_router", [128, NCH, 72])
    br_d = din("b_router", [1, 72])
    idb_d = din("ident_bf", [128, 128], BF16)
    idf_d = din("ident_f", [128, 128])
    tri_d = din("tri_bf", [128, 128], BF16)
    ust_d = din("ustrict_bf", [128, 128], BF16)
    bb_d = din("bias_tab", [128, HEADS, 3])
    fd_d = din("facd_tab", [128, HEADS, 2])
    dist_d = din("dist_tab", [128, 16, NBLK])
    gm_d = din("gmask", [128, 16, NBLK])
    pidx_d = din("pidx", [128, 1])
    bst_d = din("blkstart", [128, MOE_BLOCKS])
    out_d = nc.dram_tensor("out", [NOWN, D], F32, kind="ExternalOutput").ap()

    kt_scr = dscr("kt_scr", [HEADS, 128, SEQ], BF16)
    v_scr = dscr("v_scr", [HEADS, 128, 64, VP], BF16)
    h_scr = dscr("h_scr", [NOWN, D], F32)
    hn_scr = dscr("hn_scr", [NOWN, D], BF16)
    xs_scr = dscr("xs_scr", [NSLOT, D], BF16)
    ys_scr = dscr("ys_scr", [NSLOT, D], F32)

    w_in_v = w_in.rearrange("(c p) n -> p c n", p=128)

    with contextlib.ExitStack() as st:
        P = Prog(nc, st)

        uniq = [0]

        def sb(stack, name, shape, dt):
            uniq[0] += 1
            return stack.enter_context(nc.sbuf_tensor("s%d_%s" % (uniq[0], name), list(shape), dt))

        def psum(stack, name, shape, dt):
            return stack.enter_context(nc.psum_tensor("p_" + name, list(shape), dt))

        idb = sb(st, "idb", [128, 128], BF16)
        idf = sb(st, "idf", [128, 128], F32)
        tri = sb(st, "tri", [128, 128], BF16)
        ust = sb(st, "ust", [128, 128], BF16)
        ones_b = sb(st, "ones_b", [128, 128], BF16)
        ones_f = sb(st, "ones_f", [128, 128], F32)
        bbt = sb(st, "bbt", [128, HEADS, 3], F32)
        fdt = sb(st, "fdt", [128, HEADS, 2], F32)
        distt = sb(st, "distt", [128, 16, NBLK], F32)
        gmt = sb(st, "gmt", [128, 16, NBLK], F32)
        pidx = sb(st, "pidx", [128, 1], F32)
        bstt = sb(st, "bstt", [128, MOE_BLOCKS], F32)
        dww = sb(st, "dww", [128, 8, TAPS], F32)
        chv = sb(st, "chv", [128, 8, 3], F32)
        gbt = sb(st, "gbt", [128, 32], F32)
        wr32 = sb(st, "wr32", [128, NCH, 72], F32)
        brb = sb(st, "brb", [128, 72], F32)
        gA = sb(st, "gA", [128, D], F32)
        gB = sb(st, "gB", [128, D], F32)
        kmean = sb(st, "kmean", [128, HEADS, NBLK], F32)
        kmb = sb(st, "kmb", [128, HEADS, NBLK], BF16)
        Mall = sb(st, "Mall", [128, 16, NEXP], BF16)
        M1all = sb(st, "M1all", [128, 16, NEXP], BF16)
        M2all = sb(st, "M2all", [128, 16, NEXP], BF16)
        zhalo = sb(st, "zhalo", [128, 8, 32], F32)
        comb = sb(st, "comb", [128, 16, 2], F32)
        desti = sb(st, "desti", [128, 16, 2], I32)
        xt = [sb(st, "xt%d" % i, [128, D], F32) for i in range(2)]
        junk = sb(st, "junk", [128, D], BF16)
        xs = [sb(st, "xs%d" % i, [128, D], BF16) for i in range(2)]
        ssr = [sb(st, "ss%d" % i, [128, 1], F32) for i in range(2)]
        rsr = [sb(st, "rs%d" % i, [128, 1], F32) for i in range(2)]

        pA = [psum(st, "pA%d" % i, [128, 512], F32) for i in range(4)]
        pT = psum(st, "pT", [128, 2, 8, 128], BF16)
        pO = psum(st, "pO", [128, 2, 512], F32)
        pa_i = [0]

        def nextpA():
            i = pa_i[0] % 4
            pa_i[0] += 1
            return i

        for (t_, d_, nm) in [(idb, idb_d, "idb"), (idf, idf_d, "idf"), (tri, tri_d, "tri"), (ust, ust_d, "ust"),
                             (bbt, bb_d, "bbt"), (fdt, fd_d, "fdt"), (distt, dist_d, "distt"), (gmt, gm_d, "gmt"),
                             (pidx, pidx_d, "pidx"), (bstt, bst_d, "bstt"), (dww, dww_d, "dww"), (chv, chv_d, "chv"),
                             (gbt, gb_d, "gbt"), (wr32, wr_d, "wr32")]:
            P.dma("sp", lambda e, t_=t_, d_=d_: e.dma_start(out=t_[:], in_=d_), writes=[nm])
        P.dma("sp", lambda e: e.dma_start(out=brb[:], in_=br_d.partition_broadcast(128)), writes=["brb"])
        P.dma("sp", lambda e: e.dma_start(out=gA[:], in_=g1_d.partition_broadcast(128)), writes=["gA"])
        P.dma("sp", lambda e: e.dma_start(out=gB[:], in_=g2_d.partition_broadcast(128)), writes=["gB"])
        P.op("dve", lambda e: e.memset(ones_b[:], 1.0), writes=["ones_b"])
        P.op("dve", lambda e: e.memset(ones_f[:], 1.0), writes=["ones_f"])

        if stage == 0:
            dbg_km = nc.dram_tensor("dbg_km", [128, HEADS, NBLK], F32, kind="ExternalOutput").ap()
            P.dma("sp", lambda e: e.dma_start(out=dbg_km, in_=distt[:, 0:8, :]), reads=["distt"])
            P.barrier()
            P.finish([])
            return nc
        nt_i = [0]

        def norm_tile(src_rows, gbuf, gname, dstT, dname, col):
            i = nt_i[0] % 2
            nt_i[0] += 1
            P.dma("sp", lambda e: e.dma_start(out=xt[i][:], in_=src_rows), writes=["xt%d" % i])
            rms_scale(xt[i], "xt%d" % i, i, gbuf, gname, xs[i], "xs%d" % i)
            transp16(xs[i], "xs%d" % i, dstT, dname, col)

        def rms_scale(src, sname, i, gbuf, gname, dst, dname):
            P.op("act", lambda e: e.activation(out=junk[:], in_=src[:], func=AF.Square, accum_out=ssr[i][:]),
                 reads=[sname], writes=["junk", "ss%d" % i])
            P.op("dve", lambda e: e.tensor_scalar(out=rsr[i][:], in0=ssr[i][:], scalar1=1.0 / D, scalar2=EPS,
                                                 op0=ALU.mult, op1=ALU.add), reads=["ss%d" % i], writes=["rs%d" % i])
            P.op("act", lambda e: e.sqrt(out=rsr[i][:], in_=rsr[i][:]), reads=["rs%d" % i], writes=["rs%d" % i])
            P.op("dve", lambda e: e.reciprocal(out=rsr[i][:], in_=rsr[i][:]), reads=["rs%d" % i], writes=["rs%d" % i])
            P.op("dve", lambda e: e.scalar_tensor_tensor(out=dst[:], in0=src[:], scalar=rsr[i][:], in1=gbuf[:],
                                                        op0=ALU.mult, op1=ALU.mult),
                 reads=[sname, "rs%d" % i, gname], writes=[dname])

        def transp16(src, sname, dstT, dname, col):
            for c in range(NCH):
                P.op("pe", lambda e, c=c: e.transpose(out=pT[:, c // 8, c % 8, :], in_=src[:, c * 128:(c + 1) * 128],
                                                      identity=idb[:]),
                     reads=[sname, "idb"], writes=["pT%d" % (c // 8)])
            P.op("dve", lambda e: e.tensor_copy(out=dstT[:, 0:8, col:col + 128], in_=pT[:, 0, :, :]),
                 reads=["pT0"], writes=[dname])
            P.op("act", lambda e: e.copy(out=dstT[:, 8:16, col:col + 128], in_=pT[:, 1, :, :]),
                 reads=["pT1"], writes=[dname])

        def load_w(dst_ap, src_ap, name, nsplit=4):
            C = src_ap.shape[1]
            step = max(1, C // nsplit)
            parts = ["%s#%d" % (name, c0) for c0 in range(0, C, step)]
            P.groups.pop(name, None)
            for c0 in range(0, C, step):
                P.dma("pool", lambda e, c0=c0: e.dma_start(out=dst_ap[:, c0:c0 + step, :],
                                                          in_=src_ap[:, c0:c0 + step, :]),
                      writes=["%s#%d" % (name, c0)])
            P.groups[name] = parts

        with contextlib.ExitStack() as s1:
            wk = sb(s1, "wk", [128, NCH, 1024], BF16)
            wv = sb(s1, "wv", [128, NCH, 1024], BF16)
            uT = [sb(s1, "uT%d" % i, [128, NCH, 512], BF16) for i in range(2)]
            ktg = [sb(s1, "ktg%d" % i, [128, HEADS, 512], BF16) for i in range(2)]
            vg = [sb(s1, "vg%d" % i, [128, HEADS, 4, VP], BF16) for i in range(2)]
            load_w(wk[:], w_in_v[:, :, C_K:C_K + 1024], "wk")
            load_w(wv[:], w_in_v[:, :, C_V:C_V + 1024], "wv")
            for i in range(2):
                P.op("pool", lambda e, i=i: e.memset(vg[i][:], 1.0), writes=["vg%d" % i])
            for g in range(16 if stage >= 1 else 1):
                b = g % 2
                for t in range(4):
                    r0 = g * 512 + t * 128
                    norm_tile(xw[r0:r0 + 128, :], gA, "gA", uT[b], "uT%d" % b, t * 128)
                for h in range(HEADS):
                    k = nextpA()
                    for c in range(NCH):
                        P.op("pe", lambda e, c=c, k=k, h=h: e.matmul(pA[k][:], lhsT=wk[:, c, h * 128:(h + 1) * 128],
                                                                    rhs=uT[b][:, c, :], start=(c == 0), stop=(c == NCH - 1)),
                             reads=["wk", "uT%d" % b], writes=["pA%d" % k])
                    P.op("act", lambda e, k=k, h=h: e.copy(out=ktg[b][:, h, :], in_=pA[k][:]),
                         reads=["pA%d" % k], writes=["ktg%d" % b])
                    P.op("dve", lambda e, k=k, h=h: e.tensor_reduce(
                        out=kmean[:, h, 2 * g:2 * g + 2], in_=pA[k][:].rearrange("p (b t) -> p b t", b=2),
                        axis=AX.X, op=ALU.add), reads=["pA%d" % k], writes=["kmean"])
                for t in range(4):
                    for half in range(2):
                        k = nextpA()
                        for c in range(NCH):
                            P.op("pe", lambda e, c=c, k=k, t=t, half=half: e.matmul(
                                pA[k][:], lhsT=uT[b][:, c, t * 128:(t + 1) * 128],
                                rhs=wv[:, c, half * 512:(half + 1) * 512], start=(c == 0), stop=(c == NCH - 1)),
                                reads=["wv", "uT%d" % b], writes=["pA%d" % k])
                        eng = "act" if half == 0 else "dve"
                        if eng == "act":
                            P.op("act", lambda e, k=k, t=t, half=half: e.copy(
                                out=vg[b][:, half * 4:half * 4 + 4, t, 0:128],
                                in_=pA[k][:].rearrange("p (h d) -> p h d", h=4)),
                                reads=["pA%d" % k], writes=["vg%d" % b])
                        else:
                            P.op("dve", lambda e, k=k, t=t, half=half: e.tensor_copy(
                                out=vg[b][:, half * 4:half * 4 + 4, t, 0:128],
                                in_=pA[k][:].rearrange("p (h d) -> p h d", h=4)),
                                reads=["pA%d" % k], writes=["vg%d" % b])
                P.dma("pool", lambda e, g=g, b=b: e.dma_start(
                    out=kt_scr[:, :, g * 512:(g + 1) * 512].rearrange("h p t -> p h t"), in_=ktg[b][:]),
                    reads=["ktg%d" % b], writes=["kt_scr%d" % g])
                P.dma("pool", lambda e, g=g, b=b: e.dma_start(
                    out=v_scr[:, :, g * 4:(g + 1) * 4, :].rearrange("h p t v -> p h t v"), in_=vg[b][:]),
                    reads=["vg%d" % b], writes=["v_scr%d" % g])
            P.op("dve", lambda e: e.tensor_scalar(out=kmb[:], in0=kmean[:], scalar1=1.0 / 256.0, scalar2=None,
                                                 op0=ALU.mult), reads=["kmean"], writes=["kmb"])
        P.barrier()
        if stage == 1:
            dbg_km = nc.dram_tensor("dbg_km", [128, HEADS, NBLK], F32, kind="ExternalOutput").ap()
            P.dma("sp", lambda e: e.dma_start(out=dbg_km, in_=kmean[:]))
            P.barrier()
            P.finish([])
            return nc

        SCALE = float(HD) ** -0.5
        SLOPES = [2.0 ** (-8.0 * (i + 1) / HEADS) for i in range(HEADS)]

        with contextlib.ExitStack() as s2:
            wst = [sb(s2, "wst%d" % i, [128, NCH, 512], BF16) for i in range(2)]
            uTo = sb(s2, "uTo", [128, NCH, 512], BF16)
            qT = sb(s2, "qT", [128, HEADS, 512], BF16)
            mcT = sb(s2, "mcT", [128, NCH, 512], BF16)
            ws_i = [0]

            def stream_w(src_ap, buf=None):
                if buf is None:
                    i = ws_i[0] % 2
                    ws_i[0] += 1
                else:
                    i = buf
                C, N = src_ap.shape[1], src_ap.shape[2]
                dst = wst[i][:].rearrange("p c n -> p (c n)").rearrange("p (c n) -> p c n", c=C)
                load_w(dst, src_ap, "wst%d" % i)
                return dst, "wst%d" % i

            def lin_fm(wt, wname, col0, src, sname, K, N=512):
                k = nextpA()
                for c in range(K):
                    P.op("pe", lambda e, c=c: e.matmul(pA[k][:, 0:N], lhsT=wt[:, c, col0:col0 + 128],
                                                       rhs=src[:, c, 0:N], start=(c == 0), stop=(c == K - 1)),
                         reads=[wname, sname], writes=["pA%d" % k])
                return k

            def glu(src, sname, N, zdst):
                for i2 in range(2):
                    wv_, nv = stream_w(w_in_v[:, :, C_VAL + i2 * 512:C_VAL + (i2 + 1) * 512])
                    wg_, ng = stream_w(w_in_v[:, :, C_GATE + i2 * 512:C_GATE + (i2 + 1) * 512])
                    for c4 in range(4):
                        cc = i2 * 4 + c4
                        kv = lin_fm(wv_, nv, c4 * 128, src, sname, NCH, N)
                        kg = lin_fm(wg_, ng, c4 * 128, src, sname, NCH, N)
                        P.op("act", lambda e, kg=kg: e.activation(out=sgt[:, 0:N], in_=pA[kg][:, 0:N], func=AF.Sigmoid),
                             reads=["pA%d" % kg], writes=["sgt"])
                        P.op("dve", lambda e, kv=kv, cc=cc: e.tensor_tensor(out=zdst(cc), in0=pA[kv][:, 0:N],
                                                                           in1=sgt[:, 0:N], op=ALU.mult),
                             reads=["pA%d" % kv, "sgt"], writes=["z"])

            for gi in range(4):
                g = 12 + gi
                row0 = g * 512
                for t in range(4):
                    norm_tile(xw[row0 + t * 128:row0 + (t + 1) * 128, :], gA, "gA", uTo, "uTo", t * 128)
                for i2 in range(2):
                    wq_, nq = stream_w(w_in_v[:, :, C_Q + i2 * 512:C_Q + (i2 + 1) * 512])
                    for c4 in range(4):
                        k = lin_fm(wq_, nq, c4 * 128, uTo, "uTo", NCH)
                        P.op("act", lambda e, k=k, h=i2 * 4 + c4: e.copy(out=qT[:, h, :], in_=pA[k][:]),
                             reads=["pA%d" % k], writes=["qT"])
                with contextlib.ExitStack() as sA:
                    z = sb(sA, "z", [128, 8, 544], F32)
                    zc = sb(sA, "zc", [128, 8, 512], F32)
                    sgt = sb(sA, "sgt", [128, 512], F32)
                    sqb = sb(sA, "sqb", [128, 512], F32)
                    mean = sb(sA, "mean", [128, 512], F32)
                    rstd = sb(sA, "rstd", [128, 512], F32)
                    tmpc = [sb(sA, "tmpc%d" % i, [128, 512], F32) for i in range(2)]
                    zact = sb(sA, "zact", [128, 8, 512], BF16)
                    if gi == 0:
                        zh = sb(sA, "zh", [128, 8, 128], F32)
                        uTh = sb(sA, "uTh", [128, NCH, 128], BF16)
                        norm_tile(xw[row0 - 128:row0, :], gA, "gA", uTh, "uTh", 0)
                        glu(uTh, "uTh", 128, lambda cc: zh[:, cc, :])
                        P.op("dve", lambda e: e.tensor_copy(out=zhalo[:], in_=zh[:, :, 96:128]),
                             reads=["z"], writes=["zhalo"])
                    P.op("dve", lambda e: e.tensor_copy(out=z[:, :, 0:32], in_=zhalo[:]),
                         reads=["zhalo"], writes=["z"])
                    glu(uTo, "uTo", 512, lambda cc: z[:, cc, 32:544])
                    P.op("dve", lambda e: e.tensor_copy(out=zhalo[:], in_=z[:, :, 512:544]),
                         reads=["z"], writes=["zhalo"])
                    tmpp = sb(sA, "tmpp", [128, 512], F32)
                    for cc in range(8):
                        en = "dve"
                        rn = "zc%d" % cc
                        P.op(en, lambda e, cc=cc: e.tensor_scalar(out=zc[:, cc, :], in0=z[:, cc, 2:514],
                                                                 scalar1=dww[:, cc, 0:1], scalar2=chv[:, cc, 0:1],
                                                                 op0=ALU.mult, op1=ALU.add),
                             reads=["z", "dww", "chv"], writes=[rn])
                        for kk in range(1, TAPS):
                            if en == "dve":
                                P.op(en, lambda e, cc=cc, kk=kk: e.scalar_tensor_tensor(
                                    out=zc[:, cc, :], in0=z[:, cc, 2 + kk:514 + kk], scalar=dww[:, cc, kk:kk + 1],
                                    in1=zc[:, cc, :], op0=ALU.mult, op1=ALU.add),
                                    reads=["z", rn], writes=[rn])
                            else:
                                P.op(en, lambda e, cc=cc, kk=kk: e.tensor_scalar(
                                    out=tmpp[:], in0=z[:, cc, 2 + kk:514 + kk], scalar1=dww[:, cc, kk:kk + 1],
                                    scalar2=None, op0=ALU.mult), reads=["z"], writes=["tmpp"])
                                P.op(en, lambda e, cc=cc: e.tensor_tensor(out=zc[:, cc, :], in0=zc[:, cc, :], in1=tmpp[:],
                                                                          op=ALU.add), reads=["tmpp", rn], writes=[rn])
                    k1 = nextpA()
                    for cc in range(8):
                        P.op("pe", lambda e, cc=cc: e.matmul(pA[k1][:], lhsT=ones_f[:], rhs=zc[:, cc, :],
                                                             start=(cc == 0), stop=(cc == 7)),
                             reads=["ones_f", "zc%d" % cc], writes=["pA%d" % k1])
                    k2 = nextpA()
                    for cc in range(8):
                        tb = tmpc[cc % 2]
                        P.op("act", lambda e, cc=cc, tb=tb: e.activation(out=tb[:], in_=zc[:, cc, :], func=AF.Square),
                             reads=["zc%d" % cc], writes=["tmpc%d" % (cc % 2)])
                        P.op("pe", lambda e, cc=cc, tb=tb: e.matmul(pA[k2][:], lhsT=ones_f[:], rhs=tb[:],
                                                                    start=(cc == 0), stop=(cc == 7)),
                             reads=["ones_f", "tmpc%d" % (cc % 2)], writes=["pA%d" % k2])
                    P.op("dve", lambda e: e.tensor_scalar(out=mean[:], in0=pA[k1][:], scalar1=1.0 / CC, scalar2=None,
                                                         op0=ALU.mult), reads=["pA%d" % k1], writes=["mean"])
                    P.op("dve", lambda e: e.tensor_tensor(out=sqb[:], in0=mean[:], in1=mean[:], op=ALU.mult),
                         reads=["mean"], writes=["sqb"])
                    P.op("dve", lambda e: e.scalar_tensor_tensor(out=rstd[:], in0=pA[k2][:], scalar=1.0 / CC, in1=sqb[:],
                                                                op0=ALU.mult, op1=ALU.subtract),
                         reads=["pA%d" % k2, "sqb"], writes=["rstd"])
                    P.op("dve", lambda e: e.tensor_scalar(out=rstd[:], in0=rstd[:], scalar1=EPS, scalar2=None,
                                                         op0=ALU.add), reads=["rstd"], writes=["rstd"])
                    P.op("act", lambda e: e.sqrt(out=rstd[:], in_=rstd[:]), reads=["rstd"], writes=["rstd"])
                    P.op("dve", lambda e: e.reciprocal(out=rstd[:], in_=rstd[:]), reads=["rstd"], writes=["rstd"])
                    for cc in range(8):
                        tb = tmpc[cc % 2]
                        tn = "tmpc%d" % (cc % 2)
                        P.op("dve", lambda e, cc=cc, tb=tb: e.tensor_tensor(out=tb[:], in0=zc[:, cc, :], in1=mean[:],
                                                                            op=ALU.subtract),
                             reads=["zc%d" % cc, "mean"], writes=[tn])
                        P.op("dve", lambda e, tb=tb: e.tensor_tensor(out=tb[:], in0=tb[:], in1=rstd[:], op=ALU.mult),
                             reads=[tn, "rstd"], writes=[tn])
                        P.op("act", lambda e, cc=cc, tb=tb: e.activation(out=zact[:, cc, :], in_=tb[:], func=AF.Silu,
                                                                         bias=chv[:, cc, 2:3], scale=chv[:, cc, 1:2]),
                             reads=[tn, "chv"], writes=["zact"])
                    w_co_v = w_co.rearrange("(c p) n -> p c n", p=128)
                    for half in range(2):
                        wco_, nco = stream_w(w_co_v[:, :, half * 1024:(half + 1) * 1024], buf=0)
                        for q2 in range(2):
                            gcol = C_GC + (half * 2 + q2) * 512
                            wgc_, ngc = stream_w(w_in_v[:, :, gcol:gcol + 512], buf=1)
                            for d4 in range(4):
                                dc = half * 8 + q2 * 4 + d4
                                ky = lin_fm(wco_, nco, (q2 * 4 + d4) * 128, zact, "zact", 8)
                                kg = lin_fm(wgc_, ngc, d4 * 128, uTo, "uTo", NCH)
                                P.op("act", lambda e, kg=kg, dc=dc: e.activation(out=sgt[:], in_=pA[kg][:], func=AF.Sigmoid,
                                                                                 bias=gbt[:, dc:dc + 1]),
                                     reads=["pA%d" % kg, "gbt"], writes=["sgt"])
                                P.op("dve", lambda e, ky=ky, dc=dc: e.tensor_tensor(out=mcT[:, dc, :], in0=pA[ky][:],
                                                                                   in1=sgt[:], op=ALU.mult),
                                     reads=["pA%d" % ky, "sgt"], writes=["mcT"])
                    if stage == 1.3:
                        dbg_mc = nc.dram_tensor("dbg_mc", [128, NCH, 512], BF16, kind="ExternalOutput").ap()
                        dbg_qt = nc.dram_tensor("dbg_qt", [128, HEADS, 512], BF16, kind="ExternalOutput").ap()
                        P.dma("sp", lambda e: e.dma_start(out=dbg_mc, in_=mcT[:]), reads=["mcT"])
                        P.dma("sp", lambda e: e.dma_start(out=dbg_qt, in_=qT[:]), reads=["qT"])
                        P.barrier()
                        P.finish([])
                        return nc
                    P.barrier()
                with contextlib.ExitStack() as sB:
                    KT = sb(sB, "KT", [128, SEQ], BF16)
                    VH = sb(sB, "VH", [128, 64, VP], BF16)
                    PT = [sb(sB, "PT%d" % i, [128, 512], BF16) for i in range(4)]
                    acc = sb(sB, "acc", [128, 4, 129], F32)
                    rec = sb(sB, "rec", [128, 4], F32)
                    gs = sb(sB, "gs", [128, 4, NBLK], F32)
                    top8 = sb(sB, "top8", [128, 4, 8], F32)
                    fac = sb(sB, "fac", [128, 4, NBLK], F32)
                    wsel = sb(sB, "wsel", [128, 4, NBLK], F32)
                    atok = sb(sB, "atok", [128, 4, CC], BF16)
                    attnT = sb(sB, "attnT", [128, HEADS, 512], BF16)
                    pt_i = [0]
                    po_i = [0]
                    nkeys = (g + 1) * 512

                    def s_exp(h, ktile, q0, q1, bcol):
                        k = nextpA()
                        P.op("pe", lambda e: e.matmul(pA[k][:, q0:q1], lhsT=KT[:, ktile * 128:(ktile + 1) * 128],
                                                      rhs=qT[:, h, q0:q1], start=True, stop=True),
                             reads=["KT", "qT"], writes=["pA%d" % k])
                        pi = pt_i[0] % 4
                        pt_i[0] += 1
                        P.op("act", lambda e: e.activation(out=PT[pi][:, q0:q1], in_=pA[k][:, q0:q1], func=AF.Exp,
                                                           bias=bbt[:, h, bcol:bcol + 1], scale=SCALE),
                             reads=["pA%d" % k, "bbt"], writes=["PT%d" % pi])
                        return pi

                    def pv_acc(pairs, qt, wap, wname):
                        r = po_i[0] % 2
                        po_i[0] += 1
                        oap = pO[:, r, 0:129]
                        for j, (pi, ktile) in enumerate(pairs):
                            P.op("pe", lambda e, j=j, pi=pi, ktile=ktile: e.matmul(
                                oap, lhsT=PT[pi][:, qt * 128:(qt + 1) * 128], rhs=VH[:, ktile, 0:129],
                                start=(j == 0), stop=(j == len(pairs) - 1)),
                                reads=["PT%d" % pi, "VH"], writes=["pO%d" % r])
                        P.op("dve", lambda e: e.scalar_tensor_tensor(out=acc[:, qt, :], in0=oap, scalar=wap,
                                                                    in1=acc[:, qt, :], op0=ALU.mult, op1=ALU.add),
                             reads=["pO%d" % r, wname, "acc"], writes=["acc"])

                    for h in range(HEADS):
                        n_first = sum(1 for n in range(2 * g) if SLOPES[h] * (g * 512 - n * 256 - 255) >= 104.0)
                        k0 = n_first * 256
                        P.dma("sp", lambda e, h=h: e.dma_start(out=KT[:, k0:nkeys], in_=kt_scr[h, :, k0:nkeys]),
                              reads=["kt_scr"], writes=["KT"])
                        P.dma("sp", lambda e, h=h: e.dma_start(out=VH[:, k0 // 128:nkeys // 128, :],
                                                              in_=v_scr[h, :, k0 // 128:nkeys // 128, :]),
                              reads=["v_scr"], writes=["VH"])
                        P.op("pool", lambda e: e.memset(acc[:], 0.0), writes=["acc"])
                        kq = nextpA()
                        for qt in range(4):
                            P.op("pe", lambda e, qt=qt: e.matmul(pA[kq][:, qt * NBLK:(qt + 1) * NBLK],
                                                                 lhsT=qT[:, h, qt * 128:(qt + 1) * 128], rhs=kmb[:, h, :],
                                                                 start=True, stop=True),
                                 reads=["qT", "kmb"], writes=["pA%d" % kq])
                        P.op("dve", lambda e: e.tensor_tensor(out=gs[:], in0=pA[kq][:, 0:4 * NBLK].rearrange("p (a b) -> p a b", a=4),
                                                              in1=gmt[:, gi * 4:gi * 4 + 4, :], op=ALU.add),
                             reads=["pA%d" % kq, "gmt"], writes=["gs"])
                        for qt in range(4):
                            P.op("dve", lambda e, qt=qt: e.max(out=top8[:, qt, :], in_=gs[:, qt, :]),
                                 reads=["gs"], writes=["top8"])
                        P.op("dve", lambda e: e.tensor_scalar(out=top8[:, :, 2:3], in0=top8[:, :, 2:3], scalar1=-1e29,
                                                             scalar2=None, op0=ALU.max), reads=["top8"], writes=["top8"])
                        P.op("act", lambda e, h=h: e.activation(out=fac[:], in_=distt[:, gi * 4:gi * 4 + 4, :], func=AF.Exp,
                                                                scale=-SLOPES[h]), reads=["distt"], writes=["fac"])
                        for qt in range(4):
                            P.op("dve", lambda e, qt=qt: e.scalar_tensor_tensor(
                                out=wsel[:, qt, :], in0=gs[:, qt, :], scalar=top8[:, qt, 2:3], in1=fac[:, qt, :],
                                op0=ALU.is_ge, op1=ALU.mult), reads=["gs", "top8", "fac"], writes=["wsel"])
                        for n in range(n_first, 2 * g):
                            pis = [s_exp(h, 2 * n + kt, 0, 512, kt) for kt in range(2)]
                            for qt in range(4):
                                pv_acc([(pis[0], 2 * n), (pis[1], 2 * n + 1)], qt, wsel[:, qt, n:n + 1], "wsel")
                        n = 2 * g
                        pis = [s_exp(h, 2 * n + kt, 256, 512, kt) for kt in range(2)]
                        for qt in (2, 3):
                            pv_acc([(pis[0], 2 * n), (pis[1], 2 * n + 1)], qt, wsel[:, qt, n:n + 1], "wsel")
                        for (qt, ktile, diag) in [(0, 4 * g, True), (1, 4 * g, False), (1, 4 * g + 1, True),
                                                  (2, 4 * g + 2, True), (3, 4 * g + 2, False), (3, 4 * g + 3, True)]:
                            pi = s_exp(h, ktile, qt * 128, (qt + 1) * 128, 2 if diag else 1)
                            if diag:
                                P.op("pool", lambda e, pi=pi, qt=qt: e.tensor_tensor(
                                    out=PT[pi][:, qt * 128:(qt + 1) * 128], in0=PT[pi][:, qt * 128:(qt + 1) * 128],
                                    in1=tri[:], op=ALU.mult), reads=["PT%d" % pi, "tri"], writes=["PT%d" % pi])
                            pv_acc([(pi, ktile)], qt, fdt[:, h, 0:1] if diag else fdt[:, h, 1:2], "fdt")
                        P.op("dve", lambda e: e.reciprocal(out=rec[:], in_=acc[:, :, 128]), reads=["acc"], writes=["rec"])
                        for qt in range(4):
                            P.op("dve", lambda e, qt=qt, h=h: e.tensor_scalar(
                                out=atok[:, qt, h * 128:(h + 1) * 128], in0=acc[:, qt, 0:128], scalar1=rec[:, qt:qt + 1],
                                scalar2=None, op0=ALU.mult), reads=["acc", "rec"], writes=["atok"])
                    for qt in range(4):
                        bk = qt % 2
                        for h in range(HEADS):
                            P.op("pe", lambda e, qt=qt, h=h, bk=bk: e.transpose(
                                out=pT[:, bk, h, :], in_=atok[:, qt, h * 128:(h + 1) * 128], identity=idb[:]),
                                reads=["atok", "idb"], writes=["pT%d" % bk])
                        P.op("act", lambda e, qt=qt, bk=bk: e.copy(out=attnT[:, :, qt * 128:(qt + 1) * 128], in_=pT[:, bk, :, :]),
                             reads=["pT%d" % bk], writes=["attnT"])
                    sgt2 = sb(sB, "sgt2", [128, 512], F32)
                    tmpm = sb(sB, "tmpm", [128, 512], F32)
                    w_ao_v = w_ao.rearrange("(c p) n -> p c n", p=128)
                    for half in range(2):
                        wao_, nao = stream_w(w_ao_v[:, :, half * 1024:(half + 1) * 1024], buf=0)
                        for q2 in range(2):
                            gcol = C_GA + (half * 2 + q2) * 512
                            wga_, nga = stream_w(w_in_v[:, :, gcol:gcol + 512], buf=1)
                            for d4 in range(4):
                                dc = half * 8 + q2 * 4 + d4
                                ky = lin_fm(wao_, nao, (q2 * 4 + d4) * 128, attnT, "attnT", 8)
                                kg = lin_fm(wga_, nga, d4 * 128, uTo, "uTo", NCH)
                                P.op("act", lambda e, kg=kg, dc=dc: e.activation(out=sgt2[:], in_=pA[kg][:], func=AF.Sigmoid,
                                                                                 bias=gbt[:, 16 + dc:17 + dc]),
                                     reads=["pA%d" % kg, "gbt"], writes=["sgt2"])
                                P.op("dve", lambda e, ky=ky: e.tensor_tensor(out=tmpm[:], in0=pA[ky][:], in1=sgt2[:],
                                                                            op=ALU.mult),
                                     reads=["pA%d" % ky, "sgt2"], writes=["tmpm"])
                                P.op("dve", lambda e, dc=dc: e.tensor_tensor(out=mcT[:, dc, :], in0=tmpm[:], in1=mcT[:, dc, :],
                                                                            op=ALU.add),
                                     reads=["tmpm", "mcT"], writes=["mcT"])
                    if stage == 1.6:
                        dbg_mc = nc.dram_tensor("dbg_mc", [128, NCH, 512], BF16, kind="ExternalOutput").ap()
                        dbg_at = nc.dram_tensor("dbg_at", [128, HEADS, 512], BF16, kind="ExternalOutput").ap()
                        P.dma("sp", lambda e: e.dma_start(out=dbg_mc, in_=mcT[:]), reads=["mcT"])
                        P.dma("sp", lambda e: e.dma_start(out=dbg_at, in_=attnT[:]), reads=["attnT"])
                        P.barrier()
                        P.finish([])
                        return nc
                    P.barrier()
                with contextlib.ExitStack() as sC:
                    hj = [sb(sC, "hj%d" % i, [128, 4, 512], F32) for i in range(2)]
                    hn32 = sb(sC, "hn32", [128, D], F32)
                    hnT = sb(sC, "hnT", [128, NCH, 128], F32)
                    lg = sb(sC, "lg", [128, 72], F32)
                    sm = sb(sC, "sm", [128, 16], F32)
                    oh = sb(sC, "oh", [128, 8], F32)
                    el = sb(sC, "el", [128, 8], F32)
                    t64 = sb(sC, "t64", [128, 8, 8], F32)
                    e8 = sb(sC, "e8", [128, 8], F32)
                    m12 = sb(sC, "m12", [128, 2, 8], F32)
                    w_out_v = w_out.rearrange("(c p) n -> p c n", p=128)
                    for j in range(4):
                        wo_, no = stream_w(w_out_v[:, :, j * 512:(j + 1) * 512])
                        hb = hj[j % 2]
                        hbn = "hj%d" % (j % 2)
                        P.dma("sp", lambda e, j=j, hb=hb: e.dma_start(
                            out=hb[:], in_=xw[row0:row0 + 512, j * 512:(j + 1) * 512].rearrange("(t p) c -> p t c", p=128)),
                            writes=[hbn])
                        for t in range(4):
                            k = nextpA()
                            for c in range(NCH):
                                P.op("pe", lambda e, c=c, k=k, t=t: e.matmul(pA[k][:], lhsT=mcT[:, c, t * 128:(t + 1) * 128],
                                                                            rhs=wo_[:, c, :], start=(c == 0), stop=(c == NCH - 1)),
                                     reads=["mcT", no], writes=["pA%d" % k])
                            P.op("dve", lambda e, k=k, t=t, hb=hb: e.tensor_tensor(out=hb[:, t, :], in0=pA[k][:], in1=hb[:, t, :],
                                                                                  op=ALU.add),
                                 reads=["pA%d" % k, hbn], writes=[hbn])
                        P.dma("sp", lambda e, j=j, hb=hb: e.dma_start(
                            out=h_scr[gi * 512:(gi + 1) * 512, j * 512:(j + 1) * 512].rearrange("(t p) c -> p t c", p=128),
                            in_=hb[:]), reads=[hbn], writes=["h_scr"])
                    for t in range(4):
                        ti = gi * 4 + t
                        i = nt_i[0] % 2
                        nt_i[0] += 1
                        r_ = gi * 512 + t * 128
                        P.dma("sp", lambda e, i=i, r_=r_: e.dma_start(out=xt[i][:], in_=h_scr[r_:r_ + 128, :]),
                              reads=["h_scr"], writes=["xt%d" % i])
                        rms_scale(xt[i], "xt%d" % i, i, gB, "gB", hn32, "hn32")
                        P.op("act", lambda e, i=i: e.copy(out=xs[i][:], in_=hn32[:]), reads=["hn32"], writes=["xs%d" % i])
                        P.dma("sp", lambda e, i=i, r_=r_: e.dma_start(out=hn_scr[r_:r_ + 128, :], in_=xs[i][:]),
                              reads=["xs%d" % i], writes=["hn_scr"])
                        for c4 in range(4):
                            k = nextpA()
                            for c1 in range(4):
                                c = c4 * 4 + c1
                                P.op("pe", lambda e, c=c, c1=c1, k=k: e.transpose(out=pA[k][:, c1 * 128:(c1 + 1) * 128],
                                                                                  in_=hn32[:, c * 128:(c + 1) * 128], identity=idf[:]),
                                     reads=["hn32", "idf"], writes=["pA%d" % k])
                            P.op("dve" if c4 % 2 else "act",
                                 (lambda e, k=k, c4=c4: e.tensor_copy(out=hnT[:, c4 * 4:c4 * 4 + 4, :],
                                                                      in_=pA[k][:].rearrange("p (a b) -> p a b", a=4))) if c4 % 2 else
                                 (lambda e, k=k, c4=c4: e.copy(out=hnT[:, c4 * 4:c4 * 4 + 4, :],
                                                               in_=pA[k][:].rearrange("p (a b) -> p a b", a=4))),
                                 reads=["pA%d" % k], writes=["hnT"])
                        k = nextpA()
                        for c in range(NCH):
                            P.op("pe", lambda e, c=c, k=k: e.matmul(pA[k][:, 0:72], lhsT=hnT[:, c, :], rhs=wr32[:, c, :],
                                                                    start=(c == 0), stop=(c == NCH - 1)),
                                 reads=["hnT", "wr32"], writes=["pA%d" % k])
                        P.op("dve", lambda e, k=k: e.tensor_tensor(out=lg[:], in0=pA[k][:, 0:72], in1=brb[:], op=ALU.add),
                             reads=["pA%d" % k, "brb"], writes=["lg"])
                        P.op("dve", lambda e: e.max(out=e8[:], in_=lg[:, 0:8]), reads=["lg"], writes=["e8"])
                        P.op("dve", lambda e: e.tensor_scalar(out=oh[:], in0=lg[:, 0:8], scalar1=e8[:, 0:1], scalar2=None,
                                                             op0=ALU.is_ge), reads=["lg", "e8"], writes=["oh"])
                        P.op("dve", lambda e: e.tensor_scalar(out=sm[:, 0:1], in0=e8[:, 0:1], scalar1=-1.0, scalar2=None,
                                                             op0=ALU.mult), reads=["e8"], writes=["sm"])
                        P.op("act", lambda e: e.activation(out=sm[:, 8:16], in_=lg[:, 0:8], func=AF.Exp, bias=sm[:, 0:1],
                                                           accum_out=sm[:, 1:2]), reads=["lg", "sm"], writes=["sm"])
                        P.op("dve", lambda e: e.tensor_tensor(out=t64[:], in0=lg[:, 8:72].rearrange("p (g e) -> p g e", g=8),
                                                              in1=oh[:].unsqueeze(2).to_broadcast([128, 8, 8]), op=ALU.mult),
                             reads=["lg", "oh"], writes=["t64"])
                        P.op("dve", lambda e: e.tensor_reduce(out=el[:], in_=t64[:].rearrange("p g e -> p e g"), axis=AX.X,
                                                              op=ALU.add), reads=["t64"], writes=["el"])
                        P.op("dve", lambda e: e.max(out=e8[:], in_=el[:]), reads=["el"], writes=["e8"])
                        P.op("dve", lambda e: e.tensor_scalar(out=m12[:, 0, :], in0=el[:], scalar1=e8[:, 0:1], scalar2=None,
                                                             op0=ALU.is_equal), reads=["el", "e8"], writes=["m12"])
                        P.op("dve", lambda e: e.tensor_scalar(out=m12[:, 1, :], in0=el[:], scalar1=e8[:, 1:2], scalar2=None,
                                                             op0=ALU.is_equal), reads=["el", "e8"], writes=["m12"])
                        P.op("dve", lambda e: e.tensor_tensor(out=sm[:, 2:3], in0=e8[:, 1:2], in1=e8[:, 0:1], op=ALU.subtract),
                             reads=["e8"], writes=["sm"])
                        P.op("act", lambda e: e.activation(out=sm[:, 3:4], in_=sm[:, 2:3], func=AF.Exp),
                             reads=["sm"], writes=["sm"])
                        P.op("dve", lambda e: e.tensor_scalar(out=sm[:, 3:4], in0=sm[:, 3:4], scalar1=1.0, scalar2=None,
                                                             op0=ALU.add), reads=["sm"], writes=["sm"])
                        P.op("dve", lambda e: e.tensor_tensor(out=sm[:, 4:5], in0=sm[:, 3:4], in1=sm[:, 1:2], op=ALU.mult),
                             reads=["sm"], writes=["sm"])
                        P.op("dve", lambda e, ti=ti: e.reciprocal(out=comb[:, ti, 0:1], in_=sm[:, 4:5]),
                             reads=["sm"], writes=["comb"])
                        P.op("dve", lambda e: e.reciprocal(out=sm[:, 5:6], in_=sm[:, 1:2]), reads=["sm"], writes=["sm"])
                        P.op("dve", lambda e, ti=ti: e.tensor_tensor(out=comb[:, ti, 1:2], in0=sm[:, 5:6], in1=comb[:, ti, 0:1],
                                                                    op=ALU.subtract), reads=["sm", "comb"], writes=["comb"])
                        for kx, Mk in ((0, M1all), (1, M2all)):
                            P.op("dve", lambda e, kx=kx, Mk=Mk, ti=ti: e.tensor_tensor(
                                out=Mk[:, ti, :].rearrange("p (g e) -> p g e", g=8),
                                in0=oh[:].unsqueeze(2).to_broadcast([128, 8, 8]),
                                in1=m12[:, kx, :].unsqueeze(1).to_broadcast([128, 8, 8]), op=ALU.mult),
                                reads=["oh", "m12"], writes=["M%d" % kx])
                        P.op("dve", lambda e, ti=ti: e.tensor_tensor(out=Mall[:, ti, :], in0=M1all[:, ti, :], in1=M2all[:, ti, :],
                                                                    op=ALU.add), reads=["M0", "M1"], writes=["Mall"])
                    P.barrier()
        P.barrier()
        if stage == 2:
            dbg_cb = nc.dram_tensor("dbg_cb", [128, 16, 2], F32, kind="ExternalOutput").ap()
            dbg_m = nc.dram_tensor("dbg_m", [128, 16, NEXP], BF16, kind="ExternalOutput").ap()
            P.dma("sp", lambda e: e.dma_start(out=dbg_cb, in_=comb[:]))
            P.dma("sp", lambda e: e.dma_start(out=dbg_m, in_=Mall[:]))
            P.barrier()
            P.finish([])
            return nc
        IOA = bass.IndirectOffsetOnAxis
        with contextlib.ExitStack() as s3:
            cntf = sb(s3, "cntf", [128, NEXP], F32)
            sa = sb(s3, "sa", [128, NEXP], F32)
            sbb = sb(s3, "sbb", [128, NEXP], F32)
            padded = sb(s3, "padded", [128, NEXP], F32)
            pstart = sb(s3, "pstart", [128, NEXP], F32)
            posf = sb(s3, "posf", [128, NEXP], F32)
            tmp64 = sb(s3, "tmp64", [128, NEXP], F32)
            destf = sb(s3, "destf", [128, 16, 2], F32)
            bef = sb(s3, "bef", [128, MOE_BLOCKS], F32)
            idxw = sb(s3, "idxw", [128, MOE_BLOCKS, 4], I32)
            idx4f = sb(s3, "idx4f", [128, MOE_BLOCKS, 4], F32)
            bigp = sb(s3, "bigp", [128, 1], F32)
            unus = sb(s3, "unus", [128, MOE_BLOCKS], F32)
            P.op("pool", lambda e: e.memset(junk[:], 0.0), writes=["junk"])
            for b1 in range(MOE_BLOCKS):
                P.dma("sp", lambda e, b1=b1: e.dma_start(out=xs_scr[b1 * 128:(b1 + 1) * 128, :], in_=junk[:]),
                      reads=["junk"], writes=["xs_scr"])
            kc = nextpA()
            for j in range(16):
                P.op("pe", lambda e, j=j: e.matmul(pA[kc][:, 0:NEXP], lhsT=ones_b[:], rhs=Mall[:, j, :],
                                                   start=(j == 0), stop=(j == 15)),
                     reads=["ones_b", "Mall"], writes=["pA%d" % kc])
            P.op("dve", lambda e: e.tensor_copy(out=cntf[:], in_=pA[kc][:, 0:NEXP]), reads=["pA%d" % kc], writes=["cntf"])
            with contextlib.ExitStack() as s3b:
                cmpb = sb(s3b, "cmpb", [128, NEXP, 16], F32)
                P.op("dve", lambda e: e.tensor_tensor(
                    out=cmpb[:], in0=cntf[:].unsqueeze(2).to_broadcast([128, NEXP, 16]),
                    in1=bstt[:, 0:16].unsqueeze(1).to_broadcast([128, NEXP, 16]), op=ALU.is_gt),
                    reads=["cntf", "bstt"], writes=["cmpb"])
                P.op("dve", lambda e: e.tensor_reduce(out=padded[:], in_=cmpb[:], axis=AX.X, op=ALU.add),
                     reads=["cmpb"], writes=["padded"])
                P.op("dve", lambda e: e.tensor_scalar(out=padded[:], in0=padded[:], scalar1=128.0, scalar2=None,
                                                     op0=ALU.mult), reads=["padded"], writes=["padded"])
                P.barrier()
            P.op("dve", lambda e: e.tensor_copy(out=sa[:], in_=padded[:]), reads=["padded", "sbb"], writes=["sa"])
            cur, oth, cn, on = sa, sbb, "sa", "sbb"
            for sft in (1, 2, 4, 8, 16, 32):
                P.op("dve", lambda e, cur=cur, oth=oth, sft=sft: e.tensor_copy(out=oth[:, 0:sft], in_=cur[:, 0:sft]),
                     reads=[cn], writes=[on])
                P.op("dve", lambda e, cur=cur, oth=oth, sft=sft: e.tensor_tensor(
                    out=oth[:, sft:NEXP], in0=cur[:, sft:NEXP], in1=cur[:, 0:NEXP - sft], op=ALU.add),
                    reads=[cn], writes=[on])
                cur, oth, cn, on = oth, cur, on, cn
            pend, pendn = cur, cn
            P.op("dve", lambda e: e.tensor_tensor(out=pstart[:], in0=pend[:], in1=padded[:], op=ALU.subtract),
                 reads=[pendn, "padded"], writes=["pstart"])
            for ti in range(16):
                kr = nextpA()
                for j in range(ti):
                    P.op("pe", lambda e, j=j: e.matmul(pA[kr][:, 0:NEXP], lhsT=ones_b[:], rhs=Mall[:, j, :],
                                                       start=(j == 0), stop=False),
                         reads=["ones_b", "Mall"], writes=["pA%d" % kr])
                P.op("pe", lambda e, ti=ti: e.matmul(pA[kr][:, 0:NEXP], lhsT=ust[:], rhs=Mall[:, ti, :],
                                                     start=(ti == 0), stop=True),
                     reads=["ust", "Mall"], writes=["pA%d" % kr])
                P.op("dve", lambda e: e.tensor_tensor(out=posf[:], in0=pA[kr][:, 0:NEXP], in1=pstart[:], op=ALU.add),
                     reads=["pA%d" % kr, "pstart"], writes=["posf"])
                for kx, Mk in ((0, M1all), (1, M2all)):
                    P.op("dve", lambda e, Mk=Mk, ti=ti: e.tensor_tensor(out=tmp64[:], in0=posf[:], in1=Mk[:, ti, :], op=ALU.mult),
                         reads=["posf", "M%d" % kx], writes=["tmp64"])
                    P.op("dve", lambda e, ti=ti, kx=kx: e.tensor_reduce(out=destf[:, ti, kx:kx + 1], in_=tmp64[:], axis=AX.X,
                                                                        op=ALU.add), reads=["tmp64"], writes=["destf"])
            P.op("dve", lambda e: e.tensor_copy(out=desti[:], in_=destf[:]), reads=["destf"], writes=["desti"])
            with contextlib.ExitStack() as s3a:
                cmp = sb(s3a, "cmp", [128, MOE_BLOCKS, NEXP], F32)
                P.op("dve", lambda e: e.tensor_tensor(
                    out=cmp[:], in0=pend[:].unsqueeze(1).to_broadcast([128, MOE_BLOCKS, NEXP]),
                    in1=bstt[:].unsqueeze(2).to_broadcast([128, MOE_BLOCKS, NEXP]), op=ALU.is_le),
                    reads=[pendn, "bstt"], writes=["cmp"])
                P.op("dve", lambda e: e.tensor_reduce(out=bef[:], in_=cmp[:], axis=AX.X, op=ALU.add),
                     reads=["cmp"], writes=["bef"])
                P.op("dve", lambda e: e.tensor_scalar(out=bigp[:], in0=pidx[:], scalar1=0.5, scalar2=200000.0,
                                                     op0=ALU.is_gt, op1=ALU.mult), reads=["pidx"], writes=["bigp"])
                P.op("dve", lambda e: e.tensor_scalar(out=unus[:], in0=bef[:], scalar1=float(NEXP) - 0.5, scalar2=bigp[:, 0:1],
                                                     op0=ALU.is_gt, op1=ALU.mult), reads=["bef", "bigp"], writes=["unus"])
                P.op("dve", lambda e: e.tensor_scalar(out=bef[:], in0=bef[:], scalar1=float(NEXP - 1), scalar2=None,
                                                     op0=ALU.min), reads=["bef"], writes=["bef"])
                P.op("dve", lambda e: e.tensor_scalar(out=bef[:], in0=bef[:], scalar1=128.0, scalar2=pidx[:, 0:1],
                                                     op0=ALU.mult, op1=ALU.add), reads=["bef", "pidx"], writes=["bef"])
                for a in range(4):
                    P.op("dve", lambda e: e.tensor_scalar(out=idx4f[:, :, a], in0=bef[:], scalar1=4.0, scalar2=float(a),
                                                         op0=ALU.mult, op1=ALU.add), reads=["bef"], writes=["idx4f"])
                P.op("dve", lambda e: e.tensor_copy(out=idxw[:], in_=idx4f[:]), reads=["idx4f"], writes=["idxw"])
                P.barrier()
            for ti in range(16):
                i = ti % 2
                P.dma("sp", lambda e, ti=ti, i=i: e.dma_start(out=xs[i][:], in_=hn_scr[ti * 128:(ti + 1) * 128, :]),
                      reads=["hn_scr"], writes=["xs%d" % i])
                for kx in range(2):
                    P.dma("pool", lambda e, ti=ti, i=i, kx=kx: e.indirect_dma_start(
                        out=xs_scr, out_offset=IOA(ap=desti[:, ti, kx:kx + 1], axis=0), in_=xs[i][:], in_offset=None),
                        reads=["xs%d" % i, "desti", "xs_scr"], writes=["xs_scr%d" % (ti * 2 + kx)])
            P.barrier()
            if stage == 2.5:
                dbg_di = nc.dram_tensor("dbg_di", [128, 16, 2], I32, kind="ExternalOutput").ap()
                dbg_ix = nc.dram_tensor("dbg_ix", [128, MOE_BLOCKS, 4], I32, kind="ExternalOutput").ap()
                P.dma("sp", lambda e: e.dma_start(out=dbg_di, in_=desti[:]))
                P.dma("sp", lambda e: e.dma_start(out=dbg_ix, in_=idxw[:]))
                P.barrier()
                P.finish([])
                return nc
            wgb = [sb(s3, "wgb%d" % i, [128, 4, 2048], BF16) for i in range(2)]
            wub = [sb(s3, "wub%d" % i, [128, 4, 2048], BF16) for i in range(2)]
            wdb = [sb(s3, "wdb%d" % i, [128, 4, 2048], BF16) for i in range(2)]
            xbb = [sb(s3, "xbb%d" % i, [128, D], BF16) for i in range(2)]
            xTb = [sb(s3, "xTb%d" % i, [128, NCH, 128], BF16) for i in range(2)]
            sgm = sb(s3, "sgm", [128, DEXP], F32)
            hdn = sb(s3, "hdn", [128, DEXP], BF16)
            hTb = sb(s3, "hTb", [128, 4, 128], BF16)
            yb = [sb(s3, "yb%d" % i, [128, D], F32) for i in range(2)]
            weg_v = w_eg.rearrange("e (p a b) f -> (e p a) (b f)", a=4, b=4)
            weu_v = w_eu.rearrange("e (p a b) f -> (e p a) (b f)", a=4, b=4)
            wed_v = w_ed.rearrange("e (p c) f -> (e p c) f", c=4)
            for blk in range(MOE_BLOCKS):
                b = blk % 2
                for (dst, src, nm) in ((wgb[b], weg_v, "wgb%d" % b), (wub[b], weu_v, "wub%d" % b), (wdb[b], wed_v, "wdb%d" % b)):
                    for a in range(4):
                        P.dma("pool", lambda e: e.indirect_dma_start(
                            out=dst[:, a, :], out_offset=None, in_=src, in_offset=IOA(ap=idxw[:, blk, a:a + 1], axis=0)),
                            reads=["idxw"], writes=[nm + "_%d" % a])
                P.dma("sp", lambda e, blk=blk, b=b: e.dma_start(out=xbb[b][:], in_=xs_scr[blk * 128:(blk + 1) * 128, :]),
                      reads=["xs_scr"], writes=["xbb%d" % b])
                xv = xbb[b][:].rearrange("s (p c) -> s c p", c=16)
                for c in range(NCH):
                    P.op("pe", lambda e, c=c, xv=xv: e.transpose(out=pT[:, c // 8, c % 8, :], in_=xv[:, c, :], identity=idb[:]),
                         reads=["xbb%d" % b, "idb"], writes=["pT%d" % (c // 8)])
                P.op("dve", lambda e, b=b: e.tensor_copy(out=xTb[b][:, 0:8, :], in_=pT[:, 0, :, :]),
                     reads=["pT0"], writes=["xTb%d" % b])
                P.op("act", lambda e, b=b: e.copy(out=xTb[b][:, 8:16, :], in_=pT[:, 1, :, :]),
                     reads=["pT1"], writes=["xTb%d" % b])
                kg = nextpA()
                ku = nextpA()
                for (kk_, wt, wn) in ((kg, wgb[b], "wgb%d" % b), (ku, wub[b], "wub%d" % b)):
                    wv2 = wt[:].rearrange("p a (b f) -> p (a b) f", b=4)
                    for c in range(NCH):
                        P.op("pe", lambda e, c=c, kk_=kk_, wv2=wv2, b=b: e.matmul(pA[kk_][:], lhsT=xTb[b][:, c, :], rhs=wv2[:, c, :],
                                                                                start=(c == 0), stop=(c == NCH - 1)),
                             reads=["xTb%d" % b] + [wn + "_%d" % a for a in range(4)], writes=["pA%d" % kk_])
                P.op("act", lambda e, kg=kg: e.activation(out=sgm[:], in_=pA[kg][:], func=AF.Silu),
                     reads=["pA%d" % kg], writes=["sgm"])
                P.op("dve", lambda e, ku=ku: e.tensor_tensor(out=hdn[:], in0=pA[ku][:], in1=sgm[:], op=ALU.mult),
                     reads=["pA%d" % ku, "sgm"], writes=["hdn"])
                hv = hdn[:].rearrange("s (p c) -> s c p", c=4)
                for c in range(4):
                    P.op("pe", lambda e, c=c, hv=hv: e.transpose(out=pT[:, 0, c, :], in_=hv[:, c, :], identity=idb[:]),
                         reads=["hdn", "idb"], writes=["pT0"])
                P.op("dve", lambda e: e.tensor_copy(out=hTb[:], in_=pT[:, 0, 0:4, :]), reads=["pT0"], writes=["hTb"])
                for j in range(4):
                    k = nextpA()
                    for c in range(4):
                        P.op("pe", lambda e, c=c, k=k, j=j, b=b: e.matmul(pA[k][:], lhsT=hTb[:, c, :],
                                                                         rhs=wdb[b][:, c, j * 512:(j + 1) * 512],
                                                                         start=(c == 0), stop=(c == 3)),
                             reads=["hTb"] + ["wdb%d_%d" % (b, a) for a in range(4)], writes=["pA%d" % k])
                    if j % 2 == 0:
                        P.op("act", lambda e, k=k, j=j, b=b: e.copy(out=yb[b][:, j * 512:(j + 1) * 512], in_=pA[k][:]),
                             reads=["pA%d" % k], writes=["yb%d" % b])
                    else:
                        P.op("dve", lambda e, k=k, j=j, b=b: e.tensor_copy(out=yb[b][:, j * 512:(j + 1) * 512], in_=pA[k][:]),
                             reads=["pA%d" % k], writes=["yb%d" % b])
                P.dma("sp", lambda e, blk=blk, b=b: e.dma_start(out=ys_scr[blk * 128:(blk + 1) * 128, :], in_=yb[b][:]),
                      reads=["yb%d" % b], writes=["ys_scr"])
            P.barrier()
            if stage == 3:
                dbg_di = nc.dram_tensor("dbg_di", [128, 16, 2], I32, kind="ExternalOutput").ap()
                dbg_ix = nc.dram_tensor("dbg_ix", [128, MOE_BLOCKS, 4], I32, kind="ExternalOutput").ap()
                P.dma("sp", lambda e: e.dma_start(out=dbg_di, in_=desti[:]))
                P.dma("sp", lambda e: e.dma_start(out=dbg_ix, in_=idxw[:]))
                P.barrier()
                P.finish([])
                return nc
        finals = []
        with contextlib.ExitStack() as s4:
            yg = [[sb(s4, "yg%d_%d" % (i, kx), [128, D], F32) for kx in range(2)] for i in range(2)]
            of = [sb(s4, "of%d" % i, [128, D], F32) for i in range(2)]
            P.dma("sp", lambda e: e.dma_start(out=gA[:], in_=gf_d.partition_broadcast(128)), writes=["gA"])
            for ti in range(16):
                i = ti % 2
                P.dma("sp", lambda e, ti=ti, i=i: e.dma_start(out=xt[i][:], in_=h_scr[ti * 128:(ti + 1) * 128, :]),
                      reads=["h_scr"], writes=["xt%d" % i])
                for kx in range(2):
                    P.dma("pool", lambda e, ti=ti, i=i, kx=kx: e.indirect_dma_start(
                        out=yg[i][kx][:], out_offset=None, in_=ys_scr, in_offset=IOA(ap=desti[:, ti, kx:kx + 1], axis=0)),
                        reads=["ys_scr", "desti"], writes=["yg%d_%d" % (i, kx)])
                for kx in range(2):
                    P.op("dve", lambda e, ti=ti, i=i, kx=kx: e.scalar_tensor_tensor(
                        out=xt[i][:], in0=yg[i][kx][:], scalar=comb[:, ti, kx:kx + 1], in1=xt[i][:],
                        op0=ALU.mult, op1=ALU.add), reads=["yg%d_%d" % (i, kx), "comb", "xt%d" % i], writes=["xt%d" % i])
                rms_scale(xt[i], "xt%d" % i, i, gA, "gA", of[i], "of%d" % i)
                finals.append(P.dma("sp", lambda e, ti=ti, i=i: e.dma_start(out=out_d[ti * 128:(ti + 1) * 128, :], in_=of[i][:]),
                                    reads=["of%d" % i]))
        P.finish(finals)
    return nc


def _consts():
    p = np.arange(128, dtype=np.float64)
    slopes = np.array([2.0 ** (-8.0 * (i + 1) / HEADS) for i in range(HEADS)])
    ident = np.eye(128)
    tri = (p[:, None] <= p[None, :]).astype(np.float64)
    ust = (p[:, None] < p[None, :]).astype(np.float64)
    bias = np.stack([slopes[None, :] * (p[:, None] - 255.0), slopes[None, :] * (p[:, None] - 127.0),
                     slopes[None, :] * p[:, None]], axis=-1)
    facd = np.stack([np.exp(-slopes[None, :] * p[:, None]), np.exp(-slopes[None, :] * (p[:, None] + 1.0))], axis=-1)
    qpos = 6144.0 + np.arange(16)[None, :, None] * 128.0 + p[:, None, None]
    kend = (np.arange(NBLK) * 256.0 + 255.0)[None, None, :]
    dist = np.maximum(qpos - kend, 0.0)
    return dict(
        ident_bf=ident.astype(ml_dtypes.bfloat16), ident_f=ident.astype(np.float32),
        tri_bf=tri.astype(ml_dtypes.bfloat16), ustrict_bf=ust.astype(ml_dtypes.bfloat16),
        bias_tab=bias.astype(np.float32), facd_tab=facd.astype(np.float32), dist_tab=dist.astype(np.float32),
        pidx=p.astype(np.float32)[:, None],
        blkstart=np.broadcast_to((np.arange(MOE_BLOCKS) * 128.0)[None, :], (128, MOE_BLOCKS)).astype(np.float32).copy())


def kernel(x, norm1_g, w_in, conv_dw_w, conv_dw_b, conv_ln_g, conv_ln_b, w_conv_out, w_attn_out, gate_b, w_out,
           norm2_g, w_router_group, b_router_group, w_router_expert, b_router_expert, w_exp_gate, w_exp_up,
           w_exp_down, norm_f_g):
    f = lambda a: np.ascontiguousarray(np.asarray(a, dtype=np.float32))
    x = f(x)
    fm = lambda v, n: f(v).reshape(n, 128).T.copy()
    shared = dict(
        w_in=f(w_in)[0], w_conv_out=f(w_conv_out)[0], w_attn_out=f(w_attn_out)[0], w_out=f(w_out)[0],
        w_exp_gate=f(w_exp_gate)[0], w_exp_up=f(w_exp_up)[0], w_exp_down=f(w_exp_down)[0],
        norm1_g=f(norm1_g).reshape(1, D), norm2_g=f(norm2_g).reshape(1, D), norm_f_g=f(norm_f_g).reshape(1, D),
        dw_w=np.ascontiguousarray(f(conv_dw_w)[0].T.reshape(8, 128, TAPS).transpose(1, 0, 2)),
        chvec=np.ascontiguousarray(np.stack([fm(conv_dw_b, 8), fm(conv_ln_g, 8), fm(conv_ln_b, 8)], axis=-1)),
        gate_b=fm(gate_b, 32),
        w_router=np.ascontiguousarray(np.concatenate([f(w_router_group)[0], f(w_router_expert)[0]], axis=1)
                                      .reshape(NCH, 128, 72).transpose(1, 0, 2)),
        b_router=np.concatenate([f(b_router_group).reshape(-1), f(b_router_expert).reshape(-1)])[None, :].copy(),
    )
    shared.update(_consts())
    in_maps = []
    for c in range(8):
        b, r = c // 4, c % 4
        xwin = np.zeros((SEQ, D), np.float32)
        n_valid = (r + 1) * NOWN
        xwin[SEQ - n_valid:] = x[b, :n_valid]
        first_valid_blk = (SEQ - n_valid) // 256
        gm = np.full((128, 16, NBLK), -1e30, np.float32)
        for qt in range(16):
            own = (6144 + qt * 128) // 256
            gm[:, qt, first_valid_blk:own] = 0.0
        m = dict(shared)
        m["xw"] = xwin
        m["gmask"] = gm
        in_maps.append(m)
    nc = build()
    res = run_bass_kernel_spmd(nc, in_maps, core_ids=list(range(8)))
    out = np.zeros((2, SEQ, D), np.float32)
    for c in range(8):
        b, r = c // 4, c % 4
        out[b, r * NOWN:(r + 1) * NOWN] = res.results[c]["out"]
    return out
```
